# Optimizing a Trainium2 kernel written in Bass

```python
import math
import jax, jax.numpy as jnp
from jax import lax
import numpy as np

D_MODEL = 2048
BATCH = 16
SEQ = 2048
DEPTH = 2

N_HEADS = 16
N_KV_HEADS = 2
HEAD_DIM = 64
WINDOW = 128
BLOCK = 128
ATTN_WIDTH = N_HEADS * HEAD_DIM
KV_WIDTH = N_KV_HEADS * HEAD_DIM
SSM_WIDTH = D_MODEL // 4
SSM_GROUP = 16
N_SSM_GROUPS = SSM_WIDTH // SSM_GROUP
SSM_STATE = 64
DT_MIN = 1e-3
DT_MAX = 1e-1
N_MEM = 256
MEM_HEADS = 4
MEM_HEAD_DIM = 128
MEM_WIDTH = MEM_HEADS * MEM_HEAD_DIM
N_BRANCHES = 3
MIX_WIDTH = ATTN_WIDTH + SSM_WIDTH + MEM_WIDTH
Q_END = ATTN_WIDTH
K_END = Q_END + KV_WIDTH
V_END = K_END + KV_WIDTH
U_END = V_END + SSM_WIDTH
QM_END = U_END + MEM_WIDTH
IN_WIDTH = QM_END + N_BRANCHES * D_MODEL
N_EXPERTS = 32
TOP_K = 4
D_FF = D_MODEL // 2
MOE_BLOCK = 512
SWIGLU_ALPHA = 1.702
SWIGLU_LIMIT = 7.0
LN_EPS = 1e-5
DEEPNORM_ALPHA = (2.0 * DEPTH) ** 0.25
DEEPNORM_BETA = (8.0 * DEPTH) ** -0.25

kernel_name = "hybrid_swa_s5_memxattn_moe_deepnorm"


def layer_norm(x, g, b):
    xf = x.astype(jnp.float32)
    mu = xf.mean(-1, keepdims=True)
    var = jnp.square(xf - mu).mean(-1, keepdims=True)
    y = (xf - mu) * lax.rsqrt(var + LN_EPS) * g.astype(jnp.float32) + b.astype(jnp.float32)
    return y.astype(x.dtype)


def sliding_window_attention(q, k, v, sinks):
    b, s = q.shape[:2]
    nb = s // BLOCK
    grp = N_HEADS // N_KV_HEADS
    qb = q.reshape(b, nb, BLOCK, N_KV_HEADS, grp, HEAD_DIM)

    def band(t):
        tb = t.reshape(b, nb, BLOCK, N_KV_HEADS, HEAD_DIM)
        prev = jnp.pad(tb[:, :-1], ((0, 0), (1, 0), (0, 0), (0, 0), (0, 0)))
        return jnp.concatenate([prev, tb], axis=2)

    kb, vb = band(k), band(v)
    scores = jnp.einsum('bnqhgd,bnkhd->bnhgqk', qb, kb,
                        preferred_element_type=jnp.float32) * (HEAD_DIM ** -0.5)
    qi = jnp.arange(BLOCK)[:, None]
    kj = jnp.arange(2 * BLOCK)[None, :]
    rel = qi + BLOCK - kj
    band_ok = (rel >= 0) & (rel < WINDOW)
    not_first = (jnp.arange(nb) > 0)[:, None, None]
    valid = band_ok[None] & (not_first | (kj >= BLOCK)[None])
    scores = jnp.where(valid[None, :, None, None], scores, -jnp.inf)
    sink = sinks.astype(jnp.float32).reshape(1, 1, N_KV_HEADS, grp, 1, 1)
    m = jnp.maximum(scores.max(-1, keepdims=True), sink)
    e = jnp.exp(scores - m)
    probs = e / (e.sum(-1, keepdims=True) + jnp.exp(sink - m))
    out = jnp.einsum('bnhgqk,bnkhd->bnqhgd', probs.astype(v.dtype), vb)
    return out.reshape(b, s, ATTN_WIDTH)


def s5_ssm(u, lambda_re, lambda_im, log_dt, b_re, b_im, c_re, c_im, d_skip):
    b, s = u.shape[:2]
    f32 = jnp.float32
    uf = u.astype(f32).reshape(b, s, N_SSM_GROUPS, SSM_GROUP)
    lam = lax.complex(lambda_re.astype(f32), lambda_im.astype(f32))
    dt = jnp.exp(log_dt.astype(f32))[:, None]
    lam_bar = jnp.exp(lam * dt)
    b_mat = lax.complex(b_re.astype(f32), b_im.astype(f32))
    b_bar = ((lam_bar - 1.0) / lam)[..., None] * b_mat
    bu = jnp.einsum('gph,bsgh->bsgp', b_bar, uf.astype(jnp.complex64))

    def combine(left, right):
        a_l, x_l = left
        a_r, x_r = right
        return a_r * a_l, a_r * x_l + x_r

    a = jnp.broadcast_to(lam_bar, bu.shape)
    _, states = lax.associative_scan(combine, (a, bu), axis=1)
    c_mat = lax.complex(c_re.astype(f32), c_im.astype(f32))
    y = jnp.einsum('ghp,bsgp->bsgh', c_mat, states).real + d_skip.astype(f32) * uf
    return y.reshape(b, s, SSM_WIDTH).astype(u.dtype)


def memory_attention(q, mem_k, mem_v):
    b, s = q.shape[:2]
    scores = jnp.einsum('bshd,bmhd->bhsm', q, mem_k,
                        preferred_element_type=jnp.float32) * (MEM_HEAD_DIM ** -0.5)
    probs = jax.nn.softmax(scores, axis=-1).astype(mem_v.dtype)
    return jnp.einsum('bhsm,bmhd->bshd', probs, mem_v).reshape(b, s, MEM_WIDTH)


def mixer(h, mem, w_in, sinks, lambda_re, lambda_im, log_dt, b_re, b_im, c_re, c_im,
          d_skip, w_glu, w_mem_kv, w_branch, w_out):
    b, s, _ = h.shape
    proj = h @ w_in
    q = proj[..., :Q_END].reshape(b, s, N_HEADS, HEAD_DIM)
    k = proj[..., Q_END:K_END].reshape(b, s, N_KV_HEADS, HEAD_DIM)
    v = proj[..., K_END:V_END].reshape(b, s, N_KV_HEADS, HEAD_DIM)
    u = proj[..., V_END:U_END]
    qm = proj[..., U_END:QM_END].reshape(b, s, MEM_HEADS, MEM_HEAD_DIM)
    gates = jax.nn.sigmoid(proj[..., QM_END:].reshape(b, s, N_BRANCHES, D_MODEL))

    attn_out = sliding_window_attention(q, k, v, sinks)

    z = jax.nn.gelu(s5_ssm(u, lambda_re, lambda_im, log_dt, b_re, b_im, c_re, c_im, d_skip))
    zg = z @ w_glu
    ssm_out = zg[..., :SSM_WIDTH] * jax.nn.sigmoid(zg[..., SSM_WIDTH:])

    mem_kv = (mem @ w_mem_kv).reshape(b, N_MEM, 2, MEM_HEADS, MEM_HEAD_DIM)
    mem_out = memory_attention(qm, mem_kv[:, :, 0], mem_kv[:, :, 1])

    br_attn = attn_out @ w_branch[:ATTN_WIDTH]
    br_ssm = ssm_out @ w_branch[ATTN_WIDTH:ATTN_WIDTH + SSM_WIDTH]
    br_mem = mem_out @ w_branch[ATTN_WIDTH + SSM_WIDTH:]
    merged = gates[:, :, 0] * br_attn + gates[:, :, 1] * br_ssm + gates[:, :, 2] * br_mem
    return merged @ w_out


def moe_ffn(h, w_router, b_router, w_up, b_up, w_down, b_down):
    b, s, d = h.shape
    hf = h.reshape(-1, d)
    n_tok = hf.shape[0]
    n_pairs = n_tok * TOP_K
    n_blocks = -(-n_pairs // MOE_BLOCK) + N_EXPERTS
    logits = (hf @ w_router).astype(jnp.float32) + b_router.astype(jnp.float32)
    top_logits, top_idx = lax.top_k(logits, TOP_K)
    gate = jax.nn.softmax(top_logits, axis=-1)
    flat_e = top_idx.reshape(-1)
    order = jnp.argsort(flat_e)
    sorted_e = flat_e[order]
    tok = order // TOP_K
    counts = jnp.bincount(flat_e, length=N_EXPERTS).astype(jnp.int32)
    padded = (counts + MOE_BLOCK - 1) // MOE_BLOCK * MOE_BLOCK
    start = jnp.cumsum(counts) - counts
    pad_end = jnp.cumsum(padded)
    pad_start = pad_end - padded
    dest = pad_start[sorted_e] + jnp.arange(n_pairs, dtype=jnp.int32) - start[sorted_e]
    x_pad = jnp.zeros((n_blocks * MOE_BLOCK, d), hf.dtype).at[dest].set(hf[tok])
    block_rows = jnp.arange(n_blocks, dtype=jnp.int32) * MOE_BLOCK
    block_e = jnp.minimum(jnp.searchsorted(pad_end, block_rows, side='right'),
                          N_EXPERTS - 1).astype(jnp.int32)

    def expert_block(args):
        xb, e = args
        up = xb @ w_up[e] + b_up[e]
        x_glu = jnp.minimum(up[:, ::2], SWIGLU_LIMIT)
        x_lin = jnp.clip(up[:, 1::2], -SWIGLU_LIMIT, SWIGLU_LIMIT)
        act = x_glu * jax.nn.sigmoid(SWIGLU_ALPHA * x_glu) * (x_lin + 1.0)
        return act @ w_down[e] + b_down[e]

    out_pad = lax.map(expert_block, (x_pad.reshape(n_blocks, MOE_BLOCK, d), block_e))
    out = out_pad.reshape(-1, d)[dest] * gate.reshape(-1)[order][:, None].astype(hf.dtype)
    y = jax.ops.segment_sum(out, tok, num_segments=n_tok)
    return y.reshape(b, s, d)


def setup_inputs(seed: int = 0) -> dict:
    key = jax.random.key(seed)
    ks = jax.random.split(key, 32)
    L, D, G, P, H = DEPTH, D_MODEL, N_SSM_GROUPS, SSM_STATE, SSM_GROUP
    nrm = jax.random.normal
    branch_row_scale = jnp.concatenate([
        jnp.full((ATTN_WIDTH,), ATTN_WIDTH ** -0.5, jnp.float32),
        jnp.full((SSM_WIDTH,), SSM_WIDTH ** -0.5, jnp.float32),
        jnp.full((MEM_WIDTH,), MEM_WIDTH ** -0.5, jnp.float32)])
    return {
        "x": nrm(ks[0], (BATCH, SEQ, D), jnp.float32),
        "mem": nrm(ks[1], (BATCH, N_MEM, D), jnp.float32),
        "ln_in_g": 1.0 + 0.02 * nrm(ks[2], (D,), jnp.float32),
        "ln_in_b": 0.02 * nrm(ks[3], (D,), jnp.float32),
        "w_in": nrm(ks[4], (L, D, IN_WIDTH), jnp.float32) * D ** -0.5,
        "attn_sinks": 0.5 * nrm(ks[5], (L, N_HEADS), jnp.float32),
        "ssm_lambda_re": -0.5 + 0.01 * nrm(ks[6], (L, G, P), jnp.float32),
        "ssm_lambda_im": jnp.pi * jnp.arange(P, dtype=jnp.float32) + 0.01 * nrm(ks[7], (L, G, P), jnp.float32),
        "ssm_log_dt": jax.random.uniform(ks[8], (L, G), jnp.float32, math.log(DT_MIN), math.log(DT_MAX)),
        "ssm_b_re": nrm(ks[9], (L, G, P, H), jnp.float32) * (2 * H) ** -0.5,
        "ssm_b_im": nrm(ks[10], (L, G, P, H), jnp.float32) * (2 * H) ** -0.5,
        "ssm_c_re": nrm(ks[11], (L, G, H, P), jnp.float32) * P ** -0.5,
        "ssm_c_im": nrm(ks[12], (L, G, H, P), jnp.float32) * P ** -0.5,
        "ssm_d": nrm(ks[13], (L, G, H), jnp.float32),
        "w_glu": nrm(ks[14], (L, SSM_WIDTH, 2 * SSM_WIDTH), jnp.float32) * SSM_WIDTH ** -0.5,
        "w_mem_kv": nrm(ks[15], (L, D, 2 * MEM_WIDTH), jnp.float32) * D ** -0.5,
        "w_branch": nrm(ks[16], (L, MIX_WIDTH, D), jnp.float32) * branch_row_scale[None, :, None],
        "w_out": nrm(ks[17], (L, D, D), jnp.float32) * (D ** -0.5 * DEEPNORM_BETA),
        "ln1_g": 1.0 + 0.02 * nrm(ks[18], (L, D), jnp.float32),
        "ln1_b": 0.02 * nrm(ks[19], (L, D), jnp.float32),
        "w_router": nrm(ks[20], (L, D, N_EXPERTS), jnp.float32) * D ** -0.5,
        "b_router": 0.01 * nrm(ks[21], (L, N_EXPERTS), jnp.float32),
        "w_up": nrm(ks[22], (L, N_EXPERTS, D, 2 * D_FF), jnp.float32) * D ** -0.5,
        "b_up": 0.01 * nrm(ks[23], (L, N_EXPERTS, 2 * D_FF), jnp.float32),
        "w_down": nrm(ks[24], (L, N_EXPERTS, D_FF, D), jnp.float32) * (D_FF ** -0.5 * DEEPNORM_BETA),
        "b_down": 0.01 * nrm(ks[25], (L, N_EXPERTS, D), jnp.float32),
        "ln2_g": 1.0 + 0.02 * nrm(ks[26], (L, D), jnp.float32),
        "ln2_b": 0.02 * nrm(ks[27], (L, D), jnp.float32),
    }


def reference(x, mem, ln_in_g, ln_in_b, w_in, attn_sinks, ssm_lambda_re, ssm_lambda_im,
              ssm_log_dt, ssm_b_re, ssm_b_im, ssm_c_re, ssm_c_im, ssm_d, w_glu, w_mem_kv,
              w_branch, w_out, ln1_g, ln1_b, w_router, b_router, w_up, b_up, w_down, b_down,
              ln2_g, ln2_b):
    h = layer_norm(x, ln_in_g, ln_in_b)
    for l in range(DEPTH):
        mix = mixer(h, mem, w_in[l], attn_sinks[l], ssm_lambda_re[l], ssm_lambda_im[l],
                    ssm_log_dt[l], ssm_b_re[l], ssm_b_im[l], ssm_c_re[l], ssm_c_im[l],
                    ssm_d[l], w_glu[l], w_mem_kv[l], w_branch[l], w_out[l])
        h = layer_norm(DEEPNORM_ALPHA * h + mix, ln1_g[l], ln1_b[l])
        ffn = moe_ffn(h, w_router[l], b_router[l], w_up[l], b_up[l], w_down[l], b_down[l])
        h = layer_norm(DEEPNORM_ALPHA * h + ffn, ln2_g[l], ln2_b[l])
    return h
```

```python
import numpy as np
from contextlib import ExitStack
import concourse.bass as bass
import concourse.mybir as mybir
from concourse.bass_utils import run_bass_kernel_spmd

F32 = mybir.dt.float32
BF16 = mybir.dt.bfloat16
I32 = mybir.dt.int32
ALU = mybir.AluOpType
AF = mybir.ActivationFunctionType
AX = mybir.AxisListType

NCORES = 8
D = 2048
S = 2048
NSEQ = 2
T = NSEQ * S
BLK = 512
NBLK = T // BLK
L = 2
NE = 32
CAP = 768
NSLOT = NE * CAP
EPS = 1e-5
ALPHA = (2.0 * L) ** 0.25
NT_IN = 10
ENG = ("pe", "act", "dve", "pool", "sp")
TWO_PI = 6.283185307179586
MAGIC = 12582912.0


class Late:
    def __init__(self, f):
        self.f = f


class _Rec:
    def __init__(self):
        self.__dict__["call"] = None

    def __getattr__(self, name):
        def f(*a, **k):
            self.__dict__["call"] = (name, a, k)
            return self
        return f


def _record(fn):
    r = _Rec()
    fn(r)
    return r.__dict__["call"]


class Prog:
    def __init__(self, nc, es):
        self.nc = nc
        self.es = es
        self.ops = {e: [] for e in ENG}
        self.sem = {}
        self.cnt = {}
        self.waited = {e: {} for e in ENG}
        self.res = {}
        self.nins = 0
        for e in ("pe", "act", "dve", "pool"):
            self.newsem(e)

    def newsem(self, key):
        self.sem[key] = self.es.enter_context(self.nc.semaphore("s_" + key))
        self.cnt[key] = 0

    def _deps(self, reads, writes):
        d = {}
        for r in reads:
            st = self.res.get(r)
            if st and st["w"]:
                k, v = st["w"]
                if v > d.get(k, 0):
                    d[k] = v
        for w in writes:
            st = self.res.get(w)
            if st:
                if st["w"]:
                    k, v = st["w"]
                    if v > d.get(k, 0):
                        d[k] = v
                for k, v in st["r"].items():
                    if v > d.get(k, 0):
                        d[k] = v
        return d

    def _emit_waits(self, eng, deps):
        for k, v in deps.items():
            if eng == "pe" and k == "pe":
                continue
            if self.waited[eng].get(k, 0) < v:
                self.waited[eng][k] = v
                s = self.sem[k]
                self.ops[eng].append(lambda e, s=s, v=v: e.wait_ge(s, v))
                self.nins += 1

    def _commit(self, reads, writes, dep):
        k, v = dep
        for r in reads:
            st = self.res.setdefault(r, {"w": None, "r": {}})
            if v > st["r"].get(k, 0):
                st["r"][k] = v
        for w in writes:
            self.res[w] = {"w": dep, "r": {}}

    def op(self, eng, fn, reads=(), writes=()):
        self._emit_waits(eng, self._deps(reads, writes))
        self.cnt[eng] += 1
        n = self.cnt[eng]
        s = self.sem[eng]
        name, a, k = _record(fn)
        self.ops[eng].append(lambda e, name=name, a=a, k=k, s=s: getattr(e, name)(*a, **k).then_inc(s, 1))
        self.nins += 1
        self._commit(reads, writes, (eng, n))

    def dma(self, q, key, fn, reads=(), writes=()):
        if key.startswith("c8_") or key.startswith("c9_"):
            self.nuniq = getattr(self, "nuniq", 0) + 1
            key = f"{key}u{self.nuniq}"
        if key not in self.sem:
            self.newsem(key)
        self._emit_waits(q, self._deps(reads, writes))
        self.cnt[key] += 16
        n = self.cnt[key]
        s = self.sem[key]
        name, a, k = _record(fn)

        def run(e, name=name, a=a, k=k, s=s):
            try:
                k2 = {kk: (v.f(e) if isinstance(v, Late) else v) for kk, v in k.items()}
                return getattr(e, name)(*a, **k2).then_inc(s, 16)
            except Exception:
                print("FAILED DMA", q, key, name, [(kk, getattr(v, "shape", v), getattr(v, "ap", None)) for kk, v in k.items()])
                raise
        self.ops[q].append(run)
        self.nins += 1
        self._commit(reads, writes, (key, n))

    def barrier(self):
        for eng in ENG:
            self._emit_waits(eng, dict(self.cnt))
        self.res = {}
        self.nuniq = 0

    def finish(self):
        for eng in ENG:
            self._emit_waits(eng, dict(self.cnt))
        nc = self.nc
        ops = self.ops
        with nc.Block() as block:
            @block.sync
            def _(e):
                for f in ops["sp"]:
                    f(e)

            @block.tensor
            def _(e):
                for f in ops["pe"]:
                    f(e)

            @block.vector
            def _(e):
                for f in ops["dve"]:
                    f(e)

            @block.scalar
            def _(e):
                for f in ops["act"]:
                    f(e)

            @block.gpsimd
            def _(e):
                for f in ops["pool"]:
                    f(e)


class WStream:
    def __init__(self, P, ring, nslots, plan):
        self.P = P
        self.ring = ring
        self.n = nslots
        self.plan = plan
        self.issued = 0
        self.pos = 0

    def _issue(self, i):
        key, src = self.plan[i]
        slot = i % self.n
        nel = src.shape[1]
        dst = self.ring[:, slot, 0:nel]
        if nel > 2048:
            assert nel % 2048 == 0
            src = src.rearrange("p (a b) -> p a b", b=2048)
            dst = dst.rearrange("p (a b) -> p a b", b=2048)
        self.P.dma("pool", f"w{slot}", lambda e, d=dst, s=src: e.dma_start(out=d, in_=s),
                   writes=[f"w{slot}"])

    def next(self, key, held=0):
        i = self.pos
        assert self.plan[i][0] == key, (self.plan[i][0], key)
        while self.issued < min(len(self.plan), i - held + self.n):
            self._issue(self.issued)
            self.issued += 1
        self.pos += 1
        slot = i % self.n
        return self.ring[:, slot, :], f"w{slot}"


def plan_all(nlayers, stop=None):
    pl = []
    for l in range(nlayers):
        for blk in range(NBLK):
            pl += [("win", l, 6), ("win", l, 7), ("wglu", l)]
        if stop == "ssm":
            break
        for blk in range(NBLK):
            if blk % 4 == 0:
                pl += [("wmkv", l, i) for i in range(4)]
            pl += [("win", l, i) for i in (0, 1, 2, 3, 4, 5, 8, 9)]
            for jj in range(8):
                pl += [("win", l, NT_IN + jj * 3 + b) for b in range(3)] + [("wbr", l, jj)]
            pl += [("wout", l, c) for c in range(8)]
        if stop == "mix":
            break
        for e in range(NE):
            pl += [("wup", l, e, j) for j in range(8)] + [("wdn", l, e, k) for k in range(8)]
    return pl


INPUT_SPECS = [
    ("x", (T, D)), ("mem", (NSEQ * 256, D)), ("lnin", (2, 128, D)), ("lnp", (L, 4, 128, D)),
    ("win", (L, NT_IN + 24, 128, 4096)), ("wbr", (L, 8, 128, 4096)), ("wout", (L, 8, 128, 4096)),
    ("wglu", (L, 128, 4096)), ("wmkv", (L, 4, 128, 4096)),
    ("wr", (L, 128, 16 * 32)), ("br", (L, 128, 32)),
    ("wup", (L, NE, 8, 128, 4096)), ("wdn", (L, NE, 8, 128, 2048)),
    ("bup", (L, 128, NE * 16)), ("bdn", (L, NE, D)),
    ("sinks", (L, 128, 16)),
    ("ident", (128, 128)), ("ltri", (128, 128)), ("ltris", (128, 128)), ("ones", (128, 128)),
    ("mask_swa", (128, 256)), ("mask_first", (128, 256)), ("ebase", (128, NE)),
    ("sp1", (128, 1)), ("tp1", (128, 128)), ("bmask", (128, 512)),
    ("s_tok", (L, 3, 128, 2048)), ("s_cm", (L, 3, 128, 16)), ("s_b", (L, 5, 128, 256)),
    ("s_c", (L, 2, 128, 16 * 64)), ("s_d", (L, 128, 4)),
]


def build(nlayers=L, debug=(), stop=None):
    nc = bass.Bass("TRN2", target_bir_lowering=False)
    es = ExitStack()
    P = Prog(nc, es)
    din = {}
    for name, shape in INPUT_SPECS:
        if stop in ("ln0", "ssm", "mix") and name in ("wup", "wdn"):
            continue
        din[name] = nc.dram_tensor(name, list(shape), F32, kind="ExternalInput").ap()
    out = nc.dram_tensor("out", [T, D], F32, kind="ExternalOutput").ap()
    skind = "ExternalOutput" if debug else "Internal"
    h_tok = nc.dram_tensor("h_tok", [T, D], F32, kind=skind).ap()
    xg = nc.dram_tensor("xg", [NSLOT, D], BF16, kind=skind).ap()
    ypad = nc.dram_tensor("ypad", [NSLOT, D], BF16, kind=skind).ap()
    ssm_scr = nc.dram_tensor("ssm_scr", [NBLK, 128, 4 * BLK], BF16, kind=skind).ap()
    dbg = {}

    def dbg_out(name, shape, dt=F32):
        dbg[name] = nc.dram_tensor("dbg_" + name, list(shape), dt, kind="ExternalOutput").ap()
        return dbg[name]

    uid = [0]

    def alloc(stack, name, shape, dt=F32):
        uid[0] += 1
        return stack.enter_context(nc.sbuf_tensor(f"sb{uid[0]}_{name}", list(shape), dt))

    NRING = 6
    ring = alloc(es, "ring", [128, NRING, 4096], BF16)
    psum = [es.enter_context(nc.psum_tensor(f"ps{i}", [128, 512], F32)) for i in range(8)]
    ps_ctr = [0]

    def ps_next():
        i = ps_ctr[0] % 8
        ps_ctr[0] += 1
        return psum[i], f"ps{i}"

    identf = alloc(es, "identf", [128, 128], F32)
    identb = alloc(es, "identb", [128, 128], BF16)
    ltri = alloc(es, "ltri", [128, 128], BF16)
    ltris = alloc(es, "ltris", [128, 128], BF16)
    onesb = alloc(es, "onesb", [128, 128], BF16)
    neghalf = alloc(es, "neghalf", [128, 1], F32)
    gates_all = alloc(es, "gates_all", [128, T // 128, 4], F32)
    dest_all = alloc(es, "dest_all", [128, T // 128, 4], I32)
    gmat_all = alloc(es, "gmat_all", [128, T // 128, NE], F32)

    P.dma("sp", "c0", lambda e: e.dma_start(out=identf[:], in_=din["ident"]), writes=["identf"])
    P.dma("pool", "c1", lambda e: e.dma_start(out=identb[:], in_=din["ident"]), writes=["identb"])
    P.dma("pool", "c2", lambda e: e.dma_start(out=ltri[:], in_=din["ltri"]), writes=["ltri"])
    P.dma("pool", "c3", lambda e: e.dma_start(out=ltris[:], in_=din["ltris"]), writes=["ltris"])
    P.dma("pool", "c4", lambda e: e.dma_start(out=onesb[:], in_=din["ones"]), writes=["onesb"])
    P.op("dve", lambda e: e.memset(neghalf[:], -0.5), writes=["neghalf"])

    reg_cache = {}

    def _bc(e):
        if "bc" not in reg_cache:
            reg_cache["bc"] = e.to_reg(NSLOT - 1)
        return reg_cache["bc"]
    bc_reg = Late(_bc)

    plan = plan_all(nlayers, stop)

    def wsrc(key):
        if key[0] == "wglu":
            return din["wglu"][key[1]]
        if key[0] in ("wup", "wdn"):
            return din[key[0]][key[1], key[2], key[3]]
        return din[key[0]][key[1], key[2]]

    WS = WStream(P, ring, NRING, [(k, wsrc(k)) for k in plan])

    def layer_norm(x, xres, g, gres, b, bres, sm, tag):
        st, mv, ve, rs = sm
        for c in range(4):
            P.op("dve", lambda e, c=c: e.bn_stats(out=st[:, c, :], in_=x[:, c * 512:(c + 1) * 512]),
                 reads=[xres], writes=[f"{tag}st{c}"])
        P.op("dve", lambda e: e.bn_aggr(out=mv[:], in_=st[:].rearrange("p a b -> p (a b)")),
             reads=[f"{tag}st{c}" for c in range(4)], writes=[tag + "mv"])
        P.op("dve", lambda e: e.tensor_scalar(out=ve[:], in0=mv[:, 1:2], scalar1=EPS, scalar2=None, op0=ALU.add),
             reads=[tag + "mv"], writes=[tag + "ve"])
        P.op("pool", lambda e: e.tensor_tensor(out=rs[:], in0=ve[:], in1=neghalf[:], op=ALU.pow),
             reads=[tag + "ve", "neghalf"], writes=[tag + "rs"])
        P.op("dve", lambda e: e.tensor_scalar(out=x, in0=x, scalar1=mv[:, 0:1], scalar2=rs[:, 0:1],
                                              op0=ALU.subtract, op1=ALU.mult),
             reads=[xres, tag + "mv", tag + "rs"], writes=[xres])
        P.op("pool", lambda e: e.tensor_tensor(out=x, in0=x, in1=g, op=ALU.mult), reads=[xres, gres], writes=[xres])
        P.op("pool", lambda e: e.tensor_tensor(out=x, in0=x, in1=b, op=ALU.add), reads=[xres, bres], writes=[xres])

    def ln_small(stack, tag):
        return (alloc(stack, tag + "st", [128, 4, 6]), alloc(stack, tag + "mv", [128, 2]),
                alloc(stack, tag + "ve", [128, 1]), alloc(stack, tag + "rs", [128, 1]))

    def dump(name, ap, res, shape, dt=F32):
        if name in debug:
            d = dbg_out(name, shape, dt)
            P.dma("sp", "dbg", lambda e: e.dma_start(out=d, in_=ap), reads=[res])

    def phase_ln0():
        with ExitStack() as ph:
            xt = [alloc(ph, f"xt{i}", [128, D]) for i in range(2)]
            g = alloc(ph, "g0", [128, D])
            b = alloc(ph, "b0", [128, D])
            sm = [ln_small(ph, f"l0s{i}") for i in range(2)]
            P.dma("sp", "c5", lambda e: e.dma_start(out=g[:], in_=din["lnin"][0]), writes=["g0"])
            P.dma("sp", "c6", lambda e: e.dma_start(out=b[:], in_=din["lnin"][1]), writes=["b0"])
            for i in range(T // 128):
                s = i % 2
                P.dma("sp", f"ldx{s}", lambda e: e.dma_start(out=xt[s][:], in_=din["x"][i * 128:(i + 1) * 128, :]),
                      writes=[f"xt{s}"])
                layer_norm(xt[s][:], f"xt{s}", g[:], "g0", b[:], "b0", sm[s], f"l0{s}")
                P.dma("sp", f"stx{s}", lambda e: e.dma_start(out=h_tok[i * 128:(i + 1) * 128, :], in_=xt[s][:]),
                      reads=[f"xt{s}"], writes=[("htok", i)])
            P.barrier()

    def load_hT(blk, hT, tokf, tokb):
        for tt in range(4):
            ti = blk * 4 + tt
            s = tt % 2
            P.dma("sp", f"ldh{s}", lambda e: e.dma_start(out=tokf[s][:], in_=h_tok[ti * 128:(ti + 1) * 128, :]),
                  reads=[("htok", ti)], writes=[f"tokf{s}"])
            P.op("act", lambda e: e.activation(out=tokb[s][:], in_=tokf[s][:], func=AF.Copy),
                 reads=[f"tokf{s}"], writes=[f"tokb{s}"])
            for a in range(2):
                pb, pres = ps_next()
                pbb = pb[:].bitcast(BF16)
                for i in range(8):
                    kc = a * 8 + i
                    P.op("pe", lambda e: e.transpose(
                        out=pbb[:, i * 128:(i + 1) * 128], in_=tokb[s][:, kc * 128:(kc + 1) * 128], identity=identb[:]),
                        reads=[f"tokb{s}", "identb"], writes=[pres])
                P.op("dve", lambda e: e.tensor_copy(
                    out=hT[:, a * 8:(a + 1) * 8, tt * 128:(tt + 1) * 128],
                    in_=pbb.rearrange("p (a b) -> p a b", b=128)),
                    reads=[pres], writes=["hT"])

    def proj_tile(wt, wres, ncols_tiles, hT, evac):
        w3 = wt.rearrange("p (k c) -> p k c", c=256)
        for ct in range(ncols_tiles):
            pb, pres = ps_next()
            for kc in range(16):
                P.op("pe", lambda e: e.matmul(
                    pb[:], w3[:, kc, ct * 128:(ct + 1) * 128], hT[:, kc, :], start=(kc == 0), stop=(kc == 15)),
                    reads=[wres, "hT"], writes=[pres])
            evac(ct, pb, pres)

    def cis(A, TH, out_re, out_im, tmp, rA, rTH, rout):
        MG, Q, N1, F, SC = tmp
        P.op("act", lambda e: e.activation(out=MG, in_=A, func=AF.Exp), reads=[rA], writes=["cMG"])
        P.op("dve", lambda e: e.tensor_scalar(out=Q, in0=TH, scalar1=1.0 / TWO_PI, scalar2=None, op0=ALU.mult),
             reads=[rTH], writes=["cQ"])
        for off, o in ((0.0, out_im), (0.25, out_re)):
            P.op("dve", lambda e: e.tensor_scalar(out=N1, in0=Q, scalar1=off + MAGIC, scalar2=None, op0=ALU.add),
                 reads=["cQ"], writes=["cN1"])
            P.op("dve", lambda e: e.tensor_scalar(out=N1, in0=N1, scalar1=-MAGIC, scalar2=None, op0=ALU.add),
                 reads=["cN1"], writes=["cN1"])
            P.op("dve", lambda e: e.scalar_tensor_tensor(out=F, in0=Q, scalar=off, in1=N1, op0=ALU.add, op1=ALU.subtract),
                 reads=["cQ", "cN1"], writes=["cF"])
            P.op("act", lambda e: e.activation(out=SC, in_=F, func=AF.Sin, scale=TWO_PI * (1.0 - 1e-6)),
                 reads=["cF"], writes=["cSC"])
            P.op("dve", lambda e: e.tensor_tensor(out=o, in0=MG, in1=SC, op=ALU.mult),
                 reads=["cMG", "cSC"], writes=[rout])

    def phase_ssm(l):
        with ExitStack() as ph:
            Em = alloc(ph, "Em", [128, 4, 2, 512], BF16)
            Ep = alloc(ph, "Ep", [128, 16, 2, 128], F32)
            Bf = alloc(ph, "Bf", [128, 4, 1024], BF16)
            Cm = alloc(ph, "Cm", [128, 16, 2, 64], BF16)
            dcol = alloc(ph, "dcol", [128, 4], F32)
            P.dma("sp", "c7", lambda e: e.dma_start(out=dcol[:], in_=din["s_d"][l]), writes=["dcol"])
            with ExitStack() as tg:
                stok = alloc(tg, "stok", [128, 3, 2048], F32)
                scm = alloc(tg, "scm", [128, 3, 16], F32)
                sb = alloc(tg, "sb", [128, 5, 256], F32)
                sc = alloc(tg, "sc", [128, 2, 1024], F32)
                sp1 = alloc(tg, "sp1", [128, 1], F32)
                tp1 = alloc(tg, "tp1", [128, 128], F32)
                bmask = alloc(tg, "bmask", [128, 512], F32)
                ctmp = [alloc(tg, f"ct{i}", [128, 512], F32) for i in range(5)]
                tA = alloc(tg, "ctA", [128, 512], F32)
                tT = alloc(tg, "ctT", [128, 512], F32)
                sm = [alloc(tg, f"csm{i}", [128, 256], F32) for i in range(8)]
                arcm = alloc(tg, "arcm", [128, 16], F32)
                aicm = alloc(tg, "aicm", [128, 16], F32)
                dtcm = alloc(tg, "dtcm", [128, 16], F32)
                for i in range(3):
                    P.dma("sp", "c8_1", lambda e: e.dma_start(out=stok[:, i, :], in_=din["s_tok"][l, i]), writes=["stok"])
                    P.dma("sp", "c8_2", lambda e: e.dma_start(out=scm[:, i, :], in_=din["s_cm"][l, i]), writes=["scm"])
                for i in range(5):
                    P.dma("sp", "c8_3", lambda e: e.dma_start(out=sb[:, i, :], in_=din["s_b"][l, i]), writes=["sb"])
                for i in range(2):
                    P.dma("sp", "c8_4", lambda e: e.dma_start(out=sc[:, i, :], in_=din["s_c"][l, i]), writes=["sc"])
                P.dma("sp", "c8_5", lambda e: e.dma_start(out=sp1[:], in_=din["sp1"]), writes=["sp1"])
                P.dma("sp", "c8_6", lambda e: e.dma_start(out=tp1[:], in_=din["tp1"]), writes=["tp1"])
                P.dma("sp", "c8_7", lambda e: e.dma_start(out=bmask[:], in_=din["bmask"]), writes=["bmask"])
                for m in range(4):
                    sl = slice(m * 512, (m + 1) * 512)
                    P.op("act", lambda e: e.activation(out=ctmp[0][:], in_=stok[:, 2, sl], func=AF.Exp),
                         reads=["stok"], writes=["cdt"])
                    P.op("dve", lambda e: e.tensor_tensor(out=tA[:], in0=stok[:, 0, sl], in1=ctmp[0][:], op=ALU.mult),
                         reads=["stok", "cdt"], writes=["cA"])
                    P.op("dve", lambda e: e.tensor_tensor(out=tT[:], in0=stok[:, 1, sl], in1=ctmp[0][:], op=ALU.mult),
                         reads=["stok", "cdt"], writes=["cT"])
                    P.op("dve", lambda e: e.tensor_scalar(out=tA[:], in0=tA[:], scalar1=sp1[:, 0:1], scalar2=-1.0, op0=ALU.mult, op1=ALU.mult),
                         reads=["cA", "sp1"], writes=["cA"])
                    P.op("dve", lambda e: e.tensor_scalar(out=tT[:], in0=tT[:], scalar1=sp1[:, 0:1], scalar2=-1.0, op0=ALU.mult, op1=ALU.mult),
                         reads=["cT", "sp1"], writes=["cT"])
                    cis(tA[:], tT[:], Em[:, m, 0, :], Em[:, m, 1, :], [t[:] for t in ctmp], "cA", "cT", "Em")
                P.op("act", lambda e: e.activation(out=dtcm[:], in_=scm[:, 2, :], func=AF.Exp), reads=["scm"], writes=["dtcm"])
                P.op("dve", lambda e: e.tensor_tensor(out=arcm[:], in0=scm[:, 0, :], in1=dtcm[:], op=ALU.mult), reads=["scm", "dtcm"], writes=["arcm"])
                P.op("dve", lambda e: e.tensor_tensor(out=aicm[:], in0=scm[:, 1, :], in1=dtcm[:], op=ALU.mult), reads=["scm", "dtcm"], writes=["aicm"])

                def r3(t):
                    return t[:].rearrange("p (a b) -> p a b", b=128)
                tb = tp1[:].unsqueeze(1).broadcast_to([128, 4, 128])
                for jg in range(4):
                    P.op("dve", lambda e: e.tensor_tensor(
                        out=r3(tA), in0=tb, in1=arcm[:, 4 * jg:4 * jg + 4].unsqueeze(2).broadcast_to([128, 4, 128]), op=ALU.mult),
                        reads=["tp1", "arcm"], writes=["cA"])
                    P.op("dve", lambda e: e.tensor_tensor(
                        out=r3(tT), in0=tb, in1=aicm[:, 4 * jg:4 * jg + 4].unsqueeze(2).broadcast_to([128, 4, 128]), op=ALU.mult),
                        reads=["tp1", "aicm"], writes=["cT"])
                    cis(r3(tA), r3(tT), Ep[:, 4 * jg:4 * jg + 4, 0, :], Ep[:, 4 * jg:4 * jg + 4, 1, :],
                        [r3(t) for t in ctmp], "cA", "cT", "Ep")

                def s2(t):
                    return t[:, 0:256]
                lre, lim, ldt, bre, bim = (sb[:, i, :] for i in range(5))
                LR, LI, NR, DEN, KR, KI, X1, X2 = (t[:] for t in sm)
                P.op("act", lambda e: e.activation(out=X1, in_=ldt, func=AF.Exp), reads=["sb"], writes=["X1"])
                P.op("dve", lambda e: e.tensor_tensor(out=s2(tA), in0=lre, in1=X1, op=ALU.mult), reads=["sb", "X1"], writes=["cA"])
                P.op("dve", lambda e: e.tensor_tensor(out=s2(tT), in0=lim, in1=X1, op=ALU.mult), reads=["sb", "X1"], writes=["cT"])
                cis(s2(tA), s2(tT), LR, LI, [s2(t) for t in ctmp], "cA", "cT", "LRI")
                P.op("dve", lambda e: e.tensor_scalar(out=NR, in0=LR, scalar1=-1.0, scalar2=None, op0=ALU.add), reads=["LRI"], writes=["NR"])
                P.op("dve", lambda e: e.tensor_tensor(out=X1, in0=lre, in1=lre, op=ALU.mult), reads=["sb"], writes=["X1"])
                P.op("dve", lambda e: e.tensor_tensor(out=X2, in0=lim, in1=lim, op=ALU.mult), reads=["sb"], writes=["X2"])
                P.op("dve", lambda e: e.tensor_tensor(out=DEN, in0=X1, in1=X2, op=ALU.add), reads=["X1", "X2"], writes=["DEN"])
                P.op("dve", lambda e: e.reciprocal(out=DEN, in_=DEN), reads=["DEN"], writes=["DEN"])
                P.op("dve", lambda e: e.tensor_tensor(out=X1, in0=NR, in1=lre, op=ALU.mult), reads=["NR", "sb"], writes=["X1"])
                P.op("dve", lambda e: e.tensor_tensor(out=X2, in0=LI, in1=lim, op=ALU.mult), reads=["LRI", "sb"], writes=["X2"])
                P.op("dve", lambda e: e.tensor_tensor(out=KR, in0=X1, in1=X2, op=ALU.add), reads=["X1", "X2"], writes=["KR"])
                P.op("dve", lambda e: e.tensor_tensor(out=KR, in0=KR, in1=DEN, op=ALU.mult), reads=["KR", "DEN"], writes=["KR"])
                P.op("dve", lambda e: e.tensor_tensor(out=X1, in0=LI, in1=lre, op=ALU.mult), reads=["LRI", "sb"], writes=["X1"])
                P.op("dve", lambda e: e.tensor_tensor(out=X2, in0=NR, in1=lim, op=ALU.mult), reads=["NR", "sb"], writes=["X2"])
                P.op("dve", lambda e: e.tensor_tensor(out=KI, in0=X1, in1=X2, op=ALU.subtract), reads=["X1", "X2"], writes=["KI"])
                P.op("dve", lambda e: e.tensor_tensor(out=KI, in0=KI, in1=DEN, op=ALU.mult), reads=["KI", "DEN"], writes=["KI"])
                P.op("dve", lambda e: e.tensor_tensor(out=X1, in0=KR, in1=bre, op=ALU.mult), reads=["KR", "sb"], writes=["X1"])
                P.op("dve", lambda e: e.tensor_tensor(out=X2, in0=KI, in1=bim, op=ALU.mult), reads=["KI", "sb"], writes=["X2"])
                P.op("dve", lambda e: e.tensor_tensor(out=LR, in0=X1, in1=X2, op=ALU.subtract), reads=["X1", "X2", "LRI"], writes=["BR"])
                P.op("dve", lambda e: e.tensor_tensor(out=X1, in0=KR, in1=bim, op=ALU.mult), reads=["KR", "sb"], writes=["X1"])
                P.op("dve", lambda e: e.tensor_tensor(out=X2, in0=KI, in1=bre, op=ALU.mult), reads=["KI", "sb"], writes=["X2"])
                P.op("dve", lambda e: e.tensor_tensor(out=LI, in0=X1, in1=X2, op=ALU.add), reads=["X1", "X2", "BR"], writes=["BI"])
                bm3 = bmask[:].rearrange("p (a b) -> p a b", b=64)
                for kc in range(4):
                    for r, src, rn in ((0, LR, "BR"), (1, LI, "BI")):
                        P.op("dve", lambda e: e.tensor_tensor(
                            out=Bf[:, kc, r * 512:(r + 1) * 512].rearrange("p (a b) -> p a b", b=64),
                            in0=src[:, kc * 64:(kc + 1) * 64].unsqueeze(1).broadcast_to([128, 8, 64]), in1=bm3, op=ALU.mult),
                            reads=[rn, "bmask"], writes=["Bf"])
                P.op("dve", lambda e: e.tensor_copy(out=Cm[:, :, 0, :], in_=sc[:, 0, :].rearrange("p (a b) -> p a b", b=64)),
                     reads=["sc"], writes=["Cm"])
                P.op("dve", lambda e: e.tensor_scalar(out=Cm[:, :, 1, :], in0=sc[:, 1, :].rearrange("p (a b) -> p a b", b=64),
                                                      scalar1=-1.0, scalar2=None, op0=ALU.mult),
                     reads=["sc"], writes=["Cm"])
                dump("Em", Em[:], "Em", [128, 4, 2, 512], BF16)
                dump("Ep", Ep[:], "Ep", [128, 16, 2, 128])
                dump("Bf", Bf[:], "Bf", [128, 4, 1024], BF16)
                dump("Cm", Cm[:], "Cm", [128, 16, 2, 64], BF16)
                P.barrier()
            hT = alloc(ph, "hT", [128, 16, BLK], BF16)
            tokf = [alloc(ph, f"tokf{i}", [128, D], F32) for i in range(2)]
            tokb = [alloc(ph, f"tokb{i}", [128, D], BF16) for i in range(2)]
            uT = alloc(ph, "uT", [128, 4, BLK], BF16)
            stt = [alloc(ph, f"st{i}", [128, 512], F32) for i in range(6)]
            wri = [[alloc(ph, f"w{a}{r}", [128, 512], BF16) for r in range(2)] for a in range(2)]
            x32 = [alloc(ph, f"x32{r}", [128, 4, 128], F32) for r in range(2)]
            Sc = alloc(ph, "Sc", [128, 4, 4, 2], F32)
            xTq = alloc(ph, "xTq", [128, 4, 2, BLK], BF16)
            zT = alloc(ph, "zT", [128, 4, BLK], BF16)
            soT = alloc(ph, "soT", [128, 4, BLK], BF16)
            t3 = [t[:].rearrange("p (a b) -> p a b", b=128) for t in stt]
            par = 0
            for blk in range(NBLK):
                if blk % 4 == 0:
                    P.op("dve", lambda e: e.memset(Sc[:], 0.0), writes=["Sc"])
                load_hT(blk, hT, tokf, tokb)
                for i, tno in enumerate((6, 7)):
                    wt, wres = WS.next(("win", l, tno))

                    def ev_u(ct, pb, pres):
                        P.op("act", lambda e: e.activation(out=uT[:, 2 * i + ct, :], in_=pb[:], func=AF.Copy),
                             reads=[pres], writes=["uT"])
                    proj_tile(wt, wres, 2, hT, ev_u)
                if blk == 0:
                    dump("uT", uT[:], "uT", [128, 4, BLK], BF16)
                for m in range(4):
                    for ck in range(4):
                        tr = slice(ck * 128, (ck + 1) * 128)
                        pB = [ps_next() for _ in range(2)]
                        for r in range(2):
                            P.op("pe", lambda e: e.matmul(
                                pB[r][0][:], uT[:, m, tr], Bf[:, m, r * 512:(r + 1) * 512], start=True, stop=True),
                                reads=["uT", "Bf"], writes=[pB[r][1]])
                        w_ = wri[par]
                        wn = [f"w{par}{r}" for r in range(2)]
                        Er, Ei = Em[:, m, 0, :], Em[:, m, 1, :]
                        P.op("dve", lambda e: e.tensor_tensor(out=stt[0][:], in0=pB[0][0][:], in1=Er, op=ALU.mult),
                             reads=[pB[0][1], "Em"], writes=["st0"])
                        P.op("dve", lambda e: e.tensor_tensor(out=stt[1][:], in0=pB[1][0][:], in1=Ei, op=ALU.mult),
                             reads=[pB[1][1], "Em"], writes=["st1"])
                        P.op("pool", lambda e: e.tensor_tensor(out=w_[0][:], in0=stt[0][:], in1=stt[1][:], op=ALU.subtract),
                             reads=["st0", "st1"], writes=[wn[0]])
                        P.op("dve", lambda e: e.tensor_tensor(out=stt[2][:], in0=pB[0][0][:], in1=Ei, op=ALU.mult),
                             reads=[pB[0][1], "Em"], writes=["st2"])
                        P.op("dve", lambda e: e.tensor_tensor(out=stt[3][:], in0=pB[1][0][:], in1=Er, op=ALU.mult),
                             reads=[pB[1][1], "Em"], writes=["st3"])
                        P.op("pool", lambda e: e.tensor_tensor(out=w_[1][:], in0=stt[2][:], in1=stt[3][:], op=ALU.add),
                             reads=["st2", "st3"], writes=[wn[1]])
                        pZ = [ps_next() for _ in range(2)]
                        for r in range(2):
                            for ct in range(4):
                                P.op("pe", lambda e: e.matmul(
                                    pZ[r][0][:, ct * 128:(ct + 1) * 128], w_[r][:, ct * 128:(ct + 1) * 128], ltri[:],
                                    start=True, stop=True),
                                    reads=[wn[r], "ltri"], writes=[pZ[r][1]])
                        par ^= 1
                        Epr, Epi = Ep[:, 4 * m:4 * m + 4, 0, :], Ep[:, 4 * m:4 * m + 4, 1, :]
                        for r in range(2):
                            P.op("dve", lambda e: e.tensor_tensor(
                                out=t3[r], in0=pZ[r][0][:].rearrange("p (a b) -> p a b", b=128),
                                in1=Sc[:, m, :, r:r + 1].broadcast_to([128, 4, 128]), op=ALU.add),
                                reads=[pZ[r][1], "Sc"], writes=[f"st{r}"])
                        P.op("pool", lambda e: e.tensor_tensor(out=t3[2], in0=t3[0], in1=Epr, op=ALU.mult), reads=["st0", "Ep"], writes=["st2"])
                        P.op("pool", lambda e: e.tensor_tensor(out=t3[3], in0=t3[1], in1=Epi, op=ALU.mult), reads=["st1", "Ep"], writes=["st3"])
                        P.op("dve", lambda e: e.tensor_tensor(out=x32[0][:], in0=t3[2], in1=t3[3], op=ALU.subtract), reads=["st2", "st3"], writes=["x320"])
                        P.op("pool", lambda e: e.tensor_tensor(out=t3[4], in0=t3[0], in1=Epi, op=ALU.mult), reads=["st0", "Ep"], writes=["st4"])
                        P.op("pool", lambda e: e.tensor_tensor(out=t3[5], in0=t3[1], in1=Epr, op=ALU.mult), reads=["st1", "Ep"], writes=["st5"])
                        P.op("dve", lambda e: e.tensor_tensor(out=x32[1][:], in0=t3[4], in1=t3[5], op=ALU.add), reads=["st4", "st5"], writes=["x321"])
                        for r in range(2):
                            P.op("act", lambda e: e.activation(out=Sc[:, m, :, r:r + 1], in_=x32[r][:, :, 127:128], func=AF.Copy),
                                 reads=[f"x32{r}"], writes=["Sc"])
                            P.op("act", lambda e: e.activation(out=xTq[:, :, r, tr], in_=x32[r][:], func=AF.Copy),
                                 reads=[f"x32{r}"], writes=["xTq"])
                    pY, pYr = ps_next()
                    for ct in range(4):
                        for r in range(2):
                            P.op("pe", lambda e: e.matmul(
                                pY[64 * (ct // 2):64 * (ct // 2) + 64, :], Cm[:, 4 * m + ct, r, :], xTq[:, ct, r, :],
                                start=(ct % 2 == 0 and r == 0), stop=(ct % 2 == 1 and r == 1)),
                                reads=["Cm", "xTq"], writes=[pYr])
                    y, x2, inn, sg = stt[0][:], stt[1][:], stt[2][:], stt[3][:]
                    P.op("dve", lambda e: e.scalar_tensor_tensor(out=y, in0=uT[:, m, :], scalar=dcol[:, m:m + 1], in1=pY[:],
                                                                 op0=ALU.mult, op1=ALU.add),
                         reads=["uT", "dcol", pYr], writes=["st0"])
                    if blk == 0:
                        dump(f"yssm{m}", y, "st0", [128, 512])
                    P.op("pool", lambda e: e.tensor_tensor(out=x2, in0=y, in1=y, op=ALU.mult), reads=["st0"], writes=["st1"])
                    P.op("dve", lambda e: e.tensor_scalar(out=x2, in0=x2, scalar1=0.044715, scalar2=1.0, op0=ALU.mult, op1=ALU.add),
                         reads=["st1"], writes=["st1"])
                    P.op("pool", lambda e: e.tensor_tensor(out=inn, in0=x2, in1=y, op=ALU.mult), reads=["st0", "st1"], writes=["st2"])
                    P.op("act", lambda e: e.activation(out=sg, in_=inn, func=AF.Sigmoid, scale=1.5957691216057308),
                         reads=["st2"], writes=["st3"])
                    P.op("dve", lambda e: e.tensor_tensor(out=zT[:, m, :], in0=y, in1=sg, op=ALU.mult),
                         reads=["st0", "st3"], writes=["zT"])
                wt, wres = WS.next(("wglu", l))
                wg = wt.rearrange("p (k c) -> p k c", c=1024)
                for i in range(4):
                    pV, pG = ps_next(), ps_next()
                    for (pb, pres), off in ((pV, 0), (pG, 512)):
                        for kc in range(4):
                            P.op("pe", lambda e: e.matmul(
                                pb[:], wg[:, kc, off + i * 128:off + (i + 1) * 128], zT[:, kc, :], start=(kc == 0), stop=(kc == 3)),
                                reads=[wres, "zT"], writes=[pres])
                    P.op("act", lambda e: e.activation(out=stt[4][:], in_=pG[0][:], func=AF.Sigmoid), reads=[pG[1]], writes=["st4"])
                    P.op("dve", lambda e: e.tensor_tensor(out=soT[:, i, :], in0=pV[0][:], in1=stt[4][:], op=ALU.mult),
                         reads=[pV[1], "st4"], writes=["soT"])
                P.dma("sp", "sso", lambda e: e.dma_start(out=ssm_scr[blk], in_=soT[:].rearrange("p a b -> p (a b)")),
                      reads=["soT"], writes=[("ssm_scr", blk)])
            P.barrier()

    def phase_mix(l):
        with ExitStack() as ph:
            hT = alloc(ph, "hT", [128, 16, BLK], BF16)
            tokf = [alloc(ph, f"tokf{i}", [128, D], F32) for i in range(2)]
            tokb = [alloc(ph, f"tokb{i}", [128, D], BF16) for i in range(2)]
            qT = alloc(ph, "qT", [128, 8, BLK], BF16)
            kTd = alloc(ph, "kTd", [128, 2, 640], BF16)
            v_tok = alloc(ph, "v_tok", [128, 5, 128], BF16)
            qmT = alloc(ph, "qmT", [128, 4, BLK], BF16)
            aoT = alloc(ph, "aoT", [128, 8, BLK], BF16)
            moT = alloc(ph, "moT", [128, 4, BLK], BF16)
            soT = alloc(ph, "soT", [128, 4, BLK], BF16)
            mergedT = alloc(ph, "mergedT", [128, 16, BLK], BF16)
            wk = alloc(ph, "wk", [128, 4096], F32)
            PT = alloc(ph, "PT", [128, 16, 128], BF16)
            mem_kT = alloc(ph, "mem_kT", [128, 4, 256], BF16)
            mem_v = alloc(ph, "mem_v", [128, 2, 512], BF16)
            lng = alloc(ph, "lng", [128, D], F32)
            lnb = alloc(ph, "lnb", [128, D], F32)
            lsm = [ln_small(ph, f"l1s{i}") for i in range(4)]
            sinks = alloc(ph, "sinks", [128, 16], F32)
            mask_swa = alloc(ph, "mask_swa", [128, 256], F32)
            mask_first = alloc(ph, "mask_first", [128, 256], F32)
            wr = alloc(ph, "wr", [128, 16, 32], F32)
            brt = alloc(ph, "brt", [128, 32], F32)
            ebase = alloc(ph, "ebase", [128, NE], F32)
            cum = alloc(ph, "cum", [128, NE], F32)
            sg = [alloc(ph, f"sg{i}", [128, BLK], F32) for i in range(3)]
            mt = [alloc(ph, f"mt{i}", [128, BLK], F32) for i in range(3)]
            mx = alloc(ph, "mx", [128, 8], F32)
            negm = alloc(ph, "negm", [128, 8], F32)
            ssum = alloc(ph, "ssum", [128, 8], F32)
            t8 = alloc(ph, "t8", [128, 8], F32)
            es8 = alloc(ph, "es8", [128, 8], F32)
            rr = alloc(ph, "rr", [128, 8], F32)
            lg = alloc(ph, "lg", [128, NE], F32)
            top8 = alloc(ph, "top8", [128, 8], F32)
            mb = alloc(ph, "mb", [128, NE], BF16)
            pos = alloc(ph, "pos", [128, NE], F32)
            ov = alloc(ph, "ov", [128, NE], F32)
            mk = alloc(ph, "mk", [128, NE], F32)
            junk = alloc(ph, "junk", [128, NE], F32)
            destf = alloc(ph, "destf", [128, 4], F32)
            nm1 = alloc(ph, "nm1", [128, 1], F32)
            eg = alloc(ph, "eg", [128, 4], F32)
            gs = alloc(ph, "gs", [128, 1], F32)

            memT = mergedT[:, :, 0:256]
            Sm = wk[:, 0:2048].rearrange("p (a b) -> p a b", b=256)
            Pb = wk[:, 2048:3072].bitcast(BF16).rearrange("p (a b) -> p a b", b=256)
            Pn = wk[:, 3072:4096].bitcast(BF16).rearrange("p (a b) -> p a b", b=256)
            rb = [tokf[0][:], tokf[1][:], wk[:, 0:2048], wk[:, 2048:4096]]
            rbres = [["tokf0"], ["tokf1"], ["Sm"], ["Pb", "Pn"]]
            h1T = qT[:].rearrange("p a b -> p (a b)").bitcast(F32).rearrange("p (a b) -> p a b", b=128)

            P.dma("sp", "c9_1", lambda e: e.dma_start(out=lng[:], in_=din["lnp"][l, 0]), writes=["lng"])
            P.dma("sp", "c9_2", lambda e: e.dma_start(out=lnb[:], in_=din["lnp"][l, 1]), writes=["lnb"])
            P.dma("sp", "c9_3", lambda e: e.dma_start(out=sinks[:], in_=din["sinks"][l]), writes=["sinks"])
            P.dma("sp", "c9_4", lambda e: e.dma_start(out=mask_swa[:], in_=din["mask_swa"]), writes=["mask_swa"])
            P.dma("sp", "c9_5", lambda e: e.dma_start(out=mask_first[:], in_=din["mask_first"]), writes=["mask_first"])
            P.dma("sp", "c9_6", lambda e: e.dma_start(out=wr[:].rearrange("p a b -> p (a b)"), in_=din["wr"][l]), writes=["wr"])
            P.dma("sp", "c9_7", lambda e: e.dma_start(out=brt[:], in_=din["br"][l]), writes=["brt"])
            P.dma("sp", "c9_8", lambda e: e.dma_start(out=ebase[:], in_=din["ebase"]), writes=["ebase"])
            P.op("dve", lambda e: e.memset(cum[:], 0.0), writes=["cum"])

            def softmax_pv(nh, pieces, sink_ap, do_pv, out_evac):
                for ap3, res, h0 in pieces:
                    n = ap3.shape[1]
                    P.op("dve", lambda e: e.tensor_reduce(out=mx[:, h0:h0 + n], in_=ap3, axis=AX.X, op=ALU.max),
                         reads=[res], writes=["mx"])
                if sink_ap is not None:
                    P.op("dve", lambda e: e.tensor_tensor(out=mx[:, 0:nh], in0=mx[:, 0:nh], in1=sink_ap, op=ALU.max),
                         reads=["mx", "sinks"], writes=["mx"])
                for ap3, res, h0 in pieces:
                    n = ap3.shape[1]
                    P.op("dve", lambda e: e.tensor_tensor(out=Sm[:, h0:h0 + n, :], in0=ap3,
                                                          in1=mx[:, h0:h0 + n].unsqueeze(2).broadcast_to([128, n, 256]), op=ALU.subtract),
                         reads=[res, "mx"], writes=["Sm"])
                P.op("act", lambda e: e.activation(out=Pb[:, 0:nh, :], in_=Sm[:, 0:nh, :], func=AF.Exp), reads=["Sm"], writes=["Pb"])
                P.op("dve", lambda e: e.tensor_reduce(out=ssum[:, 0:nh], in_=Pb[:, 0:nh, :], axis=AX.X, op=ALU.add),
                     reads=["Pb"], writes=["ssum"])
                if sink_ap is not None:
                    P.op("dve", lambda e: e.tensor_tensor(out=t8[:, 0:nh], in0=sink_ap, in1=mx[:, 0:nh], op=ALU.subtract),
                         reads=["mx", "sinks"], writes=["t8"])
                    P.op("act", lambda e: e.activation(out=es8[:, 0:nh], in_=t8[:, 0:nh], func=AF.Exp), reads=["t8"], writes=["es8"])
                    P.op("dve", lambda e: e.tensor_tensor(out=ssum[:, 0:nh], in0=ssum[:, 0:nh], in1=es8[:, 0:nh], op=ALU.add),
                         reads=["ssum", "es8"], writes=["ssum"])
                P.op("dve", lambda e: e.reciprocal(out=rr[:, 0:nh], in_=ssum[:, 0:nh]), reads=["ssum"], writes=["rr"])
                P.op("dve", lambda e: e.tensor_tensor(out=Pn[:, 0:nh, :], in0=Pb[:, 0:nh, :],
                                                      in1=rr[:, 0:nh].unsqueeze(2).broadcast_to([128, nh, 256]), op=ALU.mult),
                     reads=["Pb", "rr"], writes=["Pn"])
                for a in range(nh // 4):
                    pb, pres = ps_next()
                    pbb = pb[:].bitcast(BF16)
                    for j in range(8):
                        idx = a * 8 + j
                        i, kb = idx // 2, idx % 2
                        P.op("pe", lambda e: e.transpose(out=pbb[:, j * 128:(j + 1) * 128], in_=Pn[:, i, kb * 128:(kb + 1) * 128],
                                                         identity=identb[:]),
                             reads=["Pn", "identb"], writes=[pres])
                    P.op("act", lambda e: e.activation(out=PT[:, a * 8:(a + 1) * 8, :], in_=pbb.rearrange("p (a b) -> p a b", b=128),
                                                       func=AF.Copy),
                         reads=[pres], writes=["PT"])
                pso, psores = ps_next()
                do_pv(pso, psores)
                out_evac(pso, psores)

            for blk in range(NBLK):
                seq = blk // 4
                if blk % 4 == 0:
                    for mtile in range(2):
                        s = mtile % 2
                        r0 = seq * 256 + mtile * 128
                        P.dma("sp", f"ldh{s}", lambda e: e.dma_start(out=tokf[s][:], in_=din["mem"][r0:r0 + 128, :]), writes=[f"tokf{s}"])
                        P.op("act", lambda e: e.activation(out=tokb[s][:], in_=tokf[s][:], func=AF.Copy),
                             reads=[f"tokf{s}"], writes=[f"tokb{s}"])
                        for a in range(2):
                            pb, pres = ps_next()
                            pbb = pb[:].bitcast(BF16)
                            for i in range(8):
                                kc = a * 8 + i
                                P.op("pe", lambda e: e.transpose(out=pbb[:, i * 128:(i + 1) * 128],
                                                                 in_=tokb[s][:, kc * 128:(kc + 1) * 128], identity=identb[:]),
                                     reads=[f"tokb{s}", "identb"], writes=[pres])
                            P.op("dve", lambda e: e.tensor_copy(out=memT[:, a * 8:(a + 1) * 8, mtile * 128:(mtile + 1) * 128],
                                                                in_=pbb.rearrange("p (a b) -> p a b", b=128)),
                                 reads=[pres], writes=["mergedT"])
                    for i in range(2):
                        wt, wres = WS.next(("wmkv", l, i))
                        w3 = wt.rearrange("p (k c) -> p k c", c=256)
                        for ct in range(2):
                            pb, pres = ps_next()
                            for kc in range(16):
                                P.op("pe", lambda e: e.matmul(pb[:, 0:256], w3[:, kc, ct * 128:(ct + 1) * 128], memT[:, kc, :],
                                                              start=(kc == 0), stop=(kc == 15)),
                                     reads=[wres, "mergedT"], writes=[pres])
                            P.op("act", lambda e: e.activation(out=mem_kT[:, 2 * i + ct, :], in_=pb[:, 0:256], func=AF.Copy),
                                 reads=[pres], writes=["mem_kT"])
                    for i in range(2):
                        wt, wres = WS.next(("wmkv", l, 2 + i))
                        w3 = wt.rearrange("p (k c) -> p k c", c=256)
                        for mtile in range(2):
                            pb, pres = ps_next()
                            for kc in range(16):
                                P.op("pe", lambda e: e.matmul(pb[:, 0:256], memT[:, kc, mtile * 128:(mtile + 1) * 128], w3[:, kc, :],
                                                              start=(kc == 0), stop=(kc == 15)),
                                     reads=[wres, "mergedT"], writes=[pres])
                            P.op("act", lambda e: e.activation(out=mem_v[:, mtile, i * 256:(i + 1) * 256], in_=pb[:, 0:256], func=AF.Copy),
                                 reads=[pres], writes=["mem_v"])
                    P.op("dve", lambda e: e.memset(kTd[:, :, 0:128], 0.0), writes=["kTd"])
                    P.op("dve", lambda e: e.memset(v_tok[:, 0, :], 0.0), writes=["v_tok"])

                load_hT(blk, hT, tokf, tokb)
                P.dma("sp", "lso", lambda e: e.dma_start(out=soT[:].rearrange("p a b -> p (a b)"), in_=ssm_scr[blk]),
                      reads=[("ssm_scr", blk)], writes=["soT"])
                for i in range(4):
                    wt, wres = WS.next(("win", l, i))

                    def ev_q(ct, pb, pres):
                        P.op("act", lambda e: e.activation(out=qT[:, 2 * i + ct, :], in_=pb[:], func=AF.Copy, scale=0.125),
                             reads=[pres], writes=["qT"])
                    proj_tile(wt, wres, 2, hT, ev_q)
                wt, wres = WS.next(("win", l, 4))

                def ev_k(ct, pb, pres):
                    P.op("act", lambda e: e.activation(out=kTd[:, ct, 128:640], in_=pb[:], func=AF.Copy), reads=[pres], writes=["kTd"])
                proj_tile(wt, wres, 2, hT, ev_k)
                wt, wres = WS.next(("win", l, 5))
                w3 = wt.rearrange("p (k c) -> p k c", c=256)
                for tt in range(4):
                    pb, pres = ps_next()
                    for kc in range(16):
                        P.op("pe", lambda e: e.matmul(pb[:, 0:128], hT[:, kc, tt * 128:(tt + 1) * 128], w3[:, kc, 0:128],
                                                      start=(kc == 0), stop=(kc == 15)),
                             reads=[wres, "hT"], writes=[pres])
                    P.op("act", lambda e: e.activation(out=v_tok[:, 1 + tt, :], in_=pb[:, 0:128], func=AF.Copy), reads=[pres], writes=["v_tok"])
                for i in range(2):
                    wt, wres = WS.next(("win", l, 8 + i))

                    def ev_qm(ct, pb, pres):
                        P.op("act", lambda e: e.activation(out=qmT[:, 2 * i + ct, :], in_=pb[:], func=AF.Copy, scale=128.0 ** -0.5),
                             reads=[pres], writes=["qmT"])
                    proj_tile(wt, wres, 2, hT, ev_qm)
                if blk == 0:
                    dump("qT", qT[:], "qT", [128, 8, BLK], BF16)
                    dump("kTd", kTd[:], "kTd", [128, 2, 640], BF16)
                    dump("v_tok", v_tok[:], "v_tok", [128, 5, 128], BF16)

                for qb in range(4):
                    qs = slice(qb * 128, (qb + 1) * 128)
                    msk = mask_first if (blk % 4 == 0 and qb == 0) else mask_swa
                    mres = "mask_first" if (blk % 4 == 0 and qb == 0) else "mask_swa"
                    for g in range(2):
                        for jp in range(2):
                            for p in range(2):
                                pb, pres = ps_next()
                                for c in range(2):
                                    j = 2 * jp + c
                                    P.op("pe", lambda e: e.matmul(pb[:, c * 256:(c + 1) * 256], qT[p * 64:(p + 1) * 64, 4 * g + j, qs],
                                                                  kTd[p * 64:(p + 1) * 64, g, qb * 128:qb * 128 + 256], start=True, stop=True),
                                         reads=["qT", "kTd"], writes=[pres])
                                s0 = jp * 4 + p * 2
                                P.op("dve", lambda e: e.tensor_tensor(out=Sm[:, s0:s0 + 2, :],
                                                                      in0=pb[:].rearrange("p (a b) -> p a b", b=256),
                                                                      in1=msk[:].unsqueeze(1).broadcast_to([128, 2, 256]), op=ALU.add),
                                     reads=[pres, mres], writes=["Sm"])

                        def pv(pso, psores):
                            for sl in range(8):
                                jp, p, c = sl // 4, (sl // 2) % 2, sl % 2
                                i = 2 * (2 * jp + c) + p
                                for kb in range(2):
                                    P.op("pe", lambda e: e.matmul(
                                        pso[(i % 2) * 64:(i % 2) * 64 + 64, (i // 2) * 128:(i // 2) * 128 + 128],
                                        v_tok[:, qb + kb, g * 64:(g + 1) * 64], PT[:, 2 * sl + kb, :], start=(kb == 0), stop=(kb == 1)),
                                        reads=["v_tok", "PT"], writes=[psores])

                        def oev(pso, psores):
                            P.op("act", lambda e: e.activation(out=aoT[:, 4 * g:4 * g + 4, qs], in_=pso[:].rearrange("p (a b) -> p a b", b=128),
                                                               func=AF.Copy),
                                 reads=[psores], writes=["aoT"])
                        softmax_pv(8, [(Sm[:, 0:8, :], "Sm", 0)], sinks[:, 8 * g:8 * g + 8], pv, oev)
                P.op("dve", lambda e: e.tensor_copy(out=kTd[:, :, 0:128], in_=kTd[:, :, 512:640]), reads=["kTd"], writes=["kTd"])
                P.op("dve", lambda e: e.tensor_copy(out=v_tok[:, 0, :], in_=v_tok[:, 4, :]), reads=["v_tok"], writes=["v_tok"])

                for tt in range(4):
                    ts_ = slice(tt * 128, (tt + 1) * 128)
                    pbs = []
                    for a in range(2):
                        pb, pres = ps_next()
                        pbs.append((pb, pres))
                        for p in range(2):
                            P.op("pe", lambda e: e.matmul(pb[:, p * 256:(p + 1) * 256], qmT[:, 2 * a + p, ts_], mem_kT[:, 2 * a + p, :],
                                                          start=True, stop=True),
                                 reads=["qmT", "mem_kT"], writes=[pres])

                    def pvm(pso, psores):
                        for i in range(4):
                            for kb in range(2):
                                P.op("pe", lambda e: e.matmul(pso[:, i * 128:(i + 1) * 128], mem_v[:, kb, i * 128:(i + 1) * 128],
                                                              PT[:, 2 * i + kb, :], start=(kb == 0), stop=(kb == 1)),
                                     reads=["mem_v", "PT"], writes=[psores])

                    def oevm(pso, psores):
                        P.op("act", lambda e: e.activation(out=moT[:, 0:4, ts_], in_=pso[:].rearrange("p (a b) -> p a b", b=128), func=AF.Copy),
                             reads=[psores], writes=["moT"])
                    softmax_pv(4, [(pbs[x][0][:].rearrange("p (a b) -> p a b", b=256), pbs[x][1], 2 * x) for x in range(2)], None, pvm, oevm)
                if blk == 0:
                    dump("aoT", aoT[:], "aoT", [128, 8, BLK], BF16)
                    dump("moT", moT[:], "moT", [128, 4, BLK], BF16)

                for jj in range(8):
                    wgt = [WS.next(("win", l, NT_IN + jj * 3 + b), held=b) for b in range(3)]
                    wbt, wbres = WS.next(("wbr", l, jj), held=3)
                    wb3 = wbt.rearrange("p (k c) -> p k c", c=256)
                    srcs = [(aoT, "aoT", 0, 8), (soT, "soT", 8, 4), (moT, "moT", 12, 4)]
                    for ct in range(2):
                        cs = slice(ct * 128, (ct + 1) * 128)
                        gps, bps = [], []
                        for b in range(3):
                            pb, pres = ps_next()
                            gps.append((pb, pres))
                            w3 = wgt[b][0].rearrange("p (k c) -> p k c", c=256)
                            for kc in range(16):
                                P.op("pe", lambda e: e.matmul(pb[:], w3[:, kc, cs], hT[:, kc, :], start=(kc == 0), stop=(kc == 15)),
                                     reads=[wgt[b][1], "hT"], writes=[pres])
                            pb2, pres2 = ps_next()
                            bps.append((pb2, pres2))
                            src, sres, k0, nk = srcs[b]
                            for kk in range(nk):
                                P.op("pe", lambda e: e.matmul(pb2[:], wb3[:, k0 + kk, cs], src[:, kk, :], start=(kk == 0), stop=(kk == nk - 1)),
                                     reads=[wbres, sres], writes=[pres2])
                        for b in range(3):
                            P.op("act", lambda e: e.activation(out=sg[b][:], in_=gps[b][0][:], func=AF.Sigmoid), reads=[gps[b][1]], writes=[f"sg{b}"])
                            P.op("dve", lambda e: e.tensor_tensor(out=mt[b][:], in0=bps[b][0][:], in1=sg[b][:], op=ALU.mult),
                                 reads=[bps[b][1], f"sg{b}"], writes=[f"mt{b}"])
                        P.op("pool", lambda e: e.tensor_tensor(out=mt[0][:], in0=mt[0][:], in1=mt[1][:], op=ALU.add), reads=["mt0", "mt1"], writes=["mt0"])
                        P.op("pool", lambda e: e.tensor_tensor(out=mergedT[:, 2 * jj + ct, :], in0=mt[0][:], in1=mt[2][:], op=ALU.add),
                             reads=["mt0", "mt2"], writes=["mergedT"])
                if blk == 0:
                    dump("mergedT", mergedT[:], "mergedT", [128, 16, BLK], BF16)

                for tt in range(4):
                    ti = blk * 4 + tt
                    P.dma("sp", f"ldr{tt}", lambda e: e.dma_start(out=rb[tt], in_=h_tok[ti * 128:(ti + 1) * 128, :]),
                          reads=[("htok", ti)], writes=rbres[tt])
                for c in range(8):
                    wt, wres = WS.next(("wout", l, c))
                    w3 = wt.rearrange("p (k c) -> p k c", c=256)
                    for tt in range(4):
                        pb, pres = ps_next()
                        for kc in range(16):
                            P.op("pe", lambda e: e.matmul(pb[:, 0:256], mergedT[:, kc, tt * 128:(tt + 1) * 128], w3[:, kc, :],
                                                          start=(kc == 0), stop=(kc == 15)),
                                 reads=[wres, "mergedT"], writes=[pres])
                        P.op("dve", lambda e: e.scalar_tensor_tensor(out=rb[tt][:, c * 256:(c + 1) * 256], in0=rb[tt][:, c * 256:(c + 1) * 256],
                                                                     scalar=ALPHA, in1=pb[:, 0:256], op0=ALU.mult, op1=ALU.add),
                             reads=[pres] + rbres[tt], writes=rbres[tt])
                for tt in range(4):
                    ti = blk * 4 + tt
                    s = tt % 2
                    x = rb[tt]
                    xr = rbres[tt]
                    st, mv, ve, rs = lsm[tt]
                    tag = f"l1{tt}"
                    for c in range(4):
                        P.op("dve", lambda e: e.bn_stats(out=st[:, c, :], in_=x[:, c * 512:(c + 1) * 512]), reads=xr, writes=[f"{tag}st{c}"])
                    P.op("dve", lambda e: e.bn_aggr(out=mv[:], in_=st[:].rearrange("p a b -> p (a b)")),
                         reads=[f"{tag}st{c}" for c in range(4)], writes=[tag + "mv"])
                    P.op("dve", lambda e: e.tensor_scalar(out=ve[:], in0=mv[:, 1:2], scalar1=EPS, scalar2=None, op0=ALU.add),
                         reads=[tag + "mv"], writes=[tag + "ve"])
                    P.op("pool", lambda e: e.tensor_tensor(out=rs[:], in0=ve[:], in1=neghalf[:], op=ALU.pow),
                         reads=[tag + "ve", "neghalf"], writes=[tag + "rs"])
                    P.op("dve", lambda e: e.tensor_scalar(out=x, in0=x, scalar1=mv[:, 0:1], scalar2=rs[:, 0:1], op0=ALU.subtract, op1=ALU.mult),
                         reads=xr + [tag + "mv", tag + "rs"], writes=xr)
                    P.op("pool", lambda e: e.tensor_tensor(out=x, in0=x, in1=lng[:], op=ALU.mult), reads=xr + ["lng"], writes=xr)
                    P.op("pool", lambda e: e.tensor_tensor(out=x, in0=x, in1=lnb[:], op=ALU.add), reads=xr + ["lnb"], writes=xr)
                    P.dma("sp", f"sth{tt}", lambda e: e.dma_start(out=h_tok[ti * 128:(ti + 1) * 128, :], in_=x), reads=xr, writes=[("htok", ti)])
                    P.op("act", lambda e: e.activation(out=tokb[s][:], in_=x, func=AF.Copy), reads=xr, writes=[f"tokb{s}"])
                    for a in range(4):
                        pb, pres = ps_next()
                        for i in range(4):
                            kc = a * 4 + i
                            P.op("pe", lambda e: e.transpose(out=pb[:, i * 128:(i + 1) * 128], in_=x[:, kc * 128:(kc + 1) * 128], identity=identf[:]),
                                 reads=xr + ["identf"], writes=[pres])
                        P.op("dve", lambda e: e.tensor_copy(out=h1T[:, a * 4:(a + 1) * 4, :], in_=pb[:].rearrange("p (a b) -> p a b", b=128)),
                             reads=[pres], writes=["qT"])
                    pb, pres = ps_next()
                    for kc in range(16):
                        P.op("pe", lambda e: e.matmul(pb[:, 0:NE], h1T[:, kc, :], wr[:, kc, :], start=(kc == 0), stop=(kc == 15)),
                             reads=["qT", "wr"], writes=[pres])
                    P.op("dve", lambda e: e.tensor_tensor(out=lg[:], in0=pb[:, 0:NE], in1=brt[:], op=ALU.add), reads=[pres, "brt"], writes=["lg"])
                    if ti == 0:
                        dump("lg", lg[:], "lg", [128, NE])
                    P.op("dve", lambda e: e.max(out=top8[:], in_=lg[:]), reads=["lg"], writes=["top8"])
                    P.op("dve", lambda e: e.tensor_scalar(out=mb[:], in0=lg[:], scalar1=top8[:, 3:4], scalar2=None, op0=ALU.is_ge),
                         reads=["lg", "top8"], writes=["mb"])
                    pa, pares = ps_next()
                    P.op("pe", lambda e: e.matmul(pa[:, 0:NE], ltris[:], mb[:], start=True, stop=True), reads=["ltris", "mb"], writes=[pares])
                    P.op("pe", lambda e: e.matmul(pa[:, NE:2 * NE], onesb[:], mb[:], start=True, stop=True), reads=["onesb", "mb"], writes=[pares])
                    P.op("dve", lambda e: e.tensor_tensor(out=pos[:], in0=pa[:, 0:NE], in1=cum[:], op=ALU.add), reads=[pares, "cum"], writes=["pos"])
                    P.op("dve", lambda e: e.tensor_tensor(out=cum[:], in0=pa[:, NE:2 * NE], in1=cum[:], op=ALU.add), reads=[pares, "cum"], writes=["cum"])
                    P.op("dve", lambda e: e.tensor_scalar(out=ov[:], in0=pos[:], scalar1=float(CAP), scalar2=1.0e7, op0=ALU.is_ge, op1=ALU.mult),
                         reads=["pos"], writes=["ov"])
                    P.op("dve", lambda e: e.tensor_tensor(out=pos[:], in0=pos[:], in1=ebase[:], op=ALU.add), reads=["pos", "ebase"], writes=["pos"])
                    P.op("dve", lambda e: e.tensor_tensor(out=pos[:], in0=pos[:], in1=ov[:], op=ALU.add), reads=["pos", "ov"], writes=["pos"])
                    P.op("dve", lambda e: e.tensor_scalar(out=nm1[:], in0=top8[:, 0:1], scalar1=-1.0, scalar2=None, op0=ALU.mult),
                         reads=["top8"], writes=["nm1"])
                    P.op("act", lambda e: e.activation(out=eg[:], in_=top8[:, 0:4], func=AF.Exp, bias=nm1[:, 0:1], scale=1.0, accum_out=gs[:]),
                         reads=["top8", "nm1"], writes=["eg", "gs"])
                    P.op("dve", lambda e: e.reciprocal(out=gs[:], in_=gs[:]), reads=["gs"], writes=["gs"])
                    P.op("dve", lambda e: e.tensor_scalar(out=gates_all[:, ti, :], in0=eg[:], scalar1=gs[:, 0:1], scalar2=None, op0=ALU.mult),
                         reads=["eg", "gs"], writes=["gates_all"])
                    for k in range(4):
                        P.op("dve", lambda e: e.tensor_scalar(out=mk[:], in0=lg[:], scalar1=top8[:, k:k + 1], scalar2=None, op0=ALU.is_equal),
                             reads=["lg", "top8"], writes=["mk"])
                        P.op("dve", lambda e: e.tensor_tensor(out=junk[:], in0=mk[:], in1=pos[:], op=ALU.mult),
                             reads=["mk", "pos"], writes=["junk"])
                        P.op("dve", lambda e: e.tensor_reduce(out=destf[:, k:k + 1], in_=junk[:], axis=AX.X, op=ALU.add),
                             reads=["junk"], writes=["destf"])
                        if k == 0:
                            P.op("dve", lambda e: e.tensor_scalar(out=gmat_all[:, ti, :], in0=mk[:], scalar1=gates_all[:, ti, 0:1], scalar2=None,
                                                                  op0=ALU.mult),
                                 reads=["mk", "gates_all"], writes=["gmat_all"])
                        else:
                            P.op("dve", lambda e: e.scalar_tensor_tensor(out=gmat_all[:, ti, :], in0=mk[:], scalar=gates_all[:, ti, k:k + 1],
                                                                         in1=gmat_all[:, ti, :], op0=ALU.mult, op1=ALU.add),
                                 reads=["mk", "gates_all", "gmat_all"], writes=["gmat_all"])
                    P.op("act", lambda e: e.activation(out=dest_all[:, ti, :], in_=destf[:], func=AF.Copy), reads=["destf"], writes=["dest_all"])
                    for k in range(4):
                        P.dma("pool", f"sc{s}", lambda e: e.indirect_dma_start(
                            out=xg[:, :], out_offset=bass.IndirectOffsetOnAxis(ap=dest_all[:, ti, k:k + 1], axis=0),
                            in_=tokb[s][:, :], in_offset=None, bounds_check=bc_reg, oob_is_err=False),
                            reads=[f"tokb{s}", "dest_all"], writes=[("xg", ti, k)])
            P.barrier()

    STILES = [(i * 128, min(128, CAP - i * 128)) for i in range((CAP + 127) // 128)]
    NH = CAP // 2

    def phase_experts(l):
        with ExitStack() as ph:
            xrow = [alloc(ph, f"xrow{i}", [128, D], BF16) for i in range(2)]
            xeTs = [alloc(ph, f"xeT{i}", [128, 16, CAP], BF16) for i in range(2)]
            actT = alloc(ph, "actT", [128, 8, CAP], BF16)
            yst = alloc(ph, "yst", [128, len(STILES), D], BF16)
            bup = alloc(ph, "bup", [128, NE, 16], F32)
            tg = [alloc(ph, f"tg{i}", [128, NH], F32) for i in range(2)]
            tsg = [alloc(ph, f"tsg{i}", [128, NH], F32) for i in range(2)]
            tl = [alloc(ph, f"tl{i}", [128, NH], F32) for i in range(2)]
            P.dma("sp", "c10", lambda e: e.dma_start(out=bup[:].rearrange("p a b -> p (a b)"), in_=din["bup"][l]), writes=["bup"])
            par = 0
            for ex in range(NE):
                base = ex * CAP
                xeT = xeTs[ex % 2]
                xres = f"xeT{ex % 2}"
                for si, (r0, rows) in enumerate(STILES):
                    s = si % 2
                    P.dma("sp", f"ldxg{s}", lambda e: e.dma_start(out=xrow[s][0:rows, :], in_=xg[base + r0:base + r0 + rows, :]),
                          writes=[f"xrow{s}"])
                    for a in range(2):
                        pb, pres = ps_next()
                        pbb = pb[:].bitcast(BF16)
                        for i in range(8):
                            kc = a * 8 + i
                            P.op("pe", lambda e: e.transpose(out=pbb[:, i * 128:i * 128 + rows], in_=xrow[s][0:rows, kc * 128:(kc + 1) * 128],
                                                             identity=identb[0:rows, 0:rows]),
                                 reads=[f"xrow{s}", "identb"], writes=[pres])
                        P.op("dve", lambda e: e.tensor_copy(out=xeT[:, a * 8:(a + 1) * 8, r0:r0 + rows],
                                                            in_=pbb.rearrange("p (a b) -> p a b", b=128)[:, :, 0:rows]),
                             reads=[pres], writes=[xres])
                for j in range(8):
                    wt, wres = WS.next(("wup", l, ex, j))
                    w3 = wt.rearrange("p (k c) -> p k c", c=256)
                    for nh in range(2):
                        ns = slice(nh * NH, (nh + 1) * NH)
                        pg, pgres = ps_next()
                        pl, plres = ps_next()
                        for kc in range(16):
                            P.op("pe", lambda e: e.matmul(pg[:, 0:NH], w3[:, kc, 0:128], xeT[:, kc, ns], start=(kc == 0), stop=(kc == 15)),
                                 reads=[wres, xres], writes=[pgres])
                        for kc in range(16):
                            P.op("pe", lambda e: e.matmul(pl[:, 0:NH], w3[:, kc, 128:256], xeT[:, kc, ns], start=(kc == 0), stop=(kc == 15)),
                                 reads=[wres, xres], writes=[plres])
                        q = par
                        par ^= 1
                        P.op("dve", lambda e: e.tensor_scalar(out=tg[q][:], in0=pg[:, 0:NH], scalar1=bup[:, ex, j:j + 1], scalar2=7.0,
                                                              op0=ALU.add, op1=ALU.min),
                             reads=[pgres, "bup"], writes=[f"tg{q}"])
                        P.op("act", lambda e: e.activation(out=tsg[q][:], in_=tg[q][:], func=AF.Sigmoid, scale=1.702),
                             reads=[f"tg{q}"], writes=[f"tsg{q}"])
                        P.op("dve", lambda e: e.tensor_scalar(out=tl[q][:], in0=pl[:, 0:NH], scalar1=bup[:, ex, 8 + j:9 + j], scalar2=-7.0,
                                                              op0=ALU.add, op1=ALU.max),
                             reads=[plres, "bup"], writes=[f"tl{q}"])
                        P.op("pool", lambda e: e.tensor_scalar(out=tl[q][:], in0=tl[q][:], scalar1=7.0, scalar2=1.0, op0=ALU.min, op1=ALU.add),
                             reads=[f"tl{q}"], writes=[f"tl{q}"])
                        P.op("pool", lambda e: e.tensor_tensor(out=tg[q][:], in0=tg[q][:], in1=tsg[q][:], op=ALU.mult),
                             reads=[f"tg{q}", f"tsg{q}"], writes=[f"tg{q}"])
                        P.op("pool", lambda e: e.tensor_tensor(out=actT[:, j, ns], in0=tg[q][:], in1=tl[q][:], op=ALU.mult),
                             reads=[f"tg{q}", f"tl{q}"], writes=["actT"])
                for c in range(8):
                    wt, wres = WS.next(("wdn", l, ex, c))
                    w3 = wt.rearrange("p (k c) -> p k c", c=256)
                    for si, (r0, rows) in enumerate(STILES):
                        pb, pres = ps_next()
                        for k in range(8):
                            P.op("pe", lambda e: e.matmul(pb[0:rows, 0:256], actT[:, k, r0:r0 + rows], w3[:, k, :], start=(k == 0), stop=(k == 7)),
                                 reads=[wres, "actT"], writes=[pres])
                        P.op("act", lambda e: e.activation(out=yst[0:rows, si, c * 256:(c + 1) * 256], in_=pb[0:rows, 0:256], func=AF.Copy),
                             reads=[pres], writes=[("yst", si)])
                for si, (r0, rows) in enumerate(STILES):
                    P.dma("sp", f"sty{si}", lambda e: e.dma_start(out=ypad[base + r0:base + r0 + rows, :], in_=yst[0:rows, si, :]),
                          reads=[("yst", si)], writes=[("ypad", ex, si)])
            P.barrier()

    def phase_combine(l, dst):
        with ExitStack() as ph:
            yk = [[alloc(ph, f"yk{s}{k}", [128, D], BF16) for k in range(4)] for s in range(2)]
            hb = [alloc(ph, f"hb{i}", [128, D], F32) for i in range(2)]
            lng = alloc(ph, "lng2", [128, D], F32)
            lnb = alloc(ph, "lnb2", [128, D], F32)
            bdn = alloc(ph, "bdn", [NE, D], F32)
            gT = [alloc(ph, f"gT{i}", [NE, 128], F32) for i in range(2)]
            lsm = [ln_small(ph, f"l2s{i}") for i in range(2)]
            P.dma("sp", "c11", lambda e: e.dma_start(out=lng[:], in_=din["lnp"][l, 2]), writes=["lng2"])
            P.dma("sp", "c12", lambda e: e.dma_start(out=lnb[:], in_=din["lnp"][l, 3]), writes=["lnb2"])
            P.dma("sp", "c13", lambda e: e.dma_start(out=bdn[:], in_=din["bdn"][l]), writes=["bdn"])
            for ti in range(T // 128):
                s = ti % 2
                for k in range(4):
                    P.dma("pool", f"ga{s}{k}", lambda e: e.indirect_dma_start(
                        out=yk[s][k][:, :], out_offset=None, in_=ypad[:, :],
                        in_offset=bass.IndirectOffsetOnAxis(ap=dest_all[:, ti, k:k + 1], axis=0),
                        bounds_check=bc_reg, oob_is_err=False),
                        reads=["dest_all"], writes=[f"yk{s}{k}"])
                P.dma("sp", f"ldh2{s}", lambda e: e.dma_start(out=hb[s][:], in_=h_tok[ti * 128:(ti + 1) * 128, :]),
                      reads=[("htok", ti)], writes=[f"hb{s}"])
                pt, ptres = ps_next()
                P.op("pe", lambda e: e.transpose(out=pt[0:NE, 0:128], in_=gmat_all[:, ti, :], identity=identf[:]),
                     reads=["gmat_all", "identf"], writes=[ptres])
                P.op("dve", lambda e: e.tensor_copy(out=gT[s][:], in_=pt[0:NE, 0:128]), reads=[ptres], writes=[f"gT{s}"])
                for cgi in range(4):
                    cs = slice(cgi * 512, (cgi + 1) * 512)
                    pb, pres = ps_next()
                    P.op("pe", lambda e: e.matmul(pb[:], gT[s][:], bdn[:, cs], start=True, stop=True), reads=[f"gT{s}", "bdn"], writes=[pres])
                    P.op("dve", lambda e: e.scalar_tensor_tensor(out=hb[s][:, cs], in0=hb[s][:, cs], scalar=ALPHA, in1=pb[:], op0=ALU.mult, op1=ALU.add),
                         reads=[pres, f"hb{s}"], writes=[f"hb{s}"])
                for k in range(4):
                    P.op("dve", lambda e: e.scalar_tensor_tensor(out=hb[s][:], in0=yk[s][k][:], scalar=gates_all[:, ti, k:k + 1], in1=hb[s][:],
                                                                 op0=ALU.mult, op1=ALU.add),
                         reads=[f"yk{s}{k}", "gates_all", f"hb{s}"], writes=[f"hb{s}"])
                layer_norm(hb[s][:], f"hb{s}", lng[:], "lng2", lnb[:], "lnb2", lsm[s], f"l2{s}")
                P.dma("sp", f"sto{s}", lambda e: e.dma_start(out=dst[ti * 128:(ti + 1) * 128, :], in_=hb[s][:]),
                      reads=[f"hb{s}"], writes=[("htok", ti)])
            P.barrier()

    phase_ln0()
    for l in range(nlayers):
        if stop == "ln0":
            break
        phase_ssm(l)
        if stop == "ssm":
            break
        phase_mix(l)
        if stop == "mix":
            break
        phase_experts(l)
        if stop == "exp":
            break
        phase_combine(l, out if l == nlayers - 1 else h_tok)
    P.finish()
    return nc, dbg


def _kc(W):
    K, C = W.shape
    return np.ascontiguousarray(W.reshape(K // 128, 128, C).transpose(1, 0, 2)).reshape(128, (K // 128) * C)


def _rep(v, n=128):
    return np.ascontiguousarray(np.broadcast_to(np.asarray(v, np.float32)[None, :], (n, len(v))))


def prep_shared(inp, moe=True):
    f = np.float32
    sh = {}
    sh["lnin"] = np.stack([_rep(inp["ln_in_g"]), _rep(inp["ln_in_b"])])
    sh["lnp"] = np.stack([np.stack([_rep(inp[k][l]) for k in ("ln1_g", "ln1_b", "ln2_g", "ln2_b")]) for l in range(L)])
    win = np.zeros((L, NT_IN + 24, 128, 4096), f)
    for l in range(L):
        W = inp["w_in"][l]
        k0 = W[:, 1024:1088]
        k1 = W[:, 1088:1152]
        tiles = [W[:, i * 256:(i + 1) * 256] for i in range(4)]
        tiles.append(np.concatenate([k0, k0, k1, k1], axis=1))
        tiles.append(np.concatenate([W[:, 1152:1280], np.zeros((D, 128), f)], axis=1))
        tiles += [W[:, 1280 + i * 256:1280 + (i + 1) * 256] for i in range(2)]
        tiles += [W[:, 1792 + i * 256:1792 + (i + 1) * 256] for i in range(2)]
        for jj in range(8):
            for b in range(3):
                c0 = 2304 + b * 2048 + jj * 256
                tiles.append(W[:, c0:c0 + 256])
        for i, t in enumerate(tiles):
            win[l, i] = _kc(t)
    sh["win"] = win
    sh["wbr"] = np.stack([np.stack([_kc(inp["w_branch"][l][:, j * 256:(j + 1) * 256]) for j in range(8)]) for l in range(L)])
    sh["wout"] = np.stack([np.stack([_kc(inp["w_out"][l][:, j * 256:(j + 1) * 256]) for j in range(8)]) for l in range(L)])
    sh["wglu"] = np.stack([_kc(inp["w_glu"][l]) for l in range(L)])
    sh["wmkv"] = np.stack([np.stack([_kc(inp["w_mem_kv"][l][:, j * 256:(j + 1) * 256]) for j in range(4)]) for l in range(L)])
    sh["wr"] = np.stack([_kc(inp["w_router"][l]) for l in range(L)])
    sh["br"] = np.stack([_rep(inp["b_router"][l]) for l in range(L)])
    wup = np.empty((L, NE, 8, 128, 4096), f) if moe else None
    wdn = np.empty((L, NE, 8, 128, 2048), f) if moe else None
    bup = np.empty((L, 128, NE, 16), f)
    for l in range(L):
        for e in range(NE):
            bu = inp["b_up"][l, e]
            bup[l, :, e, 0:8] = bu[0::2].reshape(8, 128).T
            bup[l, :, e, 8:16] = bu[1::2].reshape(8, 128).T
            if not moe:
                continue
            Wu = inp["w_up"][l, e]
            g, li = Wu[:, 0::2], Wu[:, 1::2]
            for j in range(8):
                wup[l, e, j] = _kc(np.concatenate([g[:, j * 128:(j + 1) * 128], li[:, j * 128:(j + 1) * 128]], axis=1))
            Wd = inp["w_down"][l, e]
            for c in range(8):
                wdn[l, e, c] = _kc(Wd[:, c * 256:(c + 1) * 256])
    if moe:
        sh["wup"], sh["wdn"] = wup, wdn
    sh["bup"] = bup.reshape(L, 128, NE * 16)
    sh["bdn"] = np.ascontiguousarray(inp["b_down"]).astype(f)
    slot2head = [8 * g + 2 * (2 * (s // 4) + s % 2) + (s // 2) % 2 for g in range(2) for s in range(8)]
    sh["sinks"] = np.stack([_rep(np.asarray(inp["attn_sinks"][l])[slot2head]) for l in range(L)])
    sh["ident"] = np.eye(128, dtype=f)
    s_, t_ = np.meshgrid(np.arange(128), np.arange(128), indexing="ij")
    sh["ltri"] = (s_ <= t_).astype(f)
    sh["ltris"] = (s_ < t_).astype(f)
    sh["ones"] = np.ones((128, 128), f)
    q_, k_ = np.meshgrid(np.arange(128), np.arange(256), indexing="ij")
    valid = (k_ > q_) & (k_ <= q_ + 128)
    sh["mask_swa"] = np.where(valid, 0.0, -30000.0).astype(f)
    sh["mask_first"] = np.where(valid & (k_ >= 128), 0.0, -30000.0).astype(f)
    sh["ebase"] = _rep(np.arange(NE, dtype=f) * CAP)
    sh["sp1"] = (np.arange(128, dtype=f) + 1.0).reshape(128, 1)
    sh["tp1"] = _rep(np.arange(128, dtype=f) + 1.0)
    pp = np.arange(128)[:, None, None] // 16
    sh["bmask"] = np.broadcast_to((pp == np.arange(8)[None, :, None]), (128, 8, 64)).astype(f).reshape(128, 512)
    s_tok = np.empty((L, 3, 128, 2048), f)
    s_cm = np.empty((L, 3, 128, 16), f)
    s_b = np.empty((L, 5, 128, 256), f)
    s_c = np.zeros((L, 2, 128, 16, 4, 16), f)
    s_d = np.empty((L, 128, 4), f)
    for l in range(L):
        lre, lim = inp["ssm_lambda_re"][l], inp["ssm_lambda_im"][l]
        ldt = np.broadcast_to(inp["ssm_log_dt"][l][:, None], (32, 64))
        for i, a in enumerate((lre, lim, ldt)):
            flat = np.ascontiguousarray(a).reshape(2048)
            s_tok[l, i] = _rep(flat)
            s_cm[l, i] = flat.reshape(16, 128).T
            s_b[l, i] = np.broadcast_to(np.ascontiguousarray(a).reshape(4, 8, 1, 64), (4, 8, 16, 64)).transpose(1, 2, 0, 3).reshape(128, 256)
        for i, bb in enumerate((inp["ssm_b_re"][l], inp["ssm_b_im"][l])):
            s_b[l, 3 + i] = bb.reshape(4, 8, 64, 16).transpose(1, 3, 0, 2).reshape(128, 256)
        for i, cc in enumerate((inp["ssm_c_re"][l], inp["ssm_c_im"][l])):
            c4 = cc.reshape(16, 2, 16, 64)
            for g2 in range(2):
                for par in range(2):
                    s_c[l, i, g2 * 64:(g2 + 1) * 64, par::2, 2 * par + g2, :] = c4[par::2, g2].transpose(2, 0, 1)
        s_d[l] = inp["ssm_d"][l].reshape(4, 128).T
    sh["s_tok"], sh["s_cm"], sh["s_b"], sh["s_d"] = s_tok, s_cm, s_b, s_d
    sh["s_c"] = s_c.reshape(L, 2, 128, 1024)
    return {k: np.ascontiguousarray(v, dtype=f) for k, v in sh.items()}


def prep_core(inp, c):
    x = np.ascontiguousarray(inp["x"][NSEQ * c:NSEQ * (c + 1)]).reshape(T, D).astype(np.float32)
    mem = np.ascontiguousarray(inp["mem"][NSEQ * c:NSEQ * (c + 1)]).reshape(NSEQ * 256, D).astype(np.float32)
    return {"x": x, "mem": mem}


_CACHE = {}


def kernel(**inputs):
    inp = {k: np.asarray(v) for k, v in inputs.items()}
    sh = prep_shared(inp)
    if "nc" not in _CACHE:
        _CACHE["nc"] = build()[0]
    nc = _CACHE["nc"]
    in_maps = [{**sh, **prep_core(inp, c)} for c in range(NCORES)]
    res = run_bass_kernel_spmd(nc, in_maps, core_ids=list(range(NCORES)))
    outs = [np.asarray(r["out"]).reshape(NSEQ, S, D) for r in res.results]
    return np.concatenate(outs, axis=0).astype(np.float32)
```

```python
import numpy as np
from contextlib import ExitStack
import concourse.bass as bass
import concourse.mybir as mybir
from concourse.bass_utils import run_bass_kernel_spmd

F32 = mybir.dt.float32
BF16 = mybir.dt.bfloat16
I32 = mybir.dt.int32
ALU = mybir.AluOpType
AF = mybir.ActivationFunctionType
AX = mybir.AxisListType

NCORES = 8
D = 2048
S = 2048
NSEQ = 2
T = NSEQ * S
BLK = 512
NBLK = T // BLK
L = 2
NE = 32
CAP = 768
NSLOT = NE * CAP
EPS = 1e-5
ALPHA = (2.0 * L) ** 0.25
NT_IN = 10
ENG = ("pe", "act", "dve", "pool", "sp")
TWO_PI = 6.283185307179586
MAGIC = 12582912.0


class Late:
    def __init__(self, f):
        self.f = f


class _Rec:
    def __init__(self):
        self.__dict__["call"] = None

    def __getattr__(self, name):
        def f(*a, **k):
            self.__dict__["call"] = (name, a, k)
            return self
        return f


def _record(fn):
    r = _Rec()
    fn(r)
    return r.__dict__["call"]


class Prog:
    def __init__(self, nc, es):
        self.nc = nc
        self.es = es
        self.ops = {e: [] for e in ENG}
        self.sem = {}
        self.cnt = {}
        self.waited = {e: {} for e in ENG}
        self.res = {}
        self.nins = 0
        for e in ("pe", "act", "dve", "pool"):
            self.newsem(e)

    def newsem(self, key):
        self.sem[key] = self.es.enter_context(self.nc.semaphore("s_" + key))
        self.cnt[key] = 0

    def _deps(self, reads, writes):
        d = {}
        for r in reads:
            st = self.res.get(r)
            if st and st["w"]:
                k, v = st["w"]
                if v > d.get(k, 0):
                    d[k] = v
        for w in writes:
            st = self.res.get(w)
            if st:
                if st["w"]:
                    k, v = st["w"]
                    if v > d.get(k, 0):
                        d[k] = v
                for k, v in st["r"].items():
                    if v > d.get(k, 0):
                        d[k] = v
        return d

    def _emit_waits(self, eng, deps):
        for k, v in deps.items():
            if eng == "pe" and k == "pe":
                continue
            if self.waited[eng].get(k, 0) < v:
                self.waited[eng][k] = v
                s = self.sem[k]
                self.ops[eng].append(lambda e, s=s, v=v: e.wait_ge(s, v))
                self.nins += 1

    def _commit(self, reads, writes, dep):
        k, v = dep
        for r in reads:
            st = self.res.setdefault(r, {"w": None, "r": {}})
            if v > st["r"].get(k, 0):
                st["r"][k] = v
        for w in writes:
            self.res[w] = {"w": dep, "r": {}}

    def op(self, eng, fn, reads=(), writes=()):
        self._emit_waits(eng, self._deps(reads, writes))
        self.cnt[eng] += 1
        n = self.cnt[eng]
        s = self.sem[eng]
        name, a, k = _record(fn)
        self.ops[eng].append(lambda e, name=name, a=a, k=k, s=s: getattr(e, name)(*a, **k).then_inc(s, 1))
        self.nins += 1
        self._commit(reads, writes, (eng, n))

    def dma(self, q, key, fn, reads=(), writes=()):
        if key.startswith("c8_") or key.startswith("c9_"):
            self.nuniq = getattr(self, "nuniq", 0) + 1
            key = f"{key}u{self.nuniq}"
        if key not in self.sem:
            self.newsem(key)
        self._emit_waits(q, self._deps(reads, writes))
        self.cnt[key] += 16
        n = self.cnt[key]
        s = self.sem[key]
        name, a, k = _record(fn)

        def run(e, name=name, a=a, k=k, s=s):
            try:
                k2 = {kk: (v.f(e) if isinstance(v, Late) else v) for kk, v in k.items()}
                return getattr(e, name)(*a, **k2).then_inc(s, 16)
            except Exception:
                print("FAILED DMA", q, key, name, [(kk, getattr(v, "shape", v), getattr(v, "ap", None)) for kk, v in k.items()])
                raise
        self.ops[q].append(run)
        self.nins += 1
        self._commit(reads, writes, (key, n))

    def barrier(self):
        for eng in ENG:
            self._emit_waits(eng, dict(self.cnt))
        self.res = {}
        self.nuniq = 0

    def finish(self):
        for eng in ENG:
            self._emit_waits(eng, dict(self.cnt))
        nc = self.nc
        ops = self.ops
        with nc.Block() as block:
            @block.sync
            def _(e):
                for f in ops["sp"]:
                    f(e)

            @block.tensor
            def _(e):
                for f in ops["pe"]:
                    f(e)

            @block.vector
            def _(e):
                for f in ops["dve"]:
                    f(e)

            @block.scalar
            def _(e):
                for f in ops["act"]:
                    f(e)

            @block.gpsimd
            def _(e):
                for f in ops["pool"]:
                    f(e)


class WStream:
    def __init__(self, P, ring, nslots, plan):
        self.P = P
        self.ring = ring
        self.n = nslots
        self.plan = plan
        self.issued = 0
        self.pos = 0

    def _issue(self, i):
        key, src = self.plan[i]
        slot = i % self.n
        nel = src.shape[1]
        dst = self.ring[:, slot, 0:nel]
        if nel > 2048:
            assert nel % 2048 == 0
            src = src.rearrange("p (a b) -> p a b", b=2048)
            dst = dst.rearrange("p (a b) -> p a b", b=2048)
        self.P.dma("pool", f"w{slot}", lambda e, d=dst, s=src: e.dma_start(out=d, in_=s),
                   writes=[f"w{slot}"])

    def next(self, key, held=0):
        i = self.pos
        assert self.plan[i][0] == key, (self.plan[i][0], key)
        while self.issued < min(len(self.plan), i - held + self.n):
            self._issue(self.issued)
            self.issued += 1
        self.pos += 1
        slot = i % self.n
        return self.ring[:, slot, :], f"w{slot}"


def plan_all(nlayers, stop=None):
    pl = []
    for l in range(nlayers):
        for blk in range(NBLK):
            pl += [("win", l, 6), ("win", l, 7), ("wglu", l)]
        if stop == "ssm":
            break
        for blk in range(NBLK):
            if blk % 4 == 0:
                pl += [("wmkv", l, i) for i in range(4)]
            pl += [("win", l, i) for i in (0, 1, 2, 3, 4, 5, 8, 9)]
            for jj in range(8):
                pl += [("win", l, NT_IN + jj * 3 + b) for b in range(3)] + [("wbr", l, jj)]
            pl += [("wout", l, c) for c in range(8)]
        if stop == "mix":
            break
        for e in range(NE):
            pl += [("wup", l, e, j) for j in range(8)] + [("wdn", l, e, k) for k in range(8)]
    return pl


INPUT_SPECS = [
    ("x", (T, D)), ("mem", (NSEQ * 256, D)), ("lnin", (2, 128, D)), ("lnp", (L, 4, 128, D)),
    ("win", (L, NT_IN + 24, 128, 4096)), ("wbr", (L, 8, 128, 4096)), ("wout", (L, 8, 128, 4096)),
    ("wglu", (L, 128, 4096)), ("wmkv", (L, 4, 128, 4096)),
    ("wr", (L, 128, 16 * 32)), ("br", (L, 128, 32)),
    ("wup", (L, NE, 8, 128, 4096)), ("wdn", (L, NE, 8, 128, 2048)),
    ("bup", (L, 128, NE * 16)), ("bdn", (L, NE, D)),
    ("sinks", (L, 128, 16)),
    ("ident", (128, 128)), ("ltri", (128, 128)), ("ltris", (128, 128)), ("ones", (128, 128)),
    ("mask_swa", (128, 256)), ("mask_first", (128, 256)), ("ebase", (128, NE)),
    ("sp1", (128, 1)), ("tp1", (128, 128)), ("bmask", (128, 512)),
    ("s_tok", (L, 3, 128, 2048)), ("s_cm", (L, 3, 128, 16)), ("s_b", (L, 5, 128, 256)),
    ("s_c", (L, 2, 128, 16 * 64)), ("s_d", (L, 128, 4)),
]


def build(nlayers=L, debug=(), stop=None):
    nc = bass.Bass("TRN2", target_bir_lowering=False)
    es = ExitStack()
    P = Prog(nc, es)
    din = {}
    for name, shape in INPUT_SPECS:
        if stop in ("ln0", "ssm", "mix") and name in ("wup", "wdn"):
            continue
        din[name] = nc.dram_tensor(name, list(shape), F32, kind="ExternalInput").ap()
    out = nc.dram_tensor("out", [T, D], F32, kind="ExternalOutput").ap()
    skind = "ExternalOutput" if debug else "Internal"
    h_tok = nc.dram_tensor("h_tok", [T, D], F32, kind=skind).ap()
    xg = nc.dram_tensor("xg", [NSLOT, D], BF16, kind=skind).ap()
    ypad = nc.dram_tensor("ypad", [NSLOT, D], BF16, kind=skind).ap()
    ssm_scr = nc.dram_tensor("ssm_scr", [NBLK, 128, 4 * BLK], BF16, kind=skind).ap()
    dbg = {}

    def dbg_out(name, shape, dt=F32):
        dbg[name] = nc.dram_tensor("dbg_" + name, list(shape), dt, kind="ExternalOutput").ap()
        return dbg[name]

    uid = [0]

    def alloc(stack, name, shape, dt=F32):
        uid[0] += 1
        return stack.enter_context(nc.sbuf_tensor(f"sb{uid[0]}_{name}", list(shape), dt))

    NRING = 6
    ring = alloc(es, "ring", [128, NRING, 4096], BF16)
    psum = [es.enter_context(nc.psum_tensor(f"ps{i}", [128, 512], F32)) for i in range(8)]
    ps_ctr = [0]

    def ps_next():
        i = ps_ctr[0] % 8
        ps_ctr[0] += 1
        return psum[i], f"ps{i}"

    identf = alloc(es, "identf", [128, 128], F32)
    identb = alloc(es, "identb", [128, 128], BF16)
    ltri = alloc(es, "ltri", [128, 128], BF16)
    ltris = alloc(es, "ltris", [128, 128], BF16)
    onesb = alloc(es, "onesb", [128, 128], BF16)
    neghalf = alloc(es, "neghalf", [128, 1], F32)
    gates_all = alloc(es, "gates_all", [128, T // 128, 4], F32)
    dest_all = alloc(es, "dest_all", [128, T // 128, 4], I32)
    gmat_all = alloc(es, "gmat_all", [128, T // 128, NE], F32)

    P.dma("sp", "c0", lambda e: e.dma_start(out=identf[:], in_=din["ident"]), writes=["identf"])
    P.dma("pool", "c1", lambda e: e.dma_start(out=identb[:], in_=din["ident"]), writes=["identb"])
    P.dma("pool", "c2", lambda e: e.dma_start(out=ltri[:], in_=din["ltri"]), writes=["ltri"])
    P.dma("pool", "c3", lambda e: e.dma_start(out=ltris[:], in_=din["ltris"]), writes=["ltris"])
    P.dma("pool", "c4", lambda e: e.dma_start(out=onesb[:], in_=din["ones"]), writes=["onesb"])
    P.op("dve", lambda e: e.memset(neghalf[:], -0.5), writes=["neghalf"])

    reg_cache = {}

    def _bc(e):
        if "bc" not in reg_cache:
            reg_cache["bc"] = e.to_reg(NSLOT - 1)
        return reg_cache["bc"]
    bc_reg = Late(_bc)

    plan = plan_all(nlayers, stop)

    def wsrc(key):
        if key[0] == "wglu":
            return din["wglu"][key[1]]
        if key[0] in ("wup", "wdn"):
            return din[key[0]][key[1], key[2], key[3]]
        return din[key[0]][key[1], key[2]]

    WS = WStream(P, ring, NRING, [(k, wsrc(k)) for k in plan])

    def layer_norm(x, xres, g, gres, b, bres, sm, tag):
        st, mv, ve, rs = sm
        for c in range(4):
            P.op("dve", lambda e, c=c: e.bn_stats(out=st[:, c, :], in_=x[:, c * 512:(c + 1) * 512]),
                 reads=[xres], writes=[f"{tag}st{c}"])
        P.op("dve", lambda e: e.bn_aggr(out=mv[:], in_=st[:].rearrange("p a b -> p (a b)")),
             reads=[f"{tag}st{c}" for c in range(4)], writes=[tag + "mv"])
        P.op("dve", lambda e: e.tensor_scalar(out=ve[:], in0=mv[:, 1:2], scalar1=EPS, scalar2=None, op0=ALU.add),
             reads=[tag + "mv"], writes=[tag + "ve"])
        P.op("pool", lambda e: e.tensor_tensor(out=rs[:], in0=ve[:], in1=neghalf[:], op=ALU.pow),
             reads=[tag + "ve", "neghalf"], writes=[tag + "rs"])
        P.op("dve", lambda e: e.tensor_scalar(out=x, in0=x, scalar1=mv[:, 0:1], scalar2=rs[:, 0:1],
                                              op0=ALU.subtract, op1=ALU.mult),
             reads=[xres, tag + "mv", tag + "rs"], writes=[xres])
        P.op("dve", lambda e: e.tensor_tensor(out=x, in0=x, in1=g, op=ALU.mult), reads=[xres, gres], writes=[xres])
        P.op("pool", lambda e: e.tensor_tensor(out=x, in0=x, in1=b, op=ALU.add), reads=[xres, bres], writes=[xres])

    def ln_small(stack, tag):
        return (alloc(stack, tag + "st", [128, 4, 6]), alloc(stack, tag + "mv", [128, 2]),
                alloc(stack, tag + "ve", [128, 1]), alloc(stack, tag + "rs", [128, 1]))

    def dump(name, ap, res, shape, dt=F32):
        if name in debug:
            d = dbg_out(name, shape, dt)
            P.dma("sp", "dbg", lambda e: e.dma_start(out=d, in_=ap), reads=[res])

    def phase_ln0():
        with ExitStack() as ph:
            xt = [alloc(ph, f"xt{i}", [128, D]) for i in range(2)]
            g = alloc(ph, "g0", [128, D])
            b = alloc(ph, "b0", [128, D])
            sm = [ln_small(ph, f"l0s{i}") for i in range(2)]
            P.dma("sp", "c5", lambda e: e.dma_start(out=g[:], in_=din["lnin"][0]), writes=["g0"])
            P.dma("sp", "c6", lambda e: e.dma_start(out=b[:], in_=din["lnin"][1]), writes=["b0"])
            for i in range(T // 128):
                s = i % 2
                P.dma("sp", f"ldx{s}", lambda e: e.dma_start(out=xt[s][:], in_=din["x"][i * 128:(i + 1) * 128, :]),
                      writes=[f"xt{s}"])
                layer_norm(xt[s][:], f"xt{s}", g[:], "g0", b[:], "b0", sm[s], f"l0{s}")
                P.dma("sp", f"stx{s}", lambda e: e.dma_start(out=h_tok[i * 128:(i + 1) * 128, :], in_=xt[s][:]),
                      reads=[f"xt{s}"], writes=[("htok", i)])
            P.barrier()

    def load_hT(blk, hT, tokf, tokb):
        for tt in range(4):
            ti = blk * 4 + tt
            s = tt % 2
            P.dma("sp", f"ldh{s}", lambda e: e.dma_start(out=tokf[s][:], in_=h_tok[ti * 128:(ti + 1) * 128, :]),
                  reads=[("htok", ti)], writes=[f"tokf{s}"])
            P.op("act", lambda e: e.activation(out=tokb[s][:], in_=tokf[s][:], func=AF.Copy),
                 reads=[f"tokf{s}"], writes=[f"tokb{s}"])
            for a in range(2):
                pb, pres = ps_next()
                pbb = pb[:].bitcast(BF16)
                for i in range(8):
                    kc = a * 8 + i
                    P.op("pe", lambda e: e.transpose(
                        out=pbb[:, i * 128:(i + 1) * 128], in_=tokb[s][:, kc * 128:(kc + 1) * 128], identity=identb[:]),
                        reads=[f"tokb{s}", "identb"], writes=[pres])
                P.op("dve", lambda e: e.tensor_copy(
                    out=hT[:, a * 8:(a + 1) * 8, tt * 128:(tt + 1) * 128],
                    in_=pbb.rearrange("p (a b) -> p a b", b=128)),
                    reads=[pres], writes=["hT"])

    def proj_tile(wt, wres, ncols_tiles, hT, evac):
        w3 = wt.rearrange("p (k c) -> p k c", c=256)
        for ct in range(ncols_tiles):
            pb, pres = ps_next()
            for kc in range(16):
                P.op("pe", lambda e: e.matmul(
                    pb[:], w3[:, kc, ct * 128:(ct + 1) * 128], hT[:, kc, :], start=(kc == 0), stop=(kc == 15)),
                    reads=[wres, "hT"], writes=[pres])
            evac(ct, pb, pres)

    def cis(A, TH, out_re, out_im, tmp, rA, rTH, rout):
        MG, Q, N1, F, SC = tmp
        P.op("act", lambda e: e.activation(out=MG, in_=A, func=AF.Exp), reads=[rA], writes=["cMG"])
        P.op("dve", lambda e: e.tensor_scalar(out=Q, in0=TH, scalar1=1.0 / TWO_PI, scalar2=None, op0=ALU.mult),
             reads=[rTH], writes=["cQ"])
        for off, o in ((0.0, out_im), (0.25, out_re)):
            P.op("dve", lambda e: e.tensor_scalar(out=N1, in0=Q, scalar1=off + MAGIC, scalar2=None, op0=ALU.add),
                 reads=["cQ"], writes=["cN1"])
            P.op("dve", lambda e: e.tensor_scalar(out=N1, in0=N1, scalar1=-MAGIC, scalar2=None, op0=ALU.add),
                 reads=["cN1"], writes=["cN1"])
            P.op("dve", lambda e: e.scalar_tensor_tensor(out=F, in0=Q, scalar=off, in1=N1, op0=ALU.add, op1=ALU.subtract),
                 reads=["cQ", "cN1"], writes=["cF"])
            P.op("act", lambda e: e.activation(out=SC, in_=F, func=AF.Sin, scale=TWO_PI * (1.0 - 1e-6)),
                 reads=["cF"], writes=["cSC"])
            P.op("dve", lambda e: e.tensor_tensor(out=o, in0=MG, in1=SC, op=ALU.mult),
                 reads=["cMG", "cSC"], writes=[rout])

    def phase_ssm(l):
        with ExitStack() as ph:
            Em = alloc(ph, "Em", [128, 4, 2, 512], BF16)
            Ep = alloc(ph, "Ep", [128, 16, 2, 128], F32)
            Bf = alloc(ph, "Bf", [128, 4, 1024], BF16)
            Cm = alloc(ph, "Cm", [128, 16, 2, 64], BF16)
            dcol = alloc(ph, "dcol", [128, 4], F32)
            P.dma("sp", "c7", lambda e: e.dma_start(out=dcol[:], in_=din["s_d"][l]), writes=["dcol"])
            with ExitStack() as tg:
                stok = alloc(tg, "stok", [128, 3, 2048], F32)
                scm = alloc(tg, "scm", [128, 3, 16], F32)
                sb = alloc(tg, "sb", [128, 5, 256], F32)
                sc = alloc(tg, "sc", [128, 2, 1024], F32)
                sp1 = alloc(tg, "sp1", [128, 1], F32)
                tp1 = alloc(tg, "tp1", [128, 128], F32)
                bmask = alloc(tg, "bmask", [128, 512], F32)
                ctmp = [alloc(tg, f"ct{i}", [128, 512], F32) for i in range(5)]
                tA = alloc(tg, "ctA", [128, 512], F32)
                tT = alloc(tg, "ctT", [128, 512], F32)
                sm = [alloc(tg, f"csm{i}", [128, 256], F32) for i in range(8)]
                arcm = alloc(tg, "arcm", [128, 16], F32)
                aicm = alloc(tg, "aicm", [128, 16], F32)
                dtcm = alloc(tg, "dtcm", [128, 16], F32)
                for i in range(3):
                    P.dma("sp", "c8_1", lambda e: e.dma_start(out=stok[:, i, :], in_=din["s_tok"][l, i]), writes=["stok"])
                    P.dma("sp", "c8_2", lambda e: e.dma_start(out=scm[:, i, :], in_=din["s_cm"][l, i]), writes=["scm"])
                for i in range(5):
                    P.dma("sp", "c8_3", lambda e: e.dma_start(out=sb[:, i, :], in_=din["s_b"][l, i]), writes=["sb"])
                for i in range(2):
                    P.dma("sp", "c8_4", lambda e: e.dma_start(out=sc[:, i, :], in_=din["s_c"][l, i]), writes=["sc"])
                P.dma("sp", "c8_5", lambda e: e.dma_start(out=sp1[:], in_=din["sp1"]), writes=["sp1"])
                P.dma("sp", "c8_6", lambda e: e.dma_start(out=tp1[:], in_=din["tp1"]), writes=["tp1"])
                P.dma("sp", "c8_7", lambda e: e.dma_start(out=bmask[:], in_=din["bmask"]), writes=["bmask"])
                for m in range(4):
                    sl = slice(m * 512, (m + 1) * 512)
                    P.op("act", lambda e: e.activation(out=ctmp[0][:], in_=stok[:, 2, sl], func=AF.Exp),
                         reads=["stok"], writes=["cdt"])
                    P.op("dve", lambda e: e.tensor_tensor(out=tA[:], in0=stok[:, 0, sl], in1=ctmp[0][:], op=ALU.mult),
                         reads=["stok", "cdt"], writes=["cA"])
                    P.op("dve", lambda e: e.tensor_tensor(out=tT[:], in0=stok[:, 1, sl], in1=ctmp[0][:], op=ALU.mult),
                         reads=["stok", "cdt"], writes=["cT"])
                    P.op("dve", lambda e: e.tensor_scalar(out=tA[:], in0=tA[:], scalar1=sp1[:, 0:1], scalar2=-1.0, op0=ALU.mult, op1=ALU.mult),
                         reads=["cA", "sp1"], writes=["cA"])
                    P.op("dve", lambda e: e.tensor_scalar(out=tT[:], in0=tT[:], scalar1=sp1[:, 0:1], scalar2=-1.0, op0=ALU.mult, op1=ALU.mult),
                         reads=["cT", "sp1"], writes=["cT"])
                    cis(tA[:], tT[:], Em[:, m, 0, :], Em[:, m, 1, :], [t[:] for t in ctmp], "cA", "cT", "Em")
                P.op("act", lambda e: e.activation(out=dtcm[:], in_=scm[:, 2, :], func=AF.Exp), reads=["scm"], writes=["dtcm"])
                P.op("dve", lambda e: e.tensor_tensor(out=arcm[:], in0=scm[:, 0, :], in1=dtcm[:], op=ALU.mult), reads=["scm", "dtcm"], writes=["arcm"])
                P.op("dve", lambda e: e.tensor_tensor(out=aicm[:], in0=scm[:, 1, :], in1=dtcm[:], op=ALU.mult), reads=["scm", "dtcm"], writes=["aicm"])

                def r3(t):
                    return t[:].rearrange("p (a b) -> p a b", b=128)
                tb = tp1[:].unsqueeze(1).broadcast_to([128, 4, 128])
                for jg in range(4):
                    P.op("dve", lambda e: e.tensor_tensor(
                        out=r3(tA), in0=tb, in1=arcm[:, 4 * jg:4 * jg + 4].unsqueeze(2).broadcast_to([128, 4, 128]), op=ALU.mult),
                        reads=["tp1", "arcm"], writes=["cA"])
                    P.op("dve", lambda e: e.tensor_tensor(
                        out=r3(tT), in0=tb, in1=aicm[:, 4 * jg:4 * jg + 4].unsqueeze(2).broadcast_to([128, 4, 128]), op=ALU.mult),
                        reads=["tp1", "aicm"], writes=["cT"])
                    cis(r3(tA), r3(tT), Ep[:, 4 * jg:4 * jg + 4, 0, :], Ep[:, 4 * jg:4 * jg + 4, 1, :],
                        [r3(t) for t in ctmp], "cA", "cT", "Ep")

                def s2(t):
                    return t[:, 0:256]
                lre, lim, ldt, bre, bim = (sb[:, i, :] for i in range(5))
                LR, LI, NR, DEN, KR, KI, X1, X2 = (t[:] for t in sm)
                P.op("act", lambda e: e.activation(out=X1, in_=ldt, func=AF.Exp), reads=["sb"], writes=["X1"])
                P.op("dve", lambda e: e.tensor_tensor(out=s2(tA), in0=lre, in1=X1, op=ALU.mult), reads=["sb", "X1"], writes=["cA"])
                P.op("dve", lambda e: e.tensor_tensor(out=s2(tT), in0=lim, in1=X1, op=ALU.mult), reads=["sb", "X1"], writes=["cT"])
                cis(s2(tA), s2(tT), LR, LI, [s2(t) for t in ctmp], "cA", "cT", "LRI")
                P.op("dve", lambda e: e.tensor_scalar(out=NR, in0=LR, scalar1=-1.0, scalar2=None, op0=ALU.add), reads=["LRI"], writes=["NR"])
                P.op("dve", lambda e: e.tensor_tensor(out=X1, in0=lre, in1=lre, op=ALU.mult), reads=["sb"], writes=["X1"])
                P.op("dve", lambda e: e.tensor_tensor(out=X2, in0=lim, in1=lim, op=ALU.mult), reads=["sb"], writes=["X2"])
                P.op("dve", lambda e: e.tensor_tensor(out=DEN, in0=X1, in1=X2, op=ALU.add), reads=["X1", "X2"], writes=["DEN"])
                P.op("dve", lambda e: e.reciprocal(out=DEN, in_=DEN), reads=["DEN"], writes=["DEN"])
                P.op("dve", lambda e: e.tensor_tensor(out=X1, in0=NR, in1=lre, op=ALU.mult), reads=["NR", "sb"], writes=["X1"])
                P.op("dve", lambda e: e.tensor_tensor(out=X2, in0=LI, in1=lim, op=ALU.mult), reads=["LRI", "sb"], writes=["X2"])
                P.op("dve", lambda e: e.tensor_tensor(out=KR, in0=X1, in1=X2, op=ALU.add), reads=["X1", "X2"], writes=["KR"])
                P.op("dve", lambda e: e.tensor_tensor(out=KR, in0=KR, in1=DEN, op=ALU.mult), reads=["KR", "DEN"], writes=["KR"])
                P.op("dve", lambda e: e.tensor_tensor(out=X1, in0=LI, in1=lre, op=ALU.mult), reads=["LRI", "sb"], writes=["X1"])
                P.op("dve", lambda e: e.tensor_tensor(out=X2, in0=NR, in1=lim, op=ALU.mult), reads=["NR", "sb"], writes=["X2"])
                P.op("dve", lambda e: e.tensor_tensor(out=KI, in0=X1, in1=X2, op=ALU.subtract), reads=["X1", "X2"], writes=["KI"])
                P.op("dve", lambda e: e.tensor_tensor(out=KI, in0=KI, in1=DEN, op=ALU.mult), reads=["KI", "DEN"], writes=["KI"])
                P.op("dve", lambda e: e.tensor_tensor(out=X1, in0=KR, in1=bre, op=ALU.mult), reads=["KR", "sb"], writes=["X1"])
                P.op("dve", lambda e: e.tensor_tensor(out=X2, in0=KI, in1=bim, op=ALU.mult), reads=["KI", "sb"], writes=["X2"])
                P.op("dve", lambda e: e.tensor_tensor(out=LR, in0=X1, in1=X2, op=ALU.subtract), reads=["X1", "X2", "LRI"], writes=["BR"])
                P.op("dve", lambda e: e.tensor_tensor(out=X1, in0=KR, in1=bim, op=ALU.mult), reads=["KR", "sb"], writes=["X1"])
                P.op("dve", lambda e: e.tensor_tensor(out=X2, in0=KI, in1=bre, op=ALU.mult), reads=["KI", "sb"], writes=["X2"])
                P.op("dve", lambda e: e.tensor_tensor(out=LI, in0=X1, in1=X2, op=ALU.add), reads=["X1", "X2", "BR"], writes=["BI"])
                bm3 = bmask[:].rearrange("p (a b) -> p a b", b=64)
                for kc in range(4):
                    for r, src, rn in ((0, LR, "BR"), (1, LI, "BI")):
                        P.op("dve", lambda e: e.tensor_tensor(
                            out=Bf[:, kc, r * 512:(r + 1) * 512].rearrange("p (a b) -> p a b", b=64),
                            in0=src[:, kc * 64:(kc + 1) * 64].unsqueeze(1).broadcast_to([128, 8, 64]), in1=bm3, op=ALU.mult),
                            reads=[rn, "bmask"], writes=["Bf"])
                P.op("dve", lambda e: e.tensor_copy(out=Cm[:, :, 0, :], in_=sc[:, 0, :].rearrange("p (a b) -> p a b", b=64)),
                     reads=["sc"], writes=["Cm"])
                P.op("dve", lambda e: e.tensor_scalar(out=Cm[:, :, 1, :], in0=sc[:, 1, :].rearrange("p (a b) -> p a b", b=64),
                                                      scalar1=-1.0, scalar2=None, op0=ALU.mult),
                     reads=["sc"], writes=["Cm"])
                dump("Em", Em[:], "Em", [128, 4, 2, 512], BF16)
                dump("Ep", Ep[:], "Ep", [128, 16, 2, 128])
                dump("Bf", Bf[:], "Bf", [128, 4, 1024], BF16)
                dump("Cm", Cm[:], "Cm", [128, 16, 2, 64], BF16)
                P.barrier()
            hT = alloc(ph, "hT", [128, 16, BLK], BF16)
            tokf = [alloc(ph, f"tokf{i}", [128, D], F32) for i in range(2)]
            tokb = [alloc(ph, f"tokb{i}", [128, D], BF16) for i in range(2)]
            uT = alloc(ph, "uT", [128, 4, BLK], BF16)
            stt = [alloc(ph, f"st{i}", [128, 512], F32) for i in range(6)]
            wri = [[alloc(ph, f"w{a}{r}", [128, 512], BF16) for r in range(2)] for a in range(2)]
            x32 = [alloc(ph, f"x32{r}", [128, 4, 128], F32) for r in range(2)]
            Sc = alloc(ph, "Sc", [128, 4, 4, 2], F32)
            xTq = alloc(ph, "xTq", [128, 4, 2, BLK], BF16)
            zT = alloc(ph, "zT", [128, 4, BLK], BF16)
            soT = alloc(ph, "soT", [128, 4, BLK], BF16)
            t3 = [t[:].rearrange("p (a b) -> p a b", b=128) for t in stt]
            par = 0
            for blk in range(NBLK):
                if blk % 4 == 0:
                    P.op("dve", lambda e: e.memset(Sc[:], 0.0), writes=["Sc"])
                load_hT(blk, hT, tokf, tokb)
                for i, tno in enumerate((6, 7)):
                    wt, wres = WS.next(("win", l, tno))

                    def ev_u(ct, pb, pres):
                        P.op("act", lambda e: e.activation(out=uT[:, 2 * i + ct, :], in_=pb[:], func=AF.Copy),
                             reads=[pres], writes=["uT"])
                    proj_tile(wt, wres, 2, hT, ev_u)
                if blk == 0:
                    dump("uT", uT[:], "uT", [128, 4, BLK], BF16)
                for m in range(4):
                    for ck in range(4):
                        tr = slice(ck * 128, (ck + 1) * 128)
                        pB = [ps_next() for _ in range(2)]
                        for r in range(2):
                            P.op("pe", lambda e: e.matmul(
                                pB[r][0][:], uT[:, m, tr], Bf[:, m, r * 512:(r + 1) * 512], start=True, stop=True),
                                reads=["uT", "Bf"], writes=[pB[r][1]])
                        w_ = wri[par]
                        wn = [f"w{par}{r}" for r in range(2)]
                        Er, Ei = Em[:, m, 0, :], Em[:, m, 1, :]
                        P.op("dve", lambda e: e.tensor_tensor(out=stt[0][:], in0=pB[0][0][:], in1=Er, op=ALU.mult),
                             reads=[pB[0][1], "Em"], writes=["st0"])
                        P.op("dve", lambda e: e.tensor_tensor(out=stt[1][:], in0=pB[1][0][:], in1=Ei, op=ALU.mult),
                             reads=[pB[1][1], "Em"], writes=["st1"])
                        P.op("dve", lambda e: e.tensor_tensor(out=w_[0][:], in0=stt[0][:], in1=stt[1][:], op=ALU.subtract),
                             reads=["st0", "st1"], writes=[wn[0]])
                        P.op("dve", lambda e: e.tensor_tensor(out=stt[2][:], in0=pB[0][0][:], in1=Ei, op=ALU.mult),
                             reads=[pB[0][1], "Em"], writes=["st2"])
                        P.op("dve", lambda e: e.tensor_tensor(out=stt[3][:], in0=pB[1][0][:], in1=Er, op=ALU.mult),
                             reads=[pB[1][1], "Em"], writes=["st3"])
                        P.op("pool", lambda e: e.tensor_tensor(out=w_[1][:], in0=stt[2][:], in1=stt[3][:], op=ALU.add),
                             reads=["st2", "st3"], writes=[wn[1]])
                        pZ = [ps_next() for _ in range(2)]
                        for r in range(2):
                            for ct in range(4):
                                P.op("pe", lambda e: e.matmul(
                                    pZ[r][0][:, ct * 128:(ct + 1) * 128], w_[r][:, ct * 128:(ct + 1) * 128], ltri[:],
                                    start=True, stop=True),
                                    reads=[wn[r], "ltri"], writes=[pZ[r][1]])
                        par ^= 1
                        Epr, Epi = Ep[:, 4 * m:4 * m + 4, 0, :], Ep[:, 4 * m:4 * m + 4, 1, :]
                        for r in range(2):
                            P.op("dve", lambda e: e.tensor_tensor(
                                out=t3[r], in0=pZ[r][0][:].rearrange("p (a b) -> p a b", b=128),
                                in1=Sc[:, m, :, r:r + 1].broadcast_to([128, 4, 128]), op=ALU.add),
                                reads=[pZ[r][1], "Sc"], writes=[f"st{r}"])
                        P.op("pool", lambda e: e.tensor_tensor(out=t3[2], in0=t3[0], in1=Epr, op=ALU.mult), reads=["st0", "Ep"], writes=["st2"])
                        P.op("dve", lambda e: e.tensor_tensor(out=t3[3], in0=t3[1], in1=Epi, op=ALU.mult), reads=["st1", "Ep"], writes=["st3"])
                        P.op("dve", lambda e: e.tensor_tensor(out=x32[0][:], in0=t3[2], in1=t3[3], op=ALU.subtract), reads=["st2", "st3"], writes=["x320"])
                        P.op("pool", lambda e: e.tensor_tensor(out=t3[4], in0=t3[0], in1=Epi, op=ALU.mult), reads=["st0", "Ep"], writes=["st4"])
                        P.op("dve", lambda e: e.tensor_tensor(out=t3[5], in0=t3[1], in1=Epr, op=ALU.mult), reads=["st1", "Ep"], writes=["st5"])
                        P.op("dve", lambda e: e.tensor_tensor(out=x32[1][:], in0=t3[4], in1=t3[5], op=ALU.add), reads=["st4", "st5"], writes=["x321"])
                        for r in range(2):
                            P.op("act", lambda e: e.activation(out=Sc[:, m, :, r:r + 1], in_=x32[r][:, :, 127:128], func=AF.Copy),
                                 reads=[f"x32{r}"], writes=["Sc"])
                            P.op("act", lambda e: e.activation(out=xTq[:, :, r, tr], in_=x32[r][:], func=AF.Copy),
                                 reads=[f"x32{r}"], writes=["xTq"])
                    pY, pYr = ps_next()
                    for ct in range(4):
                        for r in range(2):
                            P.op("pe", lambda e: e.matmul(
                                pY[64 * (ct // 2):64 * (ct // 2) + 64, :], Cm[:, 4 * m + ct, r, :], xTq[:, ct, r, :],
                                start=(ct % 2 == 0 and r == 0), stop=(ct % 2 == 1 and r == 1)),
                                reads=["Cm", "xTq"], writes=[pYr])
                    y, x2, inn, sg = stt[0][:], stt[1][:], stt[2][:], stt[3][:]
                    P.op("dve", lambda e: e.scalar_tensor_tensor(out=y, in0=uT[:, m, :], scalar=dcol[:, m:m + 1], in1=pY[:],
                                                                 op0=ALU.mult, op1=ALU.add),
                         reads=["uT", "dcol", pYr], writes=["st0"])
                    if blk == 0:
                        dump(f"yssm{m}", y, "st0", [128, 512])
                    P.op("pool", lambda e: e.tensor_tensor(out=x2, in0=y, in1=y, op=ALU.mult), reads=["st0"], writes=["st1"])
                    P.op("dve", lambda e: e.tensor_scalar(out=x2, in0=x2, scalar1=0.044715, scalar2=1.0, op0=ALU.mult, op1=ALU.add),
                         reads=["st1"], writes=["st1"])
                    P.op("pool", lambda e: e.tensor_tensor(out=inn, in0=x2, in1=y, op=ALU.mult), reads=["st0", "st1"], writes=["st2"])
                    P.op("act", lambda e: e.activation(out=sg, in_=inn, func=AF.Sigmoid, scale=1.5957691216057308),
                         reads=["st2"], writes=["st3"])
                    P.op("dve", lambda e: e.tensor_tensor(out=zT[:, m, :], in0=y, in1=sg, op=ALU.mult),
                         reads=["st0", "st3"], writes=["zT"])
                wt, wres = WS.next(("wglu", l))
                wg = wt.rearrange("p (k c) -> p k c", c=1024)
                for i in range(4):
                    pV, pG = ps_next(), ps_next()
                    for (pb, pres), off in ((pV, 0), (pG, 512)):
                        for kc in range(4):
                            P.op("pe", lambda e: e.matmul(
                                pb[:], wg[:, kc, off + i * 128:off + (i + 1) * 128], zT[:, kc, :], start=(kc == 0), stop=(kc == 3)),
                                reads=[wres, "zT"], writes=[pres])
                    P.op("act", lambda e: e.activation(out=stt[4][:], in_=pG[0][:], func=AF.Sigmoid), reads=[pG[1]], writes=["st4"])
                    P.op("dve", lambda e: e.tensor_tensor(out=soT[:, i, :], in0=pV[0][:], in1=stt[4][:], op=ALU.mult),
                         reads=[pV[1], "st4"], writes=["soT"])
                P.dma("sp", "sso", lambda e: e.dma_start(out=ssm_scr[blk], in_=soT[:].rearrange("p a b -> p (a b)")),
                      reads=["soT"], writes=[("ssm_scr", blk)])
            P.barrier()

    def phase_mix(l):
        with ExitStack() as ph:
            hT = alloc(ph, "hT", [128, 16, BLK], BF16)
            tokf = [alloc(ph, f"tokf{i}", [128, D], F32) for i in range(2)]
            tokb = [alloc(ph, f"tokb{i}", [128, D], BF16) for i in range(2)]
            qT = alloc(ph, "qT", [128, 8, BLK], BF16)
            kTd = alloc(ph, "kTd", [128, 2, 640], BF16)
            v_tok = alloc(ph, "v_tok", [128, 5, 128], BF16)
            qmT = alloc(ph, "qmT", [128, 4, BLK], BF16)
            aoT = alloc(ph, "aoT", [128, 8, BLK], BF16)
            moT = alloc(ph, "moT", [128, 4, BLK], BF16)
            soT = alloc(ph, "soT", [128, 4, BLK], BF16)
            mergedT = alloc(ph, "mergedT", [128, 16, BLK], BF16)
            wk = alloc(ph, "wk", [128, 4096], F32)
            PT = alloc(ph, "PT", [128, 16, 128], BF16)
            mem_kT = alloc(ph, "mem_kT", [128, 4, 256], BF16)
            mem_v = alloc(ph, "mem_v", [128, 2, 512], BF16)
            lng = alloc(ph, "lng", [128, D], F32)
            lnb = alloc(ph, "lnb", [128, D], F32)
            lsm = [ln_small(ph, f"l1s{i}") for i in range(4)]
            sinks = alloc(ph, "sinks", [128, 16], F32)
            mask_swa = alloc(ph, "mask_swa", [128, 256], F32)
            mask_first = alloc(ph, "mask_first", [128, 256], F32)
            wr = alloc(ph, "wr", [128, 16, 32], F32)
            brt = alloc(ph, "brt", [128, 32], F32)
            ebase = alloc(ph, "ebase", [128, NE], F32)
            cum = alloc(ph, "cum", [128, NE], F32)
            sg = [alloc(ph, f"sg{i}", [128, BLK], F32) for i in range(3)]
            mt = [alloc(ph, f"mt{i}", [128, BLK], F32) for i in range(3)]
            mx = alloc(ph, "mx", [128, 8], F32)
            negm = alloc(ph, "negm", [128, 8], F32)
            ssum = alloc(ph, "ssum", [128, 8], F32)
            t8 = alloc(ph, "t8", [128, 8], F32)
            es8 = alloc(ph, "es8", [128, 8], F32)
            rr = alloc(ph, "rr", [128, 8], F32)
            lg = alloc(ph, "lg", [128, NE], F32)
            top8 = alloc(ph, "top8", [128, 8], F32)
            mb = alloc(ph, "mb", [128, NE], BF16)
            pos = alloc(ph, "pos", [128, NE], F32)
            ov = alloc(ph, "ov", [128, NE], F32)
            mk = alloc(ph, "mk", [128, NE], F32)
            junk = alloc(ph, "junk", [128, NE], F32)
            destf = alloc(ph, "destf", [128, 4], F32)
            nm1 = alloc(ph, "nm1", [128, 1], F32)
            eg = alloc(ph, "eg", [128, 4], F32)
            gs = alloc(ph, "gs", [128, 1], F32)

            memT = mergedT[:, :, 0:256]
            Sm = wk[:, 0:2048].rearrange("p (a b) -> p a b", b=256)
            Pb = wk[:, 2048:3072].bitcast(BF16).rearrange("p (a b) -> p a b", b=256)
            Pn = wk[:, 3072:4096].bitcast(BF16).rearrange("p (a b) -> p a b", b=256)
            rb = [tokf[0][:], tokf[1][:], wk[:, 0:2048], wk[:, 2048:4096]]
            rbres = [["tokf0"], ["tokf1"], ["Sm"], ["Pb", "Pn"]]
            h1T = qT[:].rearrange("p a b -> p (a b)").bitcast(F32).rearrange("p (a b) -> p a b", b=128)

            P.dma("sp", "c9_1", lambda e: e.dma_start(out=lng[:], in_=din["lnp"][l, 0]), writes=["lng"])
            P.dma("sp", "c9_2", lambda e: e.dma_start(out=lnb[:], in_=din["lnp"][l, 1]), writes=["lnb"])
            P.dma("sp", "c9_3", lambda e: e.dma_start(out=sinks[:], in_=din["sinks"][l]), writes=["sinks"])
            P.dma("sp", "c9_4", lambda e: e.dma_start(out=mask_swa[:], in_=din["mask_swa"]), writes=["mask_swa"])
            P.dma("sp", "c9_5", lambda e: e.dma_start(out=mask_first[:], in_=din["mask_first"]), writes=["mask_first"])
            P.dma("sp", "c9_6", lambda e: e.dma_start(out=wr[:].rearrange("p a b -> p (a b)"), in_=din["wr"][l]), writes=["wr"])
            P.dma("sp", "c9_7", lambda e: e.dma_start(out=brt[:], in_=din["br"][l]), writes=["brt"])
            P.dma("sp", "c9_8", lambda e: e.dma_start(out=ebase[:], in_=din["ebase"]), writes=["ebase"])
            P.op("dve", lambda e: e.memset(cum[:], 0.0), writes=["cum"])

            def softmax_pv(nh, pieces, sink_ap, do_pv, out_evac):
                for ap3, res, h0 in pieces:
                    n = ap3.shape[1]
                    P.op("dve", lambda e: e.tensor_reduce(out=mx[:, h0:h0 + n], in_=ap3, axis=AX.X, op=ALU.max),
                         reads=[res], writes=["mx"])
                if sink_ap is not None:
                    P.op("dve", lambda e: e.tensor_tensor(out=mx[:, 0:nh], in0=mx[:, 0:nh], in1=sink_ap, op=ALU.max),
                         reads=["mx", "sinks"], writes=["mx"])
                for ap3, res, h0 in pieces:
                    n = ap3.shape[1]
                    P.op("dve", lambda e: e.tensor_tensor(out=Sm[:, h0:h0 + n, :], in0=ap3,
                                                          in1=mx[:, h0:h0 + n].unsqueeze(2).broadcast_to([128, n, 256]), op=ALU.subtract),
                         reads=[res, "mx"], writes=["Sm"])
                P.op("act", lambda e: e.activation(out=Pb[:, 0:nh, :], in_=Sm[:, 0:nh, :], func=AF.Exp), reads=["Sm"], writes=["Pb"])
                P.op("dve", lambda e: e.tensor_reduce(out=ssum[:, 0:nh], in_=Pb[:, 0:nh, :], axis=AX.X, op=ALU.add),
                     reads=["Pb"], writes=["ssum"])
                if sink_ap is not None:
                    P.op("dve", lambda e: e.tensor_tensor(out=t8[:, 0:nh], in0=sink_ap, in1=mx[:, 0:nh], op=ALU.subtract),
                         reads=["mx", "sinks"], writes=["t8"])
                    P.op("act", lambda e: e.activation(out=es8[:, 0:nh], in_=t8[:, 0:nh], func=AF.Exp), reads=["t8"], writes=["es8"])
                    P.op("dve", lambda e: e.tensor_tensor(out=ssum[:, 0:nh], in0=ssum[:, 0:nh], in1=es8[:, 0:nh], op=ALU.add),
                         reads=["ssum", "es8"], writes=["ssum"])
                P.op("dve", lambda e: e.reciprocal(out=rr[:, 0:nh], in_=ssum[:, 0:nh]), reads=["ssum"], writes=["rr"])
                P.op("dve", lambda e: e.tensor_tensor(out=Pn[:, 0:nh, :], in0=Pb[:, 0:nh, :],
                                                      in1=rr[:, 0:nh].unsqueeze(2).broadcast_to([128, nh, 256]), op=ALU.mult),
                     reads=["Pb", "rr"], writes=["Pn"])
                for a in range(nh // 4):
                    pb, pres = ps_next()
                    pbb = pb[:].bitcast(BF16)
                    for j in range(8):
                        idx = a * 8 + j
                        i, kb = idx // 2, idx % 2
                        P.op("pe", lambda e: e.transpose(out=pbb[:, j * 128:(j + 1) * 128], in_=Pn[:, i, kb * 128:(kb + 1) * 128],
                                                         identity=identb[:]),
                             reads=["Pn", "identb"], writes=[pres])
                    P.op("act", lambda e: e.activation(out=PT[:, a * 8:(a + 1) * 8, :], in_=pbb.rearrange("p (a b) -> p a b", b=128),
                                                       func=AF.Copy),
                         reads=[pres], writes=["PT"])
                pso, psores = ps_next()
                do_pv(pso, psores)
                out_evac(pso, psores)

            for blk in range(NBLK):
                seq = blk // 4
                if blk % 4 == 0:
                    for mtile in range(2):
                        s = mtile % 2
                        r0 = seq * 256 + mtile * 128
                        P.dma("sp", f"ldh{s}", lambda e: e.dma_start(out=tokf[s][:], in_=din["mem"][r0:r0 + 128, :]), writes=[f"tokf{s}"])
                        P.op("act", lambda e: e.activation(out=tokb[s][:], in_=tokf[s][:], func=AF.Copy),
                             reads=[f"tokf{s}"], writes=[f"tokb{s}"])
                        for a in range(2):
                            pb, pres = ps_next()
                            pbb = pb[:].bitcast(BF16)
                            for i in range(8):
                                kc = a * 8 + i
                                P.op("pe", lambda e: e.transpose(out=pbb[:, i * 128:(i + 1) * 128],
                                                                 in_=tokb[s][:, kc * 128:(kc + 1) * 128], identity=identb[:]),
                                     reads=[f"tokb{s}", "identb"], writes=[pres])
                            P.op("dve", lambda e: e.tensor_copy(out=memT[:, a * 8:(a + 1) * 8, mtile * 128:(mtile + 1) * 128],
                                                                in_=pbb.rearrange("p (a b) -> p a b", b=128)),
                                 reads=[pres], writes=["mergedT"])
                    for i in range(2):
                        wt, wres = WS.next(("wmkv", l, i))
                        w3 = wt.rearrange("p (k c) -> p k c", c=256)
                        for ct in range(2):
                            pb, pres = ps_next()
                            for kc in range(16):
                                P.op("pe", lambda e: e.matmul(pb[:, 0:256], w3[:, kc, ct * 128:(ct + 1) * 128], memT[:, kc, :],
                                                              start=(kc == 0), stop=(kc == 15)),
                                     reads=[wres, "mergedT"], writes=[pres])
                            P.op("act", lambda e: e.activation(out=mem_kT[:, 2 * i + ct, :], in_=pb[:, 0:256], func=AF.Copy),
                                 reads=[pres], writes=["mem_kT"])
                    for i in range(2):
                        wt, wres = WS.next(("wmkv", l, 2 + i))
                        w3 = wt.rearrange("p (k c) -> p k c", c=256)
                        for mtile in range(2):
                            pb, pres = ps_next()
                            for kc in range(16):
                                P.op("pe", lambda e: e.matmul(pb[:, 0:256], memT[:, kc, mtile * 128:(mtile + 1) * 128], w3[:, kc, :],
                                                              start=(kc == 0), stop=(kc == 15)),
                                     reads=[wres, "mergedT"], writes=[pres])
                            P.op("act", lambda e: e.activation(out=mem_v[:, mtile, i * 256:(i + 1) * 256], in_=pb[:, 0:256], func=AF.Copy),
                                 reads=[pres], writes=["mem_v"])
                    P.op("dve", lambda e: e.memset(kTd[:, :, 0:128], 0.0), writes=["kTd"])
                    P.op("dve", lambda e: e.memset(v_tok[:, 0, :], 0.0), writes=["v_tok"])

                load_hT(blk, hT, tokf, tokb)
                P.dma("sp", "lso", lambda e: e.dma_start(out=soT[:].rearrange("p a b -> p (a b)"), in_=ssm_scr[blk]),
                      reads=[("ssm_scr", blk)], writes=["soT"])
                for i in range(4):
                    wt, wres = WS.next(("win", l, i))

                    def ev_q(ct, pb, pres):
                        P.op("act", lambda e: e.activation(out=qT[:, 2 * i + ct, :], in_=pb[:], func=AF.Copy, scale=0.125),
                             reads=[pres], writes=["qT"])
                    proj_tile(wt, wres, 2, hT, ev_q)
                wt, wres = WS.next(("win", l, 4))

                def ev_k(ct, pb, pres):
                    P.op("act", lambda e: e.activation(out=kTd[:, ct, 128:640], in_=pb[:], func=AF.Copy), reads=[pres], writes=["kTd"])
                proj_tile(wt, wres, 2, hT, ev_k)
                wt, wres = WS.next(("win", l, 5))
                w3 = wt.rearrange("p (k c) -> p k c", c=256)
                for tt in range(4):
                    pb, pres = ps_next()
                    for kc in range(16):
                        P.op("pe", lambda e: e.matmul(pb[:, 0:128], hT[:, kc, tt * 128:(tt + 1) * 128], w3[:, kc, 0:128],
                                                      start=(kc == 0), stop=(kc == 15)),
                             reads=[wres, "hT"], writes=[pres])
                    P.op("act", lambda e: e.activation(out=v_tok[:, 1 + tt, :], in_=pb[:, 0:128], func=AF.Copy), reads=[pres], writes=["v_tok"])
                for i in range(2):
                    wt, wres = WS.next(("win", l, 8 + i))

                    def ev_qm(ct, pb, pres):
                        P.op("act", lambda e: e.activation(out=qmT[:, 2 * i + ct, :], in_=pb[:], func=AF.Copy, scale=128.0 ** -0.5),
                             reads=[pres], writes=["qmT"])
                    proj_tile(wt, wres, 2, hT, ev_qm)
                if blk == 0:
                    dump("qT", qT[:], "qT", [128, 8, BLK], BF16)
                    dump("kTd", kTd[:], "kTd", [128, 2, 640], BF16)
                    dump("v_tok", v_tok[:], "v_tok", [128, 5, 128], BF16)

                for qb in range(4):
                    qs = slice(qb * 128, (qb + 1) * 128)
                    msk = mask_first if (blk % 4 == 0 and qb == 0) else mask_swa
                    mres = "mask_first" if (blk % 4 == 0 and qb == 0) else "mask_swa"
                    for g in range(2):
                        for jp in range(2):
                            for p in range(2):
                                pb, pres = ps_next()
                                for c in range(2):
                                    j = 2 * jp + c
                                    P.op("pe", lambda e: e.matmul(pb[:, c * 256:(c + 1) * 256], qT[p * 64:(p + 1) * 64, 4 * g + j, qs],
                                                                  kTd[p * 64:(p + 1) * 64, g, qb * 128:qb * 128 + 256], start=True, stop=True),
                                         reads=["qT", "kTd"], writes=[pres])
                                s0 = jp * 4 + p * 2
                                P.op("dve", lambda e: e.tensor_tensor(out=Sm[:, s0:s0 + 2, :],
                                                                      in0=pb[:].rearrange("p (a b) -> p a b", b=256),
                                                                      in1=msk[:].unsqueeze(1).broadcast_to([128, 2, 256]), op=ALU.add),
                                     reads=[pres, mres], writes=["Sm"])

                        def pv(pso, psores):
                            for sl in range(8):
                                jp, p, c = sl // 4, (sl // 2) % 2, sl % 2
                                i = 2 * (2 * jp + c) + p
                                for kb in range(2):
                                    P.op("pe", lambda e: e.matmul(
                                        pso[(i % 2) * 64:(i % 2) * 64 + 64, (i // 2) * 128:(i // 2) * 128 + 128],
                                        v_tok[:, qb + kb, g * 64:(g + 1) * 64], PT[:, 2 * sl + kb, :], start=(kb == 0), stop=(kb == 1)),
                                        reads=["v_tok", "PT"], writes=[psores])

                        def oev(pso, psores):
                            P.op("act", lambda e: e.activation(out=aoT[:, 4 * g:4 * g + 4, qs], in_=pso[:].rearrange("p (a b) -> p a b", b=128),
                                                               func=AF.Copy),
                                 reads=[psores], writes=["aoT"])
                        softmax_pv(8, [(Sm[:, 0:8, :], "Sm", 0)], sinks[:, 8 * g:8 * g + 8], pv, oev)
                P.op("dve", lambda e: e.tensor_copy(out=kTd[:, :, 0:128], in_=kTd[:, :, 512:640]), reads=["kTd"], writes=["kTd"])
                P.op("dve", lambda e: e.tensor_copy(out=v_tok[:, 0, :], in_=v_tok[:, 4, :]), reads=["v_tok"], writes=["v_tok"])

                for tt in range(4):
                    ts_ = slice(tt * 128, (tt + 1) * 128)
                    pbs = []
                    for a in range(2):
                        pb, pres = ps_next()
                        pbs.append((pb, pres))
                        for p in range(2):
                            P.op("pe", lambda e: e.matmul(pb[:, p * 256:(p + 1) * 256], qmT[:, 2 * a + p, ts_], mem_kT[:, 2 * a + p, :],
                                                          start=True, stop=True),
                                 reads=["qmT", "mem_kT"], writes=[pres])

                    def pvm(pso, psores):
                        for i in range(4):
                            for kb in range(2):
                                P.op("pe", lambda e: e.matmul(pso[:, i * 128:(i + 1) * 128], mem_v[:, kb, i * 128:(i + 1) * 128],
                                                              PT[:, 2 * i + kb, :], start=(kb == 0), stop=(kb == 1)),
                                     reads=["mem_v", "PT"], writes=[psores])

                    def oevm(pso, psores):
                        P.op("act", lambda e: e.activation(out=moT[:, 0:4, ts_], in_=pso[:].rearrange("p (a b) -> p a b", b=128), func=AF.Copy),
                             reads=[psores], writes=["moT"])
                    softmax_pv(4, [(pbs[x][0][:].rearrange("p (a b) -> p a b", b=256), pbs[x][1], 2 * x) for x in range(2)], None, pvm, oevm)
                if blk == 0:
                    dump("aoT", aoT[:], "aoT", [128, 8, BLK], BF16)
                    dump("moT", moT[:], "moT", [128, 4, BLK], BF16)

                for jj in range(8):
                    wgt = [WS.next(("win", l, NT_IN + jj * 3 + b), held=b) for b in range(3)]
                    wbt, wbres = WS.next(("wbr", l, jj), held=3)
                    wb3 = wbt.rearrange("p (k c) -> p k c", c=256)
                    srcs = [(aoT, "aoT", 0, 8), (soT, "soT", 8, 4), (moT, "moT", 12, 4)]
                    for ct in range(2):
                        cs = slice(ct * 128, (ct + 1) * 128)
                        gps, bps = [], []
                        for b in range(3):
                            pb, pres = ps_next()
                            gps.append((pb, pres))
                            w3 = wgt[b][0].rearrange("p (k c) -> p k c", c=256)
                            for kc in range(16):
                                P.op("pe", lambda e: e.matmul(pb[:], w3[:, kc, cs], hT[:, kc, :], start=(kc == 0), stop=(kc == 15)),
                                     reads=[wgt[b][1], "hT"], writes=[pres])
                            pb2, pres2 = ps_next()
                            bps.append((pb2, pres2))
                            src, sres, k0, nk = srcs[b]
                            for kk in range(nk):
                                P.op("pe", lambda e: e.matmul(pb2[:], wb3[:, k0 + kk, cs], src[:, kk, :], start=(kk == 0), stop=(kk == nk - 1)),
                                     reads=[wbres, sres], writes=[pres2])
                        for b in range(3):
                            P.op("act", lambda e: e.activation(out=sg[b][:], in_=gps[b][0][:], func=AF.Sigmoid), reads=[gps[b][1]], writes=[f"sg{b}"])
                            P.op("dve", lambda e: e.tensor_tensor(out=mt[b][:], in0=bps[b][0][:], in1=sg[b][:], op=ALU.mult),
                                 reads=[bps[b][1], f"sg{b}"], writes=[f"mt{b}"])
                        P.op("pool", lambda e: e.tensor_tensor(out=mt[0][:], in0=mt[0][:], in1=mt[1][:], op=ALU.add), reads=["mt0", "mt1"], writes=["mt0"])
                        P.op("pool", lambda e: e.tensor_tensor(out=mergedT[:, 2 * jj + ct, :], in0=mt[0][:], in1=mt[2][:], op=ALU.add),
                             reads=["mt0", "mt2"], writes=["mergedT"])
                if blk == 0:
                    dump("mergedT", mergedT[:], "mergedT", [128, 16, BLK], BF16)

                for tt in range(4):
                    ti = blk * 4 + tt
                    P.dma("sp", f"ldr{tt}", lambda e: e.dma_start(out=rb[tt], in_=h_tok[ti * 128:(ti + 1) * 128, :]),
                          reads=[("htok", ti)], writes=rbres[tt])
                for c in range(8):
                    wt, wres = WS.next(("wout", l, c))
                    w3 = wt.rearrange("p (k c) -> p k c", c=256)
                    for tt in range(4):
                        pb, pres = ps_next()
                        for kc in range(16):
                            P.op("pe", lambda e: e.matmul(pb[:, 0:256], mergedT[:, kc, tt * 128:(tt + 1) * 128], w3[:, kc, :],
                                                          start=(kc == 0), stop=(kc == 15)),
                                 reads=[wres, "mergedT"], writes=[pres])
                        P.op("dve", lambda e: e.scalar_tensor_tensor(out=rb[tt][:, c * 256:(c + 1) * 256], in0=rb[tt][:, c * 256:(c + 1) * 256],
                                                                     scalar=ALPHA, in1=pb[:, 0:256], op0=ALU.mult, op1=ALU.add),
                             reads=[pres] + rbres[tt], writes=rbres[tt])
                for tt in range(4):
                    ti = blk * 4 + tt
                    s = tt % 2
                    x = rb[tt]
                    xr = rbres[tt]
                    st, mv, ve, rs = lsm[tt]
                    tag = f"l1{tt}"
                    for c in range(4):
                        P.op("dve", lambda e: e.bn_stats(out=st[:, c, :], in_=x[:, c * 512:(c + 1) * 512]), reads=xr, writes=[f"{tag}st{c}"])
                    P.op("dve", lambda e: e.bn_aggr(out=mv[:], in_=st[:].rearrange("p a b -> p (a b)")),
                         reads=[f"{tag}st{c}" for c in range(4)], writes=[tag + "mv"])
                    P.op("dve", lambda e: e.tensor_scalar(out=ve[:], in0=mv[:, 1:2], scalar1=EPS, scalar2=None, op0=ALU.add),
                         reads=[tag + "mv"], writes=[tag + "ve"])
                    P.op("pool", lambda e: e.tensor_tensor(out=rs[:], in0=ve[:], in1=neghalf[:], op=ALU.pow),
                         reads=[tag + "ve", "neghalf"], writes=[tag + "rs"])
                    P.op("dve", lambda e: e.tensor_scalar(out=x, in0=x, scalar1=mv[:, 0:1], scalar2=rs[:, 0:1], op0=ALU.subtract, op1=ALU.mult),
                         reads=xr + [tag + "mv", tag + "rs"], writes=xr)
                    P.op("dve", lambda e: e.tensor_tensor(out=x, in0=x, in1=lng[:], op=ALU.mult), reads=xr + ["lng"], writes=xr)
                    P.op("dve", lambda e: e.tensor_tensor(out=x, in0=x, in1=lnb[:], op=ALU.add), reads=xr + ["lnb"], writes=xr)
                    P.dma("sp", f"sth{tt}", lambda e: e.dma_start(out=h_tok[ti * 128:(ti + 1) * 128, :], in_=x), reads=xr, writes=[("htok", ti)])
                    P.op("act", lambda e: e.activation(out=tokb[s][:], in_=x, func=AF.Copy), reads=xr, writes=[f"tokb{s}"])
                    for a in range(4):
                        pb, pres = ps_next()
                        for i in range(4):
                            kc = a * 4 + i
                            P.op("pe", lambda e: e.transpose(out=pb[:, i * 128:(i + 1) * 128], in_=x[:, kc * 128:(kc + 1) * 128], identity=identf[:]),
                                 reads=xr + ["identf"], writes=[pres])
                        P.op("dve", lambda e: e.tensor_copy(out=h1T[:, a * 4:(a + 1) * 4, :], in_=pb[:].rearrange("p (a b) -> p a b", b=128)),
                             reads=[pres], writes=["qT"])
                    pb, pres = ps_next()
                    for kc in range(16):
                        P.op("pe", lambda e: e.matmul(pb[:, 0:NE], h1T[:, kc, :], wr[:, kc, :], start=(kc == 0), stop=(kc == 15)),
                             reads=["qT", "wr"], writes=[pres])
                    P.op("dve", lambda e: e.tensor_tensor(out=lg[:], in0=pb[:, 0:NE], in1=brt[:], op=ALU.add), reads=[pres, "brt"], writes=["lg"])
                    if ti == 0:
                        dump("lg", lg[:], "lg", [128, NE])
                    P.op("dve", lambda e: e.max(out=top8[:], in_=lg[:]), reads=["lg"], writes=["top8"])
                    P.op("dve", lambda e: e.tensor_scalar(out=mb[:], in0=lg[:], scalar1=top8[:, 3:4], scalar2=None, op0=ALU.is_ge),
                         reads=["lg", "top8"], writes=["mb"])
                    pa, pares = ps_next()
                    P.op("pe", lambda e: e.matmul(pa[:, 0:NE], ltris[:], mb[:], start=True, stop=True), reads=["ltris", "mb"], writes=[pares])
                    P.op("pe", lambda e: e.matmul(pa[:, NE:2 * NE], onesb[:], mb[:], start=True, stop=True), reads=["onesb", "mb"], writes=[pares])
                    P.op("dve", lambda e: e.tensor_tensor(out=pos[:], in0=pa[:, 0:NE], in1=cum[:], op=ALU.add), reads=[pares, "cum"], writes=["pos"])
                    P.op("dve", lambda e: e.tensor_tensor(out=cum[:], in0=pa[:, NE:2 * NE], in1=cum[:], op=ALU.add), reads=[pares, "cum"], writes=["cum"])
                    P.op("dve", lambda e: e.tensor_scalar(out=ov[:], in0=pos[:], scalar1=float(CAP), scalar2=1.0e7, op0=ALU.is_ge, op1=ALU.mult),
                         reads=["pos"], writes=["ov"])
                    P.op("dve", lambda e: e.tensor_tensor(out=pos[:], in0=pos[:], in1=ebase[:], op=ALU.add), reads=["pos", "ebase"], writes=["pos"])
                    P.op("dve", lambda e: e.tensor_tensor(out=pos[:], in0=pos[:], in1=ov[:], op=ALU.add), reads=["pos", "ov"], writes=["pos"])
                    P.op("dve", lambda e: e.tensor_scalar(out=nm1[:], in0=top8[:, 0:1], scalar1=-1.0, scalar2=None, op0=ALU.mult),
                         reads=["top8"], writes=["nm1"])
                    P.op("act", lambda e: e.activation(out=eg[:], in_=top8[:, 0:4], func=AF.Exp, bias=nm1[:, 0:1], scale=1.0, accum_out=gs[:]),
                         reads=["top8", "nm1"], writes=["eg", "gs"])
                    P.op("dve", lambda e: e.reciprocal(out=gs[:], in_=gs[:]), reads=["gs"], writes=["gs"])
                    P.op("dve", lambda e: e.tensor_scalar(out=gates_all[:, ti, :], in0=eg[:], scalar1=gs[:, 0:1], scalar2=None, op0=ALU.mult),
                         reads=["eg", "gs"], writes=["gates_all"])
                    for k in range(4):
                        P.op("dve", lambda e: e.tensor_scalar(out=mk[:], in0=lg[:], scalar1=top8[:, k:k + 1], scalar2=None, op0=ALU.is_equal),
                             reads=["lg", "top8"], writes=["mk"])
                        P.op("dve", lambda e: e.tensor_tensor(out=junk[:], in0=mk[:], in1=pos[:], op=ALU.mult),
                             reads=["mk", "pos"], writes=["junk"])
                        P.op("dve", lambda e: e.tensor_reduce(out=destf[:, k:k + 1], in_=junk[:], axis=AX.X, op=ALU.add),
                             reads=["junk"], writes=["destf"])
                        if k == 0:
                            P.op("dve", lambda e: e.tensor_scalar(out=gmat_all[:, ti, :], in0=mk[:], scalar1=gates_all[:, ti, 0:1], scalar2=None,
                                                                  op0=ALU.mult),
                                 reads=["mk", "gates_all"], writes=["gmat_all"])
                        else:
                            P.op("dve", lambda e: e.scalar_tensor_tensor(out=gmat_all[:, ti, :], in0=mk[:], scalar=gates_all[:, ti, k:k + 1],
                                                                         in1=gmat_all[:, ti, :], op0=ALU.mult, op1=ALU.add),
                                 reads=["mk", "gates_all", "gmat_all"], writes=["gmat_all"])
                    P.op("act", lambda e: e.activation(out=dest_all[:, ti, :], in_=destf[:], func=AF.Copy), reads=["destf"], writes=["dest_all"])
                    for k in range(4):
                        P.dma("pool", f"sc{s}", lambda e: e.indirect_dma_start(
                            out=xg[:, :], out_offset=bass.IndirectOffsetOnAxis(ap=dest_all[:, ti, k:k + 1], axis=0),
                            in_=tokb[s][:, :], in_offset=None, bounds_check=bc_reg, oob_is_err=False),
                            reads=[f"tokb{s}", "dest_all"], writes=[("xg", ti, k)])
            P.barrier()

    STILES = [(i * 128, min(128, CAP - i * 128)) for i in range((CAP + 127) // 128)]
    NH = CAP // 2

    def phase_experts(l):
        with ExitStack() as ph:
            NST = len(STILES)
            xrow = [[alloc(ph, f"xrow{a}{i}", [128, D], BF16) for i in range(NST)] for a in range(2)]
            xeTs = [alloc(ph, f"xeT{i}", [128, 16, CAP], BF16) for i in range(2)]
            actT = alloc(ph, "actT", [128, 8, CAP], BF16)
            yst = alloc(ph, "yst", [128, len(STILES), D], BF16)
            bup = alloc(ph, "bup", [128, NE, 16], F32)
            tg = [alloc(ph, f"tg{i}", [128, NH], F32) for i in range(2)]
            tsg = [alloc(ph, f"tsg{i}", [128, NH], F32) for i in range(2)]
            tl = [alloc(ph, f"tl{i}", [128, NH], F32) for i in range(2)]
            P.dma("sp", "c10", lambda e: e.dma_start(out=bup[:].rearrange("p a b -> p (a b)"), in_=din["bup"][l]), writes=["bup"])
            par = 0

            def load_rows(ex):
                a = ex % 2
                for si, (r0, rows) in enumerate(STILES):
                    P.dma("sp", f"ldxg{a}{si}", lambda e: e.dma_start(out=xrow[a][si][0:rows, :],
                                                                       in_=xg[ex * CAP + r0:ex * CAP + r0 + rows, :]),
                          writes=[f"xrow{a}{si}"])
            load_rows(0)
            for ex in range(NE):
                base = ex * CAP
                xeT = xeTs[ex % 2]
                xres = f"xeT{ex % 2}"
                if ex + 1 < NE:
                    load_rows(ex + 1)
                for si, (r0, rows) in enumerate(STILES):
                    xr_ = xrow[ex % 2][si]
                    xrr = f"xrow{ex % 2}{si}"
                    for a in range(2):
                        pb, pres = ps_next()
                        pbb = pb[:].bitcast(BF16)
                        for i in range(8):
                            kc = a * 8 + i
                            P.op("pe", lambda e: e.transpose(out=pbb[:, i * 128:i * 128 + rows], in_=xr_[0:rows, kc * 128:(kc + 1) * 128],
                                                             identity=identb[0:rows, 0:rows]),
                                 reads=[xrr, "identb"], writes=[pres])
                        P.op("dve", lambda e: e.tensor_copy(out=xeT[:, a * 8:(a + 1) * 8, r0:r0 + rows],
                                                            in_=pbb.rearrange("p (a b) -> p a b", b=128)[:, :, 0:rows]),
                             reads=[pres], writes=[xres])
                for j in range(8):
                    wt, wres = WS.next(("wup", l, ex, j))
                    w3 = wt.rearrange("p (k c) -> p k c", c=256)
                    for nh in range(2):
                        ns = slice(nh * NH, (nh + 1) * NH)
                        pg, pgres = ps_next()
                        pl, plres = ps_next()
                        for kc in range(16):
                            P.op("pe", lambda e: e.matmul(pg[:, 0:NH], w3[:, kc, 0:128], xeT[:, kc, ns], start=(kc == 0), stop=(kc == 15)),
                                 reads=[wres, xres], writes=[pgres])
                        for kc in range(16):
                            P.op("pe", lambda e: e.matmul(pl[:, 0:NH], w3[:, kc, 128:256], xeT[:, kc, ns], start=(kc == 0), stop=(kc == 15)),
                                 reads=[wres, xres], writes=[plres])
                        q = par
                        par ^= 1
                        P.op("dve", lambda e: e.tensor_scalar(out=tg[q][:], in0=pg[:, 0:NH], scalar1=bup[:, ex, j:j + 1], scalar2=7.0,
                                                              op0=ALU.add, op1=ALU.min),
                             reads=[pgres, "bup"], writes=[f"tg{q}"])
                        P.op("act", lambda e: e.activation(out=tsg[q][:], in_=tg[q][:], func=AF.Sigmoid, scale=1.702),
                             reads=[f"tg{q}"], writes=[f"tsg{q}"])
                        P.op("dve", lambda e: e.tensor_scalar(out=tl[q][:], in0=pl[:, 0:NH], scalar1=bup[:, ex, 8 + j:9 + j], scalar2=-7.0,
                                                              op0=ALU.add, op1=ALU.max),
                             reads=[plres, "bup"], writes=[f"tl{q}"])
                        P.op("dve", lambda e: e.tensor_scalar(out=tl[q][:], in0=tl[q][:], scalar1=7.0, scalar2=1.0, op0=ALU.min, op1=ALU.add),
                             reads=[f"tl{q}"], writes=[f"tl{q}"])
                        P.op("dve", lambda e: e.tensor_tensor(out=tg[q][:], in0=tg[q][:], in1=tsg[q][:], op=ALU.mult),
                             reads=[f"tg{q}", f"tsg{q}"], writes=[f"tg{q}"])
                        P.op("dve", lambda e: e.tensor_tensor(out=actT[:, j, ns], in0=tg[q][:], in1=tl[q][:], op=ALU.mult),
                             reads=[f"tg{q}", f"tl{q}"], writes=["actT"])
                for c in range(8):
                    wt, wres = WS.next(("wdn", l, ex, c))
                    w3 = wt.rearrange("p (k c) -> p k c", c=256)
                    for si, (r0, rows) in enumerate(STILES):
                        pb, pres = ps_next()
                        for k in range(8):
                            P.op("pe", lambda e: e.matmul(pb[0:rows, 0:256], actT[:, k, r0:r0 + rows], w3[:, k, :], start=(k == 0), stop=(k == 7)),
                                 reads=[wres, "actT"], writes=[pres])
                        P.op("act", lambda e: e.activation(out=yst[0:rows, si, c * 256:(c + 1) * 256], in_=pb[0:rows, 0:256], func=AF.Copy),
                             reads=[pres], writes=[("yst", si)])
                for si, (r0, rows) in enumerate(STILES):
                    P.dma("sp", f"sty{si}", lambda e: e.dma_start(out=ypad[base + r0:base + r0 + rows, :], in_=yst[0:rows, si, :]),
                          reads=[("yst", si)], writes=[("ypad", ex, si)])
            P.barrier()

    def phase_combine(l, dst):
        with ExitStack() as ph:
            yk = [[alloc(ph, f"yk{s}{k}", [128, D], BF16) for k in range(4)] for s in range(2)]
            hb = [alloc(ph, f"hb{i}", [128, D], F32) for i in range(2)]
            lng = alloc(ph, "lng2", [128, D], F32)
            lnb = alloc(ph, "lnb2", [128, D], F32)
            bdn = alloc(ph, "bdn", [NE, D], F32)
            gT = [alloc(ph, f"gT{i}", [NE, 128], F32) for i in range(2)]
            lsm = [ln_small(ph, f"l2s{i}") for i in range(2)]
            P.dma("sp", "c11", lambda e: e.dma_start(out=lng[:], in_=din["lnp"][l, 2]), writes=["lng2"])
            P.dma("sp", "c12", lambda e: e.dma_start(out=lnb[:], in_=din["lnp"][l, 3]), writes=["lnb2"])
            P.dma("sp", "c13", lambda e: e.dma_start(out=bdn[:], in_=din["bdn"][l]), writes=["bdn"])
            for ti in range(T // 128):
                s = ti % 2
                for k in range(4):
                    P.dma("pool", f"ga{s}{k}", lambda e: e.indirect_dma_start(
                        out=yk[s][k][:, :], out_offset=None, in_=ypad[:, :],
                        in_offset=bass.IndirectOffsetOnAxis(ap=dest_all[:, ti, k:k + 1], axis=0),
                        bounds_check=bc_reg, oob_is_err=False),
                        reads=["dest_all"], writes=[f"yk{s}{k}"])
                P.dma("sp", f"ldh2{s}", lambda e: e.dma_start(out=hb[s][:], in_=h_tok[ti * 128:(ti + 1) * 128, :]),
                      reads=[("htok", ti)], writes=[f"hb{s}"])
                pt, ptres = ps_next()
                P.op("pe", lambda e: e.transpose(out=pt[0:NE, 0:128], in_=gmat_all[:, ti, :], identity=identf[:]),
                     reads=["gmat_all", "identf"], writes=[ptres])
                P.op("dve", lambda e: e.tensor_copy(out=gT[s][:], in_=pt[0:NE, 0:128]), reads=[ptres], writes=[f"gT{s}"])
                for cgi in range(4):
                    cs = slice(cgi * 512, (cgi + 1) * 512)
                    pb, pres = ps_next()
                    P.op("pe", lambda e: e.matmul(pb[:], gT[s][:], bdn[:, cs], start=True, stop=True), reads=[f"gT{s}", "bdn"], writes=[pres])
                    P.op("dve", lambda e: e.scalar_tensor_tensor(out=hb[s][:, cs], in0=hb[s][:, cs], scalar=ALPHA, in1=pb[:], op0=ALU.mult, op1=ALU.add),
                         reads=[pres, f"hb{s}"], writes=[f"hb{s}"])
                for k in range(4):
                    P.op("dve", lambda e: e.scalar_tensor_tensor(out=hb[s][:], in0=yk[s][k][:], scalar=gates_all[:, ti, k:k + 1], in1=hb[s][:],
                                                                 op0=ALU.mult, op1=ALU.add),
                         reads=[f"yk{s}{k}", "gates_all", f"hb{s}"], writes=[f"hb{s}"])
                layer_norm(hb[s][:], f"hb{s}", lng[:], "lng2", lnb[:], "lnb2", lsm[s], f"l2{s}")
                P.dma("sp", f"sto{s}", lambda e: e.dma_start(out=dst[ti * 128:(ti + 1) * 128, :], in_=hb[s][:]),
                      reads=[f"hb{s}"], writes=[("htok", ti)])
            P.barrier()

    phase_ln0()
    for l in range(nlayers):
        if stop == "ln0":
            break
        phase_ssm(l)
        if stop == "ssm":
            break
        phase_mix(l)
        if stop == "mix":
            break
        phase_experts(l)
        if stop == "exp":
            break
        phase_combine(l, out if l == nlayers - 1 else h_tok)
    P.finish()
    return nc, dbg


def _kc(W):
    K, C = W.shape
    return np.ascontiguousarray(W.reshape(K // 128, 128, C).transpose(1, 0, 2)).reshape(128, (K // 128) * C)


def _rep(v, n=128):
    return np.ascontiguousarray(np.broadcast_to(np.asarray(v, np.float32)[None, :], (n, len(v))))


def prep_shared(inp, moe=True):
    f = np.float32
    sh = {}
    sh["lnin"] = np.stack([_rep(inp["ln_in_g"]), _rep(inp["ln_in_b"])])
    sh["lnp"] = np.stack([np.stack([_rep(inp[k][l]) for k in ("ln1_g", "ln1_b", "ln2_g", "ln2_b")]) for l in range(L)])
    win = np.zeros((L, NT_IN + 24, 128, 4096), f)
    for l in range(L):
        W = inp["w_in"][l]
        k0 = W[:, 1024:1088]
        k1 = W[:, 1088:1152]
        tiles = [W[:, i * 256:(i + 1) * 256] for i in range(4)]
        tiles.append(np.concatenate([k0, k0, k1, k1], axis=1))
        tiles.append(np.concatenate([W[:, 1152:1280], np.zeros((D, 128), f)], axis=1))
        tiles += [W[:, 1280 + i * 256:1280 + (i + 1) * 256] for i in range(2)]
        tiles += [W[:, 1792 + i * 256:1792 + (i + 1) * 256] for i in range(2)]
        for jj in range(8):
            for b in range(3):
                c0 = 2304 + b * 2048 + jj * 256
                tiles.append(W[:, c0:c0 + 256])
        for i, t in enumerate(tiles):
            win[l, i] = _kc(t)
    sh["win"] = win
    sh["wbr"] = np.stack([np.stack([_kc(inp["w_branch"][l][:, j * 256:(j + 1) * 256]) for j in range(8)]) for l in range(L)])
    sh["wout"] = np.stack([np.stack([_kc(inp["w_out"][l][:, j * 256:(j + 1) * 256]) for j in range(8)]) for l in range(L)])
    sh["wglu"] = np.stack([_kc(inp["w_glu"][l]) for l in range(L)])
    sh["wmkv"] = np.stack([np.stack([_kc(inp["w_mem_kv"][l][:, j * 256:(j + 1) * 256]) for j in range(4)]) for l in range(L)])
    sh["wr"] = np.stack([_kc(inp["w_router"][l]) for l in range(L)])
    sh["br"] = np.stack([_rep(inp["b_router"][l]) for l in range(L)])
    wup = np.empty((L, NE, 8, 128, 4096), f) if moe else None
    wdn = np.empty((L, NE, 8, 128, 2048), f) if moe else None
    bup = np.empty((L, 128, NE, 16), f)
    for l in range(L):
        for e in range(NE):
            bu = inp["b_up"][l, e]
            bup[l, :, e, 0:8] = bu[0::2].reshape(8, 128).T
            bup[l, :, e, 8:16] = bu[1::2].reshape(8, 128).T
            if not moe:
                continue
            Wu = inp["w_up"][l, e]
            g, li = Wu[:, 0::2], Wu[:, 1::2]
            for j in range(8):
                wup[l, e, j] = _kc(np.concatenate([g[:, j * 128:(j + 1) * 128], li[:, j * 128:(j + 1) * 128]], axis=1))
            Wd = inp["w_down"][l, e]
            for c in range(8):
                wdn[l, e, c] = _kc(Wd[:, c * 256:(c + 1) * 256])
    if moe:
        sh["wup"], sh["wdn"] = wup, wdn
    sh["bup"] = bup.reshape(L, 128, NE * 16)
    sh["bdn"] = np.ascontiguousarray(inp["b_down"]).astype(f)
    slot2head = [8 * g + 2 * (2 * (s // 4) + s % 2) + (s // 2) % 2 for g in range(2) for s in range(8)]
    sh["sinks"] = np.stack([_rep(np.asarray(inp["attn_sinks"][l])[slot2head]) for l in range(L)])
    sh["ident"] = np.eye(128, dtype=f)
    s_, t_ = np.meshgrid(np.arange(128), np.arange(128), indexing="ij")
    sh["ltri"] = (s_ <= t_).astype(f)
    sh["ltris"] = (s_ < t_).astype(f)
    sh["ones"] = np.ones((128, 128), f)
    q_, k_ = np.meshgrid(np.arange(128), np.arange(256), indexing="ij")
    valid = (k_ > q_) & (k_ <= q_ + 128)
    sh["mask_swa"] = np.where(valid, 0.0, -30000.0).astype(f)
    sh["mask_first"] = np.where(valid & (k_ >= 128), 0.0, -30000.0).astype(f)
    sh["ebase"] = _rep(np.arange(NE, dtype=f) * CAP)
    sh["sp1"] = (np.arange(128, dtype=f) + 1.0).reshape(128, 1)
    sh["tp1"] = _rep(np.arange(128, dtype=f) + 1.0)
    pp = np.arange(128)[:, None, None] // 16
    sh["bmask"] = np.broadcast_to((pp == np.arange(8)[None, :, None]), (128, 8, 64)).astype(f).reshape(128, 512)
    s_tok = np.empty((L, 3, 128, 2048), f)
    s_cm = np.empty((L, 3, 128, 16), f)
    s_b = np.empty((L, 5, 128, 256), f)
    s_c = np.zeros((L, 2, 128, 16, 4, 16), f)
    s_d = np.empty((L, 128, 4), f)
    for l in range(L):
        lre, lim = inp["ssm_lambda_re"][l], inp["ssm_lambda_im"][l]
        ldt = np.broadcast_to(inp["ssm_log_dt"][l][:, None], (32, 64))
        for i, a in enumerate((lre, lim, ldt)):
            flat = np.ascontiguousarray(a).reshape(2048)
            s_tok[l, i] = _rep(flat)
            s_cm[l, i] = flat.reshape(16, 128).T
            s_b[l, i] = np.broadcast_to(np.ascontiguousarray(a).reshape(4, 8, 1, 64), (4, 8, 16, 64)).transpose(1, 2, 0, 3).reshape(128, 256)
        for i, bb in enumerate((inp["ssm_b_re"][l], inp["ssm_b_im"][l])):
            s_b[l, 3 + i] = bb.reshape(4, 8, 64, 16).transpose(1, 3, 0, 2).reshape(128, 256)
        for i, cc in enumerate((inp["ssm_c_re"][l], inp["ssm_c_im"][l])):
            c4 = cc.reshape(16, 2, 16, 64)
            for g2 in range(2):
                for par in range(2):
                    s_c[l, i, g2 * 64:(g2 + 1) * 64, par::2, 2 * par + g2, :] = c4[par::2, g2].transpose(2, 0, 1)
        s_d[l] = inp["ssm_d"][l].reshape(4, 128).T
    sh["s_tok"], sh["s_cm"], sh["s_b"], sh["s_d"] = s_tok, s_cm, s_b, s_d
    sh["s_c"] = s_c.reshape(L, 2, 128, 1024)
    return {k: np.ascontiguousarray(v, dtype=f) for k, v in sh.items()}


def prep_core(inp, c):
    x = np.ascontiguousarray(inp["x"][NSEQ * c:NSEQ * (c + 1)]).reshape(T, D).astype(np.float32)
    mem = np.ascontiguousarray(inp["mem"][NSEQ * c:NSEQ * (c + 1)]).reshape(NSEQ * 256, D).astype(np.float32)
    return {"x": x, "mem": mem}


_CACHE = {}


def kernel(**inputs):
    inp = {k: np.asarray(v) for k, v in inputs.items()}
    sh = prep_shared(inp)
    if "nc" not in _CACHE:
        _CACHE["nc"] = build()[0]
    nc = _CACHE["nc"]
    in_maps = [{**sh, **prep_core(inp, c)} for c in range(NCORES)]
    res = run_bass_kernel_spmd(nc, in_maps, core_ids=list(range(NCORES)))
    outs = [np.asarray(r["out"]).reshape(NSEQ, S, D) for r in res.results]
    return np.concatenate(outs, axis=0).astype(np.float32)
```

```python
import numpy as np
from contextlib import ExitStack
import concourse.bass as bass
import concourse.mybir as mybir
from concourse.bass_utils import run_bass_kernel_spmd

F32 = mybir.dt.float32
BF16 = mybir.dt.bfloat16
I32 = mybir.dt.int32
ALU = mybir.AluOpType
AF = mybir.ActivationFunctionType
AX = mybir.AxisListType

NCORES = 8
D = 2048
S = 2048
NSEQ = 2
T = NSEQ * S
BLK = 512
NBLK = T // BLK
L = 2
NE = 32
CAP = 768
NSLOT = NE * CAP
EPS = 1e-5
ALPHA = (2.0 * L) ** 0.25
NT_IN = 10
ENG = ("pe", "act", "dve", "pool", "sp")
TWO_PI = 6.283185307179586
MAGIC = 12582912.0


class Late:
    def __init__(self, f):
        self.f = f


class _Rec:
    def __init__(self):
        self.__dict__["call"] = None

    def __getattr__(self, name):
        def f(*a, **k):
            self.__dict__["call"] = (name, a, k)
            return self
        return f


def _record(fn):
    r = _Rec()
    fn(r)
    return r.__dict__["call"]


class Prog:
    def __init__(self, nc, es):
        self.nc = nc
        self.es = es
        self.ops = {e: [] for e in ENG}
        self.sem = {}
        self.cnt = {}
        self.waited = {e: {} for e in ENG}
        self.res = {}
        self.nins = 0
        for e in ("pe", "act", "dve", "pool"):
            self.newsem(e)

    def newsem(self, key):
        self.sem[key] = self.es.enter_context(self.nc.semaphore("s_" + key))
        self.cnt[key] = 0

    def _deps(self, reads, writes):
        d = {}
        for r in reads:
            st = self.res.get(r)
            if st and st["w"]:
                k, v = st["w"]
                if v > d.get(k, 0):
                    d[k] = v
        for w in writes:
            st = self.res.get(w)
            if st:
                if st["w"]:
                    k, v = st["w"]
                    if v > d.get(k, 0):
                        d[k] = v
                for k, v in st["r"].items():
                    if v > d.get(k, 0):
                        d[k] = v
        return d

    def _emit_waits(self, eng, deps):
        for k, v in deps.items():
            if eng == "pe" and k == "pe":
                continue
            if self.waited[eng].get(k, 0) < v:
                self.waited[eng][k] = v
                s = self.sem[k]
                self.ops[eng].append(lambda e, s=s, v=v: e.wait_ge(s, v))
                self.nins += 1

    def _commit(self, reads, writes, dep):
        k, v = dep
        for r in reads:
            st = self.res.setdefault(r, {"w": None, "r": {}})
            if v > st["r"].get(k, 0):
                st["r"][k] = v
        for w in writes:
            self.res[w] = {"w": dep, "r": {}}

    def op(self, eng, fn, reads=(), writes=()):
        self._emit_waits(eng, self._deps(reads, writes))
        self.cnt[eng] += 1
        n = self.cnt[eng]
        s = self.sem[eng]
        name, a, k = _record(fn)
        self.ops[eng].append(lambda e, name=name, a=a, k=k, s=s: getattr(e, name)(*a, **k).then_inc(s, 1))
        self.nins += 1
        self._commit(reads, writes, (eng, n))

    def dma(self, q, key, fn, reads=(), writes=()):
        if key.startswith("c8_") or key.startswith("c9_"):
            self.nuniq = getattr(self, "nuniq", 0) + 1
            key = f"{key}u{self.nuniq}"
        if key not in self.sem:
            self.newsem(key)
        self._emit_waits(q, self._deps(reads, writes))
        self.cnt[key] += 16
        n = self.cnt[key]
        s = self.sem[key]
        name, a, k = _record(fn)

        def run(e, name=name, a=a, k=k, s=s):
            try:
                k2 = {kk: (v.f(e) if isinstance(v, Late) else v) for kk, v in k.items()}
                return getattr(e, name)(*a, **k2).then_inc(s, 16)
            except Exception:
                print("FAILED DMA", q, key, name, [(kk, getattr(v, "shape", v), getattr(v, "ap", None)) for kk, v in k.items()])
                raise
        self.ops[q].append(run)
        self.nins += 1
        self._commit(reads, writes, (key, n))

    def barrier(self):
        for eng in ENG:
            self._emit_waits(eng, dict(self.cnt))
        self.res = {}
        self.nuniq = 0

    def finish(self):
        for eng in ENG:
            self._emit_waits(eng, dict(self.cnt))
        nc = self.nc
        ops = self.ops
        with nc.Block() as block:
            @block.sync
            def _(e):
                for f in ops["sp"]:
                    f(e)

            @block.tensor
            def _(e):
                for f in ops["pe"]:
                    f(e)

            @block.vector
            def _(e):
                for f in ops["dve"]:
                    f(e)

            @block.scalar
            def _(e):
                for f in ops["act"]:
                    f(e)

            @block.gpsimd
            def _(e):
                for f in ops["pool"]:
                    f(e)


class WStream:
    def __init__(self, P, ring, nslots, plan):
        self.P = P
        self.ring = ring
        self.n = nslots
        self.plan = plan
        self.issued = 0
        self.pos = 0

    def _issue(self, i):
        key, src = self.plan[i]
        slot = i % self.n
        nel = src.shape[1]
        dst = self.ring[:, slot, 0:nel]
        if nel > 2048:
            assert nel % 2048 == 0
            src = src.rearrange("p (a b) -> p a b", b=2048)
            dst = dst.rearrange("p (a b) -> p a b", b=2048)
        self.P.dma("pool", f"w{slot}", lambda e, d=dst, s=src: e.dma_start(out=d, in_=s),
                   writes=[f"w{slot}"])

    def next(self, key, held=0):
        i = self.pos
        assert self.plan[i][0] == key, (self.plan[i][0], key)
        while self.issued < min(len(self.plan), i - held + self.n):
            self._issue(self.issued)
            self.issued += 1
        self.pos += 1
        slot = i % self.n
        return self.ring[:, slot, :], f"w{slot}"


def plan_all(nlayers, stop=None):
    pl = []
    for l in range(nlayers):
        for blk in range(NBLK):
            pl += [("win", l, 6), ("win", l, 7), ("wglu", l)]
        if stop == "ssm":
            break
        for blk in range(NBLK):
            if blk % 4 == 0:
                pl += [("wmkv", l, i) for i in range(4)]
            pl += [("win", l, i) for i in (0, 1, 2, 3, 4, 5, 8, 9)]
            for jj in range(8):
                pl += [("win", l, NT_IN + jj * 3 + b) for b in range(3)] + [("wbr", l, jj)]
            pl += [("wout", l, c) for c in range(8)]
        if stop == "mix":
            break
        for e in range(NE):
            pl += [("wup", l, e, j) for j in range(8)] + [("wdn", l, e, k) for k in range(8)]
    return pl


INPUT_SPECS = [
    ("x", (T, D)), ("mem", (NSEQ * 256, D)), ("lnin", (2, 128, D)), ("lnp", (L, 4, 128, D)),
    ("win", (L, NT_IN + 24, 128, 4096)), ("wbr", (L, 8, 128, 4096)), ("wout", (L, 8, 128, 4096)),
    ("wglu", (L, 128, 4096)), ("wmkv", (L, 4, 128, 4096)),
    ("wr", (L, 128, 16 * 32)), ("br", (L, 128, 32)),
    ("wup", (L, NE, 8, 128, 4096)), ("wdn", (L, NE, 8, 128, 2048)),
    ("bup", (L, 128, NE * 16)), ("bdn", (L, NE, D)),
    ("sinks", (L, 128, 16)),
    ("ident", (128, 128)), ("ltri", (128, 128)), ("ltris", (128, 128)), ("ones", (128, 128)),
    ("mask_swa", (128, 256)), ("mask_first", (128, 256)), ("ebase", (128, NE)),
    ("sp1", (128, 1)), ("tp1", (128, 128)), ("bmask", (128, 512)),
    ("s_tok", (L, 3, 128, 2048)), ("s_cm", (L, 3, 128, 16)), ("s_b", (L, 5, 128, 256)),
    ("s_c", (L, 2, 128, 16 * 64)), ("s_d", (L, 128, 4)),
]


def build(nlayers=L, debug=(), stop=None):
    nc = bass.Bass("TRN2", target_bir_lowering=False)
    es = ExitStack()
    P = Prog(nc, es)
    din = {}
    for name, shape in INPUT_SPECS:
        if stop in ("ln0", "ssm", "mix") and name in ("wup", "wdn"):
            continue
        din[name] = nc.dram_tensor(name, list(shape), F32, kind="ExternalInput").ap()
    out = nc.dram_tensor("out", [T, D], F32, kind="ExternalOutput").ap()
    skind = "ExternalOutput" if debug else "Internal"
    h_tok = nc.dram_tensor("h_tok", [T, D], F32, kind=skind).ap()
    xg = nc.dram_tensor("xg", [NSLOT, D], BF16, kind=skind).ap()
    ypad = nc.dram_tensor("ypad", [NSLOT, D], BF16, kind=skind).ap()
    ssm_scr = nc.dram_tensor("ssm_scr", [NBLK, 128, 4 * BLK], BF16, kind=skind).ap()
    dbg = {}

    def dbg_out(name, shape, dt=F32):
        dbg[name] = nc.dram_tensor("dbg_" + name, list(shape), dt, kind="ExternalOutput").ap()
        return dbg[name]

    uid = [0]

    def alloc(stack, name, shape, dt=F32):
        uid[0] += 1
        return stack.enter_context(nc.sbuf_tensor(f"sb{uid[0]}_{name}", list(shape), dt))

    NRING = 6
    ring = alloc(es, "ring", [128, NRING, 4096], BF16)
    psum = [es.enter_context(nc.psum_tensor(f"ps{i}", [128, 512], F32)) for i in range(8)]
    ps_ctr = [0]

    def ps_next():
        i = ps_ctr[0] % 8
        ps_ctr[0] += 1
        return psum[i], f"ps{i}"

    identf = alloc(es, "identf", [128, 128], F32)
    identb = alloc(es, "identb", [128, 128], BF16)
    ltri = alloc(es, "ltri", [128, 128], BF16)
    ltris = alloc(es, "ltris", [128, 128], BF16)
    onesb = alloc(es, "onesb", [128, 128], BF16)
    neghalf = alloc(es, "neghalf", [128, 1], F32)
    gates_all = alloc(es, "gates_all", [128, T // 128, 4], F32)
    dest_all = alloc(es, "dest_all", [128, T // 128, 4], I32)
    gmat_all = alloc(es, "gmat_all", [128, T // 128, NE], F32)

    P.dma("sp", "c0", lambda e: e.dma_start(out=identf[:], in_=din["ident"]), writes=["identf"])
    P.dma("pool", "c1", lambda e: e.dma_start(out=identb[:], in_=din["ident"]), writes=["identb"])
    P.dma("pool", "c2", lambda e: e.dma_start(out=ltri[:], in_=din["ltri"]), writes=["ltri"])
    P.dma("pool", "c3", lambda e: e.dma_start(out=ltris[:], in_=din["ltris"]), writes=["ltris"])
    P.dma("pool", "c4", lambda e: e.dma_start(out=onesb[:], in_=din["ones"]), writes=["onesb"])
    P.op("dve", lambda e: e.memset(neghalf[:], -0.5), writes=["neghalf"])

    reg_cache = {}

    def _bc(e):
        if "bc" not in reg_cache:
            reg_cache["bc"] = e.to_reg(NSLOT - 1)
        return reg_cache["bc"]
    bc_reg = Late(_bc)

    plan = plan_all(nlayers, stop)

    def wsrc(key):
        if key[0] == "wglu":
            return din["wglu"][key[1]]
        if key[0] in ("wup", "wdn"):
            return din[key[0]][key[1], key[2], key[3]]
        return din[key[0]][key[1], key[2]]

    WS = WStream(P, ring, NRING, [(k, wsrc(k)) for k in plan])

    def layer_norm(x, xres, g, gres, b, bres, sm, tag):
        st, mv, ve, rs = sm
        for c in range(4):
            P.op("dve", lambda e, c=c: e.bn_stats(out=st[:, c, :], in_=x[:, c * 512:(c + 1) * 512]),
                 reads=[xres], writes=[f"{tag}st{c}"])
        P.op("dve", lambda e: e.bn_aggr(out=mv[:], in_=st[:].rearrange("p a b -> p (a b)")),
             reads=[f"{tag}st{c}" for c in range(4)], writes=[tag + "mv"])
        P.op("dve", lambda e: e.tensor_scalar(out=ve[:], in0=mv[:, 1:2], scalar1=EPS, scalar2=None, op0=ALU.add),
             reads=[tag + "mv"], writes=[tag + "ve"])
        P.op("pool", lambda e: e.tensor_tensor(out=rs[:], in0=ve[:], in1=neghalf[:], op=ALU.pow),
             reads=[tag + "ve", "neghalf"], writes=[tag + "rs"])
        P.op("dve", lambda e: e.tensor_scalar(out=x, in0=x, scalar1=mv[:, 0:1], scalar2=rs[:, 0:1],
                                              op0=ALU.subtract, op1=ALU.mult),
             reads=[xres, tag + "mv", tag + "rs"], writes=[xres])
        P.op("dve", lambda e: e.tensor_tensor(out=x, in0=x, in1=g, op=ALU.mult), reads=[xres, gres], writes=[xres])
        P.op("pool", lambda e: e.tensor_tensor(out=x, in0=x, in1=b, op=ALU.add), reads=[xres, bres], writes=[xres])

    def ln_small(stack, tag):
        return (alloc(stack, tag + "st", [128, 4, 6]), alloc(stack, tag + "mv", [128, 2]),
                alloc(stack, tag + "ve", [128, 1]), alloc(stack, tag + "rs", [128, 1]))

    def dump(name, ap, res, shape, dt=F32):
        if name in debug:
            d = dbg_out(name, shape, dt)
            P.dma("sp", "dbg", lambda e: e.dma_start(out=d, in_=ap), reads=[res])

    def phase_ln0():
        with ExitStack() as ph:
            xt = [alloc(ph, f"xt{i}", [128, D]) for i in range(2)]
            g = alloc(ph, "g0", [128, D])
            b = alloc(ph, "b0", [128, D])
            sm = [ln_small(ph, f"l0s{i}") for i in range(2)]
            P.dma("sp", "c5", lambda e: e.dma_start(out=g[:], in_=din["lnin"][0]), writes=["g0"])
            P.dma("sp", "c6", lambda e: e.dma_start(out=b[:], in_=din["lnin"][1]), writes=["b0"])
            for i in range(T // 128):
                s = i % 2
                P.dma("sp", f"ldx{s}", lambda e: e.dma_start(out=xt[s][:], in_=din["x"][i * 128:(i + 1) * 128, :]),
                      writes=[f"xt{s}"])
                layer_norm(xt[s][:], f"xt{s}", g[:], "g0", b[:], "b0", sm[s], f"l0{s}")
                P.dma("sp", f"stx{s}", lambda e: e.dma_start(out=h_tok[i * 128:(i + 1) * 128, :], in_=xt[s][:]),
                      reads=[f"xt{s}"], writes=[("htok", i)])
            P.barrier()

    def load_hT(blk, hT, tokf, tokb):
        for tt in range(4):
            ti = blk * 4 + tt
            s = tt % 2
            P.dma("sp", f"ldh{s}", lambda e: e.dma_start(out=tokf[s][:], in_=h_tok[ti * 128:(ti + 1) * 128, :]),
                  reads=[("htok", ti)], writes=[f"tokf{s}"])
            P.op("act", lambda e: e.activation(out=tokb[s][:], in_=tokf[s][:], func=AF.Copy),
                 reads=[f"tokf{s}"], writes=[f"tokb{s}"])
            for a in range(2):
                pb, pres = ps_next()
                pbb = pb[:].bitcast(BF16)
                for i in range(8):
                    kc = a * 8 + i
                    P.op("pe", lambda e: e.transpose(
                        out=pbb[:, i * 128:(i + 1) * 128], in_=tokb[s][:, kc * 128:(kc + 1) * 128], identity=identb[:]),
                        reads=[f"tokb{s}", "identb"], writes=[pres])
                P.op("dve", lambda e: e.tensor_copy(
                    out=hT[:, a * 8:(a + 1) * 8, tt * 128:(tt + 1) * 128],
                    in_=pbb.rearrange("p (a b) -> p a b", b=128)),
                    reads=[pres], writes=["hT"])

    def proj_tile(wt, wres, ncols_tiles, hT, evac):
        w3 = wt.rearrange("p (k c) -> p k c", c=256)
        for ct in range(ncols_tiles):
            pb, pres = ps_next()
            for kc in range(16):
                P.op("pe", lambda e: e.matmul(
                    pb[:], w3[:, kc, ct * 128:(ct + 1) * 128], hT[:, kc, :], start=(kc == 0), stop=(kc == 15)),
                    reads=[wres, "hT"], writes=[pres])
            evac(ct, pb, pres)

    def cis(A, TH, out_re, out_im, tmp, rA, rTH, rout):
        MG, Q, N1, F, SC = tmp
        P.op("act", lambda e: e.activation(out=MG, in_=A, func=AF.Exp), reads=[rA], writes=["cMG"])
        P.op("dve", lambda e: e.tensor_scalar(out=Q, in0=TH, scalar1=1.0 / TWO_PI, scalar2=None, op0=ALU.mult),
             reads=[rTH], writes=["cQ"])
        for off, o in ((0.0, out_im), (0.25, out_re)):
            P.op("dve", lambda e: e.tensor_scalar(out=N1, in0=Q, scalar1=off + MAGIC, scalar2=None, op0=ALU.add),
                 reads=["cQ"], writes=["cN1"])
            P.op("dve", lambda e: e.tensor_scalar(out=N1, in0=N1, scalar1=-MAGIC, scalar2=None, op0=ALU.add),
                 reads=["cN1"], writes=["cN1"])
            P.op("dve", lambda e: e.scalar_tensor_tensor(out=F, in0=Q, scalar=off, in1=N1, op0=ALU.add, op1=ALU.subtract),
                 reads=["cQ", "cN1"], writes=["cF"])
            P.op("act", lambda e: e.activation(out=SC, in_=F, func=AF.Sin, scale=TWO_PI * (1.0 - 1e-6)),
                 reads=["cF"], writes=["cSC"])
            P.op("dve", lambda e: e.tensor_tensor(out=o, in0=MG, in1=SC, op=ALU.mult),
                 reads=["cMG", "cSC"], writes=[rout])

    def phase_ssm(l):
        with ExitStack() as ph:
            Em = alloc(ph, "Em", [128, 4, 2, 512], BF16)
            Ep = alloc(ph, "Ep", [128, 16, 2, 128], F32)
            Bf = alloc(ph, "Bf", [128, 4, 1024], BF16)
            Cm = alloc(ph, "Cm", [128, 16, 2, 64], BF16)
            dcol = alloc(ph, "dcol", [128, 4], F32)
            P.dma("sp", "c7", lambda e: e.dma_start(out=dcol[:], in_=din["s_d"][l]), writes=["dcol"])
            with ExitStack() as tg:
                stok = alloc(tg, "stok", [128, 3, 2048], F32)
                scm = alloc(tg, "scm", [128, 3, 16], F32)
                sb = alloc(tg, "sb", [128, 5, 256], F32)
                sc = alloc(tg, "sc", [128, 2, 1024], F32)
                sp1 = alloc(tg, "sp1", [128, 1], F32)
                tp1 = alloc(tg, "tp1", [128, 128], F32)
                bmask = alloc(tg, "bmask", [128, 512], F32)
                ctmp = [alloc(tg, f"ct{i}", [128, 512], F32) for i in range(5)]
                tA = alloc(tg, "ctA", [128, 512], F32)
                tT = alloc(tg, "ctT", [128, 512], F32)
                sm = [alloc(tg, f"csm{i}", [128, 256], F32) for i in range(8)]
                arcm = alloc(tg, "arcm", [128, 16], F32)
                aicm = alloc(tg, "aicm", [128, 16], F32)
                dtcm = alloc(tg, "dtcm", [128, 16], F32)
                for i in range(3):
                    P.dma("sp", "c8_1", lambda e: e.dma_start(out=stok[:, i, :], in_=din["s_tok"][l, i]), writes=["stok"])
                    P.dma("sp", "c8_2", lambda e: e.dma_start(out=scm[:, i, :], in_=din["s_cm"][l, i]), writes=["scm"])
                for i in range(5):
                    P.dma("sp", "c8_3", lambda e: e.dma_start(out=sb[:, i, :], in_=din["s_b"][l, i]), writes=["sb"])
                for i in range(2):
                    P.dma("sp", "c8_4", lambda e: e.dma_start(out=sc[:, i, :], in_=din["s_c"][l, i]), writes=["sc"])
                P.dma("sp", "c8_5", lambda e: e.dma_start(out=sp1[:], in_=din["sp1"]), writes=["sp1"])
                P.dma("sp", "c8_6", lambda e: e.dma_start(out=tp1[:], in_=din["tp1"]), writes=["tp1"])
                P.dma("sp", "c8_7", lambda e: e.dma_start(out=bmask[:], in_=din["bmask"]), writes=["bmask"])
                for m in range(4):
                    sl = slice(m * 512, (m + 1) * 512)
                    P.op("act", lambda e: e.activation(out=ctmp[0][:], in_=stok[:, 2, sl], func=AF.Exp),
                         reads=["stok"], writes=["cdt"])
                    P.op("dve", lambda e: e.tensor_tensor(out=tA[:], in0=stok[:, 0, sl], in1=ctmp[0][:], op=ALU.mult),
                         reads=["stok", "cdt"], writes=["cA"])
                    P.op("dve", lambda e: e.tensor_tensor(out=tT[:], in0=stok[:, 1, sl], in1=ctmp[0][:], op=ALU.mult),
                         reads=["stok", "cdt"], writes=["cT"])
                    P.op("dve", lambda e: e.tensor_scalar(out=tA[:], in0=tA[:], scalar1=sp1[:, 0:1], scalar2=-1.0, op0=ALU.mult, op1=ALU.mult),
                         reads=["cA", "sp1"], writes=["cA"])
                    P.op("dve", lambda e: e.tensor_scalar(out=tT[:], in0=tT[:], scalar1=sp1[:, 0:1], scalar2=-1.0, op0=ALU.mult, op1=ALU.mult),
                         reads=["cT", "sp1"], writes=["cT"])
                    cis(tA[:], tT[:], Em[:, m, 0, :], Em[:, m, 1, :], [t[:] for t in ctmp], "cA", "cT", "Em")
                P.op("act", lambda e: e.activation(out=dtcm[:], in_=scm[:, 2, :], func=AF.Exp), reads=["scm"], writes=["dtcm"])
                P.op("dve", lambda e: e.tensor_tensor(out=arcm[:], in0=scm[:, 0, :], in1=dtcm[:], op=ALU.mult), reads=["scm", "dtcm"], writes=["arcm"])
                P.op("dve", lambda e: e.tensor_tensor(out=aicm[:], in0=scm[:, 1, :], in1=dtcm[:], op=ALU.mult), reads=["scm", "dtcm"], writes=["aicm"])

                def r3(t):
                    return t[:].rearrange("p (a b) -> p a b", b=128)
                tb = tp1[:].unsqueeze(1).broadcast_to([128, 4, 128])
                for jg in range(4):
                    P.op("dve", lambda e: e.tensor_tensor(
                        out=r3(tA), in0=tb, in1=arcm[:, 4 * jg:4 * jg + 4].unsqueeze(2).broadcast_to([128, 4, 128]), op=ALU.mult),
                        reads=["tp1", "arcm"], writes=["cA"])
                    P.op("dve", lambda e: e.tensor_tensor(
                        out=r3(tT), in0=tb, in1=aicm[:, 4 * jg:4 * jg + 4].unsqueeze(2).broadcast_to([128, 4, 128]), op=ALU.mult),
                        reads=["tp1", "aicm"], writes=["cT"])
                    cis(r3(tA), r3(tT), Ep[:, 4 * jg:4 * jg + 4, 0, :], Ep[:, 4 * jg:4 * jg + 4, 1, :],
                        [r3(t) for t in ctmp], "cA", "cT", "Ep")

                def s2(t):
                    return t[:, 0:256]
                lre, lim, ldt, bre, bim = (sb[:, i, :] for i in range(5))
                LR, LI, NR, DEN, KR, KI, X1, X2 = (t[:] for t in sm)
                P.op("act", lambda e: e.activation(out=X1, in_=ldt, func=AF.Exp), reads=["sb"], writes=["X1"])
                P.op("dve", lambda e: e.tensor_tensor(out=s2(tA), in0=lre, in1=X1, op=ALU.mult), reads=["sb", "X1"], writes=["cA"])
                P.op("dve", lambda e: e.tensor_tensor(out=s2(tT), in0=lim, in1=X1, op=ALU.mult), reads=["sb", "X1"], writes=["cT"])
                cis(s2(tA), s2(tT), LR, LI, [s2(t) for t in ctmp], "cA", "cT", "LRI")
                P.op("dve", lambda e: e.tensor_scalar(out=NR, in0=LR, scalar1=-1.0, scalar2=None, op0=ALU.add), reads=["LRI"], writes=["NR"])
                P.op("dve", lambda e: e.tensor_tensor(out=X1, in0=lre, in1=lre, op=ALU.mult), reads=["sb"], writes=["X1"])
                P.op("dve", lambda e: e.tensor_tensor(out=X2, in0=lim, in1=lim, op=ALU.mult), reads=["sb"], writes=["X2"])
                P.op("dve", lambda e: e.tensor_tensor(out=DEN, in0=X1, in1=X2, op=ALU.add), reads=["X1", "X2"], writes=["DEN"])
                P.op("dve", lambda e: e.reciprocal(out=DEN, in_=DEN), reads=["DEN"], writes=["DEN"])
                P.op("dve", lambda e: e.tensor_tensor(out=X1, in0=NR, in1=lre, op=ALU.mult), reads=["NR", "sb"], writes=["X1"])
                P.op("dve", lambda e: e.tensor_tensor(out=X2, in0=LI, in1=lim, op=ALU.mult), reads=["LRI", "sb"], writes=["X2"])
                P.op("dve", lambda e: e.tensor_tensor(out=KR, in0=X1, in1=X2, op=ALU.add), reads=["X1", "X2"], writes=["KR"])
                P.op("dve", lambda e: e.tensor_tensor(out=KR, in0=KR, in1=DEN, op=ALU.mult), reads=["KR", "DEN"], writes=["KR"])
                P.op("dve", lambda e: e.tensor_tensor(out=X1, in0=LI, in1=lre, op=ALU.mult), reads=["LRI", "sb"], writes=["X1"])
                P.op("dve", lambda e: e.tensor_tensor(out=X2, in0=NR, in1=lim, op=ALU.mult), reads=["NR", "sb"], writes=["X2"])
                P.op("dve", lambda e: e.tensor_tensor(out=KI, in0=X1, in1=X2, op=ALU.subtract), reads=["X1", "X2"], writes=["KI"])
                P.op("dve", lambda e: e.tensor_tensor(out=KI, in0=KI, in1=DEN, op=ALU.mult), reads=["KI", "DEN"], writes=["KI"])
                P.op("dve", lambda e: e.tensor_tensor(out=X1, in0=KR, in1=bre, op=ALU.mult), reads=["KR", "sb"], writes=["X1"])
                P.op("dve", lambda e: e.tensor_tensor(out=X2, in0=KI, in1=bim, op=ALU.mult), reads=["KI", "sb"], writes=["X2"])
                P.op("dve", lambda e: e.tensor_tensor(out=LR, in0=X1, in1=X2, op=ALU.subtract), reads=["X1", "X2", "LRI"], writes=["BR"])
                P.op("dve", lambda e: e.tensor_tensor(out=X1, in0=KR, in1=bim, op=ALU.mult), reads=["KR", "sb"], writes=["X1"])
                P.op("dve", lambda e: e.tensor_tensor(out=X2, in0=KI, in1=bre, op=ALU.mult), reads=["KI", "sb"], writes=["X2"])
                P.op("dve", lambda e: e.tensor_tensor(out=LI, in0=X1, in1=X2, op=ALU.add), reads=["X1", "X2", "BR"], writes=["BI"])
                bm3 = bmask[:].rearrange("p (a b) -> p a b", b=64)
                for kc in range(4):
                    for r, src, rn in ((0, LR, "BR"), (1, LI, "BI")):
                        P.op("dve", lambda e: e.tensor_tensor(
                            out=Bf[:, kc, r * 512:(r + 1) * 512].rearrange("p (a b) -> p a b", b=64),
                            in0=src[:, kc * 64:(kc + 1) * 64].unsqueeze(1).broadcast_to([128, 8, 64]), in1=bm3, op=ALU.mult),
                            reads=[rn, "bmask"], writes=["Bf"])
                P.op("dve", lambda e: e.tensor_copy(out=Cm[:, :, 0, :], in_=sc[:, 0, :].rearrange("p (a b) -> p a b", b=64)),
                     reads=["sc"], writes=["Cm"])
                P.op("dve", lambda e: e.tensor_scalar(out=Cm[:, :, 1, :], in0=sc[:, 1, :].rearrange("p (a b) -> p a b", b=64),
                                                      scalar1=-1.0, scalar2=None, op0=ALU.mult),
                     reads=["sc"], writes=["Cm"])
                dump("Em", Em[:], "Em", [128, 4, 2, 512], BF16)
                dump("Ep", Ep[:], "Ep", [128, 16, 2, 128])
                dump("Bf", Bf[:], "Bf", [128, 4, 1024], BF16)
                dump("Cm", Cm[:], "Cm", [128, 16, 2, 64], BF16)
                P.barrier()
            hT = alloc(ph, "hT", [128, 16, BLK], BF16)
            tokf = [alloc(ph, f"tokf{i}", [128, D], F32) for i in range(2)]
            tokb = [alloc(ph, f"tokb{i}", [128, D], BF16) for i in range(2)]
            uT = alloc(ph, "uT", [128, 4, BLK], BF16)
            stt = [alloc(ph, f"st{i}", [128, 512], F32) for i in range(6)]
            wri = [[alloc(ph, f"w{a}{r}", [128, 512], BF16) for r in range(2)] for a in range(2)]
            x32 = [alloc(ph, f"x32{r}", [128, 4, 128], F32) for r in range(2)]
            Sc = alloc(ph, "Sc", [128, 4, 4, 2], F32)
            xTq = alloc(ph, "xTq", [128, 4, 2, BLK], BF16)
            zT = alloc(ph, "zT", [128, 4, BLK], BF16)
            soT = alloc(ph, "soT", [128, 4, BLK], BF16)
            t3 = [t[:].rearrange("p (a b) -> p a b", b=128) for t in stt]
            par = 0
            for blk in range(NBLK):
                if blk % 4 == 0:
                    P.op("dve", lambda e: e.memset(Sc[:], 0.0), writes=["Sc"])
                load_hT(blk, hT, tokf, tokb)
                for i, tno in enumerate((6, 7)):
                    wt, wres = WS.next(("win", l, tno))

                    def ev_u(ct, pb, pres):
                        P.op("act", lambda e: e.activation(out=uT[:, 2 * i + ct, :], in_=pb[:], func=AF.Copy),
                             reads=[pres], writes=["uT"])
                    proj_tile(wt, wres, 2, hT, ev_u)
                if blk == 0:
                    dump("uT", uT[:], "uT", [128, 4, BLK], BF16)
                for m in range(4):
                    for ck in range(4):
                        tr = slice(ck * 128, (ck + 1) * 128)
                        pB = [ps_next() for _ in range(2)]
                        for r in range(2):
                            P.op("pe", lambda e: e.matmul(
                                pB[r][0][:], uT[:, m, tr], Bf[:, m, r * 512:(r + 1) * 512], start=True, stop=True),
                                reads=["uT", "Bf"], writes=[pB[r][1]])
                        w_ = wri[par]
                        wn = [f"w{par}{r}" for r in range(2)]
                        Er, Ei = Em[:, m, 0, :], Em[:, m, 1, :]
                        P.op("dve", lambda e: e.tensor_tensor(out=stt[0][:], in0=pB[0][0][:], in1=Er, op=ALU.mult),
                             reads=[pB[0][1], "Em"], writes=["st0"])
                        P.op("dve", lambda e: e.tensor_tensor(out=stt[1][:], in0=pB[1][0][:], in1=Ei, op=ALU.mult),
                             reads=[pB[1][1], "Em"], writes=["st1"])
                        P.op("dve", lambda e: e.tensor_tensor(out=w_[0][:], in0=stt[0][:], in1=stt[1][:], op=ALU.subtract),
                             reads=["st0", "st1"], writes=[wn[0]])
                        P.op("dve", lambda e: e.tensor_tensor(out=stt[2][:], in0=pB[0][0][:], in1=Ei, op=ALU.mult),
                             reads=[pB[0][1], "Em"], writes=["st2"])
                        P.op("dve", lambda e: e.tensor_tensor(out=stt[3][:], in0=pB[1][0][:], in1=Er, op=ALU.mult),
                             reads=[pB[1][1], "Em"], writes=["st3"])
                        P.op("pool", lambda e: e.tensor_tensor(out=w_[1][:], in0=stt[2][:], in1=stt[3][:], op=ALU.add),
                             reads=["st2", "st3"], writes=[wn[1]])
                        pZ = [ps_next() for _ in range(2)]
                        for r in range(2):
                            for ct in range(4):
                                P.op("pe", lambda e: e.matmul(
                                    pZ[r][0][:, ct * 128:(ct + 1) * 128], w_[r][:, ct * 128:(ct + 1) * 128], ltri[:],
                                    start=True, stop=True),
                                    reads=[wn[r], "ltri"], writes=[pZ[r][1]])
                        par ^= 1
                        Epr, Epi = Ep[:, 4 * m:4 * m + 4, 0, :], Ep[:, 4 * m:4 * m + 4, 1, :]
                        for r in range(2):
                            P.op("dve", lambda e: e.tensor_tensor(
                                out=t3[r], in0=pZ[r][0][:].rearrange("p (a b) -> p a b", b=128),
                                in1=Sc[:, m, :, r:r + 1].broadcast_to([128, 4, 128]), op=ALU.add),
                                reads=[pZ[r][1], "Sc"], writes=[f"st{r}"])
                        P.op("pool", lambda e: e.tensor_tensor(out=t3[2], in0=t3[0], in1=Epr, op=ALU.mult), reads=["st0", "Ep"], writes=["st2"])
                        P.op("dve", lambda e: e.tensor_tensor(out=t3[3], in0=t3[1], in1=Epi, op=ALU.mult), reads=["st1", "Ep"], writes=["st3"])
                        P.op("dve", lambda e: e.tensor_tensor(out=x32[0][:], in0=t3[2], in1=t3[3], op=ALU.subtract), reads=["st2", "st3"], writes=["x320"])
                        P.op("pool", lambda e: e.tensor_tensor(out=t3[4], in0=t3[0], in1=Epi, op=ALU.mult), reads=["st0", "Ep"], writes=["st4"])
                        P.op("dve", lambda e: e.tensor_tensor(out=t3[5], in0=t3[1], in1=Epr, op=ALU.mult), reads=["st1", "Ep"], writes=["st5"])
                        P.op("dve", lambda e: e.tensor_tensor(out=x32[1][:], in0=t3[4], in1=t3[5], op=ALU.add), reads=["st4", "st5"], writes=["x321"])
                        for r in range(2):
                            P.op("act", lambda e: e.activation(out=Sc[:, m, :, r:r + 1], in_=x32[r][:, :, 127:128], func=AF.Copy),
                                 reads=[f"x32{r}"], writes=["Sc"])
                            P.op("act", lambda e: e.activation(out=xTq[:, :, r, tr], in_=x32[r][:], func=AF.Copy),
                                 reads=[f"x32{r}"], writes=["xTq"])
                    pY, pYr = ps_next()
                    for ct in range(4):
                        for r in range(2):
                            P.op("pe", lambda e: e.matmul(
                                pY[64 * (ct // 2):64 * (ct // 2) + 64, :], Cm[:, 4 * m + ct, r, :], xTq[:, ct, r, :],
                                start=(ct % 2 == 0 and r == 0), stop=(ct % 2 == 1 and r == 1)),
                                reads=["Cm", "xTq"], writes=[pYr])
                    y, x2, inn, sg = stt[0][:], stt[1][:], stt[2][:], stt[3][:]
                    P.op("dve", lambda e: e.scalar_tensor_tensor(out=y, in0=uT[:, m, :], scalar=dcol[:, m:m + 1], in1=pY[:],
                                                                 op0=ALU.mult, op1=ALU.add),
                         reads=["uT", "dcol", pYr], writes=["st0"])
                    if blk == 0:
                        dump(f"yssm{m}", y, "st0", [128, 512])
                    P.op("pool", lambda e: e.tensor_tensor(out=x2, in0=y, in1=y, op=ALU.mult), reads=["st0"], writes=["st1"])
                    P.op("dve", lambda e: e.tensor_scalar(out=x2, in0=x2, scalar1=0.044715, scalar2=1.0, op0=ALU.mult, op1=ALU.add),
                         reads=["st1"], writes=["st1"])
                    P.op("pool", lambda e: e.tensor_tensor(out=inn, in0=x2, in1=y, op=ALU.mult), reads=["st0", "st1"], writes=["st2"])
                    P.op("act", lambda e: e.activation(out=sg, in_=inn, func=AF.Sigmoid, scale=1.5957691216057308),
                         reads=["st2"], writes=["st3"])
                    P.op("dve", lambda e: e.tensor_tensor(out=zT[:, m, :], in0=y, in1=sg, op=ALU.mult),
                         reads=["st0", "st3"], writes=["zT"])
                wt, wres = WS.next(("wglu", l))
                wg = wt.rearrange("p (k c) -> p k c", c=1024)
                for i in range(4):
                    pV, pG = ps_next(), ps_next()
                    for (pb, pres), off in ((pV, 0), (pG, 512)):
                        for kc in range(4):
                            P.op("pe", lambda e: e.matmul(
                                pb[:], wg[:, kc, off + i * 128:off + (i + 1) * 128], zT[:, kc, :], start=(kc == 0), stop=(kc == 3)),
                                reads=[wres, "zT"], writes=[pres])
                    P.op("act", lambda e: e.activation(out=stt[4][:], in_=pG[0][:], func=AF.Sigmoid), reads=[pG[1]], writes=["st4"])
                    P.op("dve", lambda e: e.tensor_tensor(out=soT[:, i, :], in0=pV[0][:], in1=stt[4][:], op=ALU.mult),
                         reads=[pV[1], "st4"], writes=["soT"])
                P.dma("sp", "sso", lambda e: e.dma_start(out=ssm_scr[blk], in_=soT[:].rearrange("p a b -> p (a b)")),
                      reads=["soT"], writes=[("ssm_scr", blk)])
            P.barrier()

    def phase_mix(l):
        with ExitStack() as ph:
            hT = alloc(ph, "hT", [128, 16, BLK], BF16)
            tokf = [alloc(ph, f"tokf{i}", [128, D], F32) for i in range(2)]
            tokb = [alloc(ph, f"tokb{i}", [128, D], BF16) for i in range(2)]
            qT = alloc(ph, "qT", [128, 8, BLK], BF16)
            kTd = alloc(ph, "kTd", [128, 2, 640], BF16)
            v_tok = alloc(ph, "v_tok", [128, 5, 128], BF16)
            qmT = alloc(ph, "qmT", [128, 4, BLK], BF16)
            aoT = alloc(ph, "aoT", [128, 8, BLK], BF16)
            moT = alloc(ph, "moT", [128, 4, BLK], BF16)
            soT = alloc(ph, "soT", [128, 4, BLK], BF16)
            mergedT = alloc(ph, "mergedT", [128, 16, BLK], BF16)
            wk = alloc(ph, "wk", [128, 4096], F32)
            PT = alloc(ph, "PT", [128, 16, 128], BF16)
            mem_kT = alloc(ph, "mem_kT", [128, 4, 256], BF16)
            mem_v = alloc(ph, "mem_v", [128, 2, 512], BF16)
            lng = alloc(ph, "lng", [128, D], F32)
            lnb = alloc(ph, "lnb", [128, D], F32)
            lsm = [ln_small(ph, f"l1s{i}") for i in range(4)]
            sinks = alloc(ph, "sinks", [128, 16], F32)
            mask_swa = alloc(ph, "mask_swa", [128, 256], F32)
            mask_first = alloc(ph, "mask_first", [128, 256], F32)
            wr = alloc(ph, "wr", [128, 16, 32], F32)
            brt = alloc(ph, "brt", [128, 32], F32)
            ebase = alloc(ph, "ebase", [128, NE], F32)
            cum = alloc(ph, "cum", [128, NE], F32)
            sg = [alloc(ph, f"sg{i}", [128, BLK], F32) for i in range(3)]
            mt = [alloc(ph, f"mt{i}", [128, BLK], F32) for i in range(3)]
            mx = alloc(ph, "mx", [128, 8], F32)
            negm = alloc(ph, "negm", [128, 8], F32)
            ssum = alloc(ph, "ssum", [128, 8], F32)
            t8 = alloc(ph, "t8", [128, 8], F32)
            es8 = alloc(ph, "es8", [128, 8], F32)
            rr = alloc(ph, "rr", [128, 8], F32)
            lg4 = alloc(ph, "lg4", [128, 4, NE], F32)
            top84 = alloc(ph, "top84", [128, 4, 8], F32)
            mb4 = alloc(ph, "mb4", [128, 4, NE], BF16)
            pos4 = alloc(ph, "pos4", [128, 4, NE], F32)
            ov4 = alloc(ph, "ov4", [128, 4, NE], F32)
            mk4 = alloc(ph, "mk4", [128, 4, NE], F32)
            junk4 = alloc(ph, "junk4", [128, 4, NE], F32)
            destf4 = alloc(ph, "destf4", [128, 4, 4], F32)
            nm14 = alloc(ph, "nm14", [128, 4, 1], F32)
            eg4 = alloc(ph, "eg4", [128, 4, 4], F32)
            gs4 = alloc(ph, "gs4", [128, 4, 1], F32)

            memT = mergedT[:, :, 0:256]
            Sm = wk[:, 0:2048].rearrange("p (a b) -> p a b", b=256)
            Pb = wk[:, 2048:3072].bitcast(BF16).rearrange("p (a b) -> p a b", b=256)
            Pn = wk[:, 3072:4096].bitcast(BF16).rearrange("p (a b) -> p a b", b=256)
            rb = [tokf[0][:], tokf[1][:], wk[:, 0:2048], wk[:, 2048:4096]]
            rbres = [["tokf0"], ["tokf1"], ["Sm"], ["Pb", "Pn"]]

            P.dma("sp", "c9_1", lambda e: e.dma_start(out=lng[:], in_=din["lnp"][l, 0]), writes=["lng"])
            P.dma("sp", "c9_2", lambda e: e.dma_start(out=lnb[:], in_=din["lnp"][l, 1]), writes=["lnb"])
            P.dma("sp", "c9_3", lambda e: e.dma_start(out=sinks[:], in_=din["sinks"][l]), writes=["sinks"])
            P.dma("sp", "c9_4", lambda e: e.dma_start(out=mask_swa[:], in_=din["mask_swa"]), writes=["mask_swa"])
            P.dma("sp", "c9_5", lambda e: e.dma_start(out=mask_first[:], in_=din["mask_first"]), writes=["mask_first"])
            P.dma("sp", "c9_6", lambda e: e.dma_start(out=wr[:].rearrange("p a b -> p (a b)"), in_=din["wr"][l]), writes=["wr"])
            P.dma("sp", "c9_7", lambda e: e.dma_start(out=brt[:], in_=din["br"][l]), writes=["brt"])
            P.dma("sp", "c9_8", lambda e: e.dma_start(out=ebase[:], in_=din["ebase"]), writes=["ebase"])
            P.op("dve", lambda e: e.memset(cum[:], 0.0), writes=["cum"])

            def softmax_pv(nh, pieces, sink_ap, do_pv, out_evac):
                for ap3, res, h0 in pieces:
                    n = ap3.shape[1]
                    P.op("dve", lambda e: e.tensor_reduce(out=mx[:, h0:h0 + n], in_=ap3, axis=AX.X, op=ALU.max),
                         reads=[res], writes=["mx"])
                if sink_ap is not None:
                    P.op("dve", lambda e: e.tensor_tensor(out=mx[:, 0:nh], in0=mx[:, 0:nh], in1=sink_ap, op=ALU.max),
                         reads=["mx", "sinks"], writes=["mx"])
                for ap3, res, h0 in pieces:
                    n = ap3.shape[1]
                    P.op("dve", lambda e: e.tensor_tensor(out=Sm[:, h0:h0 + n, :], in0=ap3,
                                                          in1=mx[:, h0:h0 + n].unsqueeze(2).broadcast_to([128, n, 256]), op=ALU.subtract),
                         reads=[res, "mx"], writes=["Sm"])
                P.op("act", lambda e: e.activation(out=Pb[:, 0:nh, :], in_=Sm[:, 0:nh, :], func=AF.Exp), reads=["Sm"], writes=["Pb"])
                P.op("dve", lambda e: e.tensor_reduce(out=ssum[:, 0:nh], in_=Pb[:, 0:nh, :], axis=AX.X, op=ALU.add),
                     reads=["Pb"], writes=["ssum"])
                if sink_ap is not None:
                    P.op("dve", lambda e: e.tensor_tensor(out=t8[:, 0:nh], in0=sink_ap, in1=mx[:, 0:nh], op=ALU.subtract),
                         reads=["mx", "sinks"], writes=["t8"])
                    P.op("act", lambda e: e.activation(out=es8[:, 0:nh], in_=t8[:, 0:nh], func=AF.Exp), reads=["t8"], writes=["es8"])
                    P.op("dve", lambda e: e.tensor_tensor(out=ssum[:, 0:nh], in0=ssum[:, 0:nh], in1=es8[:, 0:nh], op=ALU.add),
                         reads=["ssum", "es8"], writes=["ssum"])
                P.op("dve", lambda e: e.reciprocal(out=rr[:, 0:nh], in_=ssum[:, 0:nh]), reads=["ssum"], writes=["rr"])
                P.op("dve", lambda e: e.tensor_tensor(out=Pn[:, 0:nh, :], in0=Pb[:, 0:nh, :],
                                                      in1=rr[:, 0:nh].unsqueeze(2).broadcast_to([128, nh, 256]), op=ALU.mult),
                     reads=["Pb", "rr"], writes=["Pn"])
                for a in range(nh // 4):
                    pb, pres = ps_next()
                    pbb = pb[:].bitcast(BF16)
                    for j in range(8):
                        idx = a * 8 + j
                        i, kb = idx // 2, idx % 2
                        P.op("pe", lambda e: e.transpose(out=pbb[:, j * 128:(j + 1) * 128], in_=Pn[:, i, kb * 128:(kb + 1) * 128],
                                                         identity=identb[:]),
                             reads=["Pn", "identb"], writes=[pres])
                    P.op("act", lambda e: e.activation(out=PT[:, a * 8:(a + 1) * 8, :], in_=pbb.rearrange("p (a b) -> p a b", b=128),
                                                       func=AF.Copy),
                         reads=[pres], writes=["PT"])
                pso, psores = ps_next()
                do_pv(pso, psores)
                out_evac(pso, psores)

            for blk in range(NBLK):
                seq = blk // 4
                if blk % 4 == 0:
                    for mtile in range(2):
                        s = mtile % 2
                        r0 = seq * 256 + mtile * 128
                        P.dma("sp", f"ldh{s}", lambda e: e.dma_start(out=tokf[s][:], in_=din["mem"][r0:r0 + 128, :]), writes=[f"tokf{s}"])
                        P.op("act", lambda e: e.activation(out=tokb[s][:], in_=tokf[s][:], func=AF.Copy),
                             reads=[f"tokf{s}"], writes=[f"tokb{s}"])
                        for a in range(2):
                            pb, pres = ps_next()
                            pbb = pb[:].bitcast(BF16)
                            for i in range(8):
                                kc = a * 8 + i
                                P.op("pe", lambda e: e.transpose(out=pbb[:, i * 128:(i + 1) * 128],
                                                                 in_=tokb[s][:, kc * 128:(kc + 1) * 128], identity=identb[:]),
                                     reads=[f"tokb{s}", "identb"], writes=[pres])
                            P.op("dve", lambda e: e.tensor_copy(out=memT[:, a * 8:(a + 1) * 8, mtile * 128:(mtile + 1) * 128],
                                                                in_=pbb.rearrange("p (a b) -> p a b", b=128)),
                                 reads=[pres], writes=["mergedT"])
                    for i in range(2):
                        wt, wres = WS.next(("wmkv", l, i))
                        w3 = wt.rearrange("p (k c) -> p k c", c=256)
                        for ct in range(2):
                            pb, pres = ps_next()
                            for kc in range(16):
                                P.op("pe", lambda e: e.matmul(pb[:, 0:256], w3[:, kc, ct * 128:(ct + 1) * 128], memT[:, kc, :],
                                                              start=(kc == 0), stop=(kc == 15)),
                                     reads=[wres, "mergedT"], writes=[pres])
                            P.op("act", lambda e: e.activation(out=mem_kT[:, 2 * i + ct, :], in_=pb[:, 0:256], func=AF.Copy),
                                 reads=[pres], writes=["mem_kT"])
                    for i in range(2):
                        wt, wres = WS.next(("wmkv", l, 2 + i))
                        w3 = wt.rearrange("p (k c) -> p k c", c=256)
                        for mtile in range(2):
                            pb, pres = ps_next()
                            for kc in range(16):
                                P.op("pe", lambda e: e.matmul(pb[:, 0:256], memT[:, kc, mtile * 128:(mtile + 1) * 128], w3[:, kc, :],
                                                              start=(kc == 0), stop=(kc == 15)),
                                     reads=[wres, "mergedT"], writes=[pres])
                            P.op("act", lambda e: e.activation(out=mem_v[:, mtile, i * 256:(i + 1) * 256], in_=pb[:, 0:256], func=AF.Copy),
                                 reads=[pres], writes=["mem_v"])
                    P.op("dve", lambda e: e.memset(kTd[:, :, 0:128], 0.0), writes=["kTd"])
                    P.op("dve", lambda e: e.memset(v_tok[:, 0, :], 0.0), writes=["v_tok"])

                load_hT(blk, hT, tokf, tokb)
                P.dma("sp", "lso", lambda e: e.dma_start(out=soT[:].rearrange("p a b -> p (a b)"), in_=ssm_scr[blk]),
                      reads=[("ssm_scr", blk)], writes=["soT"])
                for i in range(4):
                    wt, wres = WS.next(("win", l, i))

                    def ev_q(ct, pb, pres):
                        P.op("act", lambda e: e.activation(out=qT[:, 2 * i + ct, :], in_=pb[:], func=AF.Copy, scale=0.125),
                             reads=[pres], writes=["qT"])
                    proj_tile(wt, wres, 2, hT, ev_q)
                wt, wres = WS.next(("win", l, 4))

                def ev_k(ct, pb, pres):
                    P.op("act", lambda e: e.activation(out=kTd[:, ct, 128:640], in_=pb[:], func=AF.Copy), reads=[pres], writes=["kTd"])
                proj_tile(wt, wres, 2, hT, ev_k)
                wt, wres = WS.next(("win", l, 5))
                w3 = wt.rearrange("p (k c) -> p k c", c=256)
                for tt in range(4):
                    pb, pres = ps_next()
                    for kc in range(16):
                        P.op("pe", lambda e: e.matmul(pb[:, 0:128], hT[:, kc, tt * 128:(tt + 1) * 128], w3[:, kc, 0:128],
                                                      start=(kc == 0), stop=(kc == 15)),
                             reads=[wres, "hT"], writes=[pres])
                    P.op("act", lambda e: e.activation(out=v_tok[:, 1 + tt, :], in_=pb[:, 0:128], func=AF.Copy), reads=[pres], writes=["v_tok"])
                for i in range(2):
                    wt, wres = WS.next(("win", l, 8 + i))

                    def ev_qm(ct, pb, pres):
                        P.op("act", lambda e: e.activation(out=qmT[:, 2 * i + ct, :], in_=pb[:], func=AF.Copy, scale=128.0 ** -0.5),
                             reads=[pres], writes=["qmT"])
                    proj_tile(wt, wres, 2, hT, ev_qm)
                if blk == 0:
                    dump("qT", qT[:], "qT", [128, 8, BLK], BF16)
                    dump("kTd", kTd[:], "kTd", [128, 2, 640], BF16)
                    dump("v_tok", v_tok[:], "v_tok", [128, 5, 128], BF16)

                for qb in range(4):
                    qs = slice(qb * 128, (qb + 1) * 128)
                    msk = mask_first if (blk % 4 == 0 and qb == 0) else mask_swa
                    mres = "mask_first" if (blk % 4 == 0 and qb == 0) else "mask_swa"
                    for g in range(2):
                        for jp in range(2):
                            for p in range(2):
                                pb, pres = ps_next()
                                for c in range(2):
                                    j = 2 * jp + c
                                    P.op("pe", lambda e: e.matmul(pb[:, c * 256:(c + 1) * 256], qT[p * 64:(p + 1) * 64, 4 * g + j, qs],
                                                                  kTd[p * 64:(p + 1) * 64, g, qb * 128:qb * 128 + 256], start=True, stop=True),
                                         reads=["qT", "kTd"], writes=[pres])
                                s0 = jp * 4 + p * 2
                                P.op("dve", lambda e: e.tensor_tensor(out=Sm[:, s0:s0 + 2, :],
                                                                      in0=pb[:].rearrange("p (a b) -> p a b", b=256),
                                                                      in1=msk[:].unsqueeze(1).broadcast_to([128, 2, 256]), op=ALU.add),
                                     reads=[pres, mres], writes=["Sm"])

                        def pv(pso, psores):
                            for sl in range(8):
                                jp, p, c = sl // 4, (sl // 2) % 2, sl % 2
                                i = 2 * (2 * jp + c) + p
                                for kb in range(2):
                                    P.op("pe", lambda e: e.matmul(
                                        pso[(i % 2) * 64:(i % 2) * 64 + 64, (i // 2) * 128:(i // 2) * 128 + 128],
                                        v_tok[:, qb + kb, g * 64:(g + 1) * 64], PT[:, 2 * sl + kb, :], start=(kb == 0), stop=(kb == 1)),
                                        reads=["v_tok", "PT"], writes=[psores])

                        def oev(pso, psores):
                            P.op("act", lambda e: e.activation(out=aoT[:, 4 * g:4 * g + 4, qs], in_=pso[:].rearrange("p (a b) -> p a b", b=128),
                                                               func=AF.Copy),
                                 reads=[psores], writes=["aoT"])
                        softmax_pv(8, [(Sm[:, 0:8, :], "Sm", 0)], sinks[:, 8 * g:8 * g + 8], pv, oev)
                P.op("dve", lambda e: e.tensor_copy(out=kTd[:, :, 0:128], in_=kTd[:, :, 512:640]), reads=["kTd"], writes=["kTd"])
                P.op("dve", lambda e: e.tensor_copy(out=v_tok[:, 0, :], in_=v_tok[:, 4, :]), reads=["v_tok"], writes=["v_tok"])

                for tt in range(4):
                    ts_ = slice(tt * 128, (tt + 1) * 128)
                    pbs = []
                    for a in range(2):
                        pb, pres = ps_next()
                        pbs.append((pb, pres))
                        for p in range(2):
                            P.op("pe", lambda e: e.matmul(pb[:, p * 256:(p + 1) * 256], qmT[:, 2 * a + p, ts_], mem_kT[:, 2 * a + p, :],
                                                          start=True, stop=True),
                                 reads=["qmT", "mem_kT"], writes=[pres])

                    def pvm(pso, psores):
                        for i in range(4):
                            for kb in range(2):
                                P.op("pe", lambda e: e.matmul(pso[:, i * 128:(i + 1) * 128], mem_v[:, kb, i * 128:(i + 1) * 128],
                                                              PT[:, 2 * i + kb, :], start=(kb == 0), stop=(kb == 1)),
                                     reads=["mem_v", "PT"], writes=[psores])

                    def oevm(pso, psores):
                        P.op("act", lambda e: e.activation(out=moT[:, 0:4, ts_], in_=pso[:].rearrange("p (a b) -> p a b", b=128), func=AF.Copy),
                             reads=[psores], writes=["moT"])
                    softmax_pv(4, [(pbs[x][0][:].rearrange("p (a b) -> p a b", b=256), pbs[x][1], 2 * x) for x in range(2)], None, pvm, oevm)
                if blk == 0:
                    dump("aoT", aoT[:], "aoT", [128, 8, BLK], BF16)
                    dump("moT", moT[:], "moT", [128, 4, BLK], BF16)

                for jj in range(8):
                    wgt = [WS.next(("win", l, NT_IN + jj * 3 + b), held=b) for b in range(3)]
                    wbt, wbres = WS.next(("wbr", l, jj), held=3)
                    wb3 = wbt.rearrange("p (k c) -> p k c", c=256)
                    srcs = [(aoT, "aoT", 0, 8), (soT, "soT", 8, 4), (moT, "moT", 12, 4)]
                    for ct in range(2):
                        cs = slice(ct * 128, (ct + 1) * 128)
                        gps, bps = [], []
                        for b in range(3):
                            pb, pres = ps_next()
                            gps.append((pb, pres))
                            w3 = wgt[b][0].rearrange("p (k c) -> p k c", c=256)
                            for kc in range(16):
                                P.op("pe", lambda e: e.matmul(pb[:], w3[:, kc, cs], hT[:, kc, :], start=(kc == 0), stop=(kc == 15)),
                                     reads=[wgt[b][1], "hT"], writes=[pres])
                            pb2, pres2 = ps_next()
                            bps.append((pb2, pres2))
                            src, sres, k0, nk = srcs[b]
                            for kk in range(nk):
                                P.op("pe", lambda e: e.matmul(pb2[:], wb3[:, k0 + kk, cs], src[:, kk, :], start=(kk == 0), stop=(kk == nk - 1)),
                                     reads=[wbres, sres], writes=[pres2])
                        for b in range(3):
                            P.op("act", lambda e: e.activation(out=sg[b][:], in_=gps[b][0][:], func=AF.Sigmoid), reads=[gps[b][1]], writes=[f"sg{b}"])
                            P.op("dve", lambda e: e.tensor_tensor(out=mt[b][:], in0=bps[b][0][:], in1=sg[b][:], op=ALU.mult),
                                 reads=[bps[b][1], f"sg{b}"], writes=[f"mt{b}"])
                        P.op("pool", lambda e: e.tensor_tensor(out=mt[0][:], in0=mt[0][:], in1=mt[1][:], op=ALU.add), reads=["mt0", "mt1"], writes=["mt0"])
                        P.op("pool", lambda e: e.tensor_tensor(out=mergedT[:, 2 * jj + ct, :], in0=mt[0][:], in1=mt[2][:], op=ALU.add),
                             reads=["mt0", "mt2"], writes=["mergedT"])
                if blk == 0:
                    dump("mergedT", mergedT[:], "mergedT", [128, 16, BLK], BF16)

                for tt in range(4):
                    ti = blk * 4 + tt
                    P.dma("sp", f"ldr{tt}", lambda e: e.dma_start(out=rb[tt], in_=h_tok[ti * 128:(ti + 1) * 128, :]),
                          reads=[("htok", ti)], writes=rbres[tt])
                for c in range(8):
                    wt, wres = WS.next(("wout", l, c))
                    w3 = wt.rearrange("p (k c) -> p k c", c=256)
                    for tt in range(4):
                        pb, pres = ps_next()
                        for kc in range(16):
                            P.op("pe", lambda e: e.matmul(pb[:, 0:256], mergedT[:, kc, tt * 128:(tt + 1) * 128], w3[:, kc, :],
                                                          start=(kc == 0), stop=(kc == 15)),
                                 reads=[wres, "mergedT"], writes=[pres])
                        P.op("dve", lambda e: e.scalar_tensor_tensor(out=rb[tt][:, c * 256:(c + 1) * 256], in0=rb[tt][:, c * 256:(c + 1) * 256],
                                                                     scalar=ALPHA, in1=pb[:, 0:256], op0=ALU.mult, op1=ALU.add),
                             reads=[pres] + rbres[tt], writes=rbres[tt])
                def f32view(t2d, lo):
                    return t2d.bitcast(F32)[:, lo:lo + 2048].rearrange("p (a b) -> p a b", b=128)
                mflat = mergedT[:].rearrange("p a b -> p (a b)")
                h1Ts = [f32view(qT[:].rearrange("p a b -> p (a b)"), 0), f32view(aoT[:].rearrange("p a b -> p (a b)"), 0),
                        f32view(mflat, 0), f32view(mflat, 2048)]
                h1rs = ["qT", "aoT", "mergedT", "mergedT"]
                for tt in range(4):
                    ti = blk * 4 + tt
                    x = rb[tt]
                    xr = rbres[tt]
                    st, mv, ve, rs = lsm[tt]
                    tag = f"l1{tt}"
                    for c in range(4):
                        P.op("dve", lambda e: e.bn_stats(out=st[:, c, :], in_=x[:, c * 512:(c + 1) * 512]), reads=xr, writes=[f"{tag}st{c}"])
                    P.op("dve", lambda e: e.bn_aggr(out=mv[:], in_=st[:].rearrange("p a b -> p (a b)")),
                         reads=[f"{tag}st{c}" for c in range(4)], writes=[tag + "mv"])
                    P.op("dve", lambda e: e.tensor_scalar(out=ve[:], in0=mv[:, 1:2], scalar1=EPS, scalar2=None, op0=ALU.add),
                         reads=[tag + "mv"], writes=[tag + "ve"])
                    P.op("pool", lambda e: e.tensor_tensor(out=rs[:], in0=ve[:], in1=neghalf[:], op=ALU.pow),
                         reads=[tag + "ve", "neghalf"], writes=[tag + "rs"])
                    P.op("dve", lambda e: e.tensor_scalar(out=x, in0=x, scalar1=mv[:, 0:1], scalar2=rs[:, 0:1], op0=ALU.subtract, op1=ALU.mult),
                         reads=xr + [tag + "mv", tag + "rs"], writes=xr)
                    P.op("dve", lambda e: e.tensor_tensor(out=x, in0=x, in1=lng[:], op=ALU.mult), reads=xr + ["lng"], writes=xr)
                    P.op("dve", lambda e: e.tensor_tensor(out=x, in0=x, in1=lnb[:], op=ALU.add), reads=xr + ["lnb"], writes=xr)
                    P.dma("sp", f"sth{tt}", lambda e: e.dma_start(out=h_tok[ti * 128:(ti + 1) * 128, :], in_=x), reads=xr, writes=[("htok", ti)])
                    h1T, h1r = h1Ts[tt], h1rs[tt]
                    for a in range(4):
                        pb, pres = ps_next()
                        for i in range(4):
                            kc = a * 4 + i
                            P.op("pe", lambda e: e.transpose(out=pb[:, i * 128:(i + 1) * 128], in_=x[:, kc * 128:(kc + 1) * 128], identity=identf[:]),
                                 reads=xr + ["identf"], writes=[pres])
                        P.op("dve", lambda e: e.tensor_copy(out=h1T[:, a * 4:(a + 1) * 4, :], in_=pb[:].rearrange("p (a b) -> p a b", b=128)),
                             reads=[pres], writes=[h1r])
                    pb, pres = ps_next()
                    for kc in range(16):
                        P.op("pe", lambda e: e.matmul(pb[:, 0:NE], h1T[:, kc, :], wr[:, kc, :], start=(kc == 0), stop=(kc == 15)),
                             reads=[h1r, "wr"], writes=[pres])
                    P.op("dve", lambda e: e.tensor_tensor(out=lg4[:, tt, :], in0=pb[:, 0:NE], in1=brt[:], op=ALU.add),
                         reads=[pres, "brt"], writes=[f"lg{tt}"])
                    if ti == 0:
                        dump("lg", lg4[:, 0, :], "lg0", [128, NE])
                for tt in range(4):
                    lg, top8, mb, pos, ov = lg4[:, tt, :], top84[:, tt, :], mb4[:, tt, :], pos4[:, tt, :], ov4[:, tt, :]
                    P.op("dve", lambda e: e.max(out=top8, in_=lg), reads=[f"lg{tt}"], writes=[f"top8{tt}"])
                    P.op("dve", lambda e: e.tensor_scalar(out=mb, in0=lg, scalar1=top8[:, 3:4], scalar2=None, op0=ALU.is_ge),
                         reads=[f"lg{tt}", f"top8{tt}"], writes=[f"mb{tt}"])
                    pa, pares = ps_next()
                    P.op("pe", lambda e: e.matmul(pa[:, 0:NE], ltris[:], mb, start=True, stop=True), reads=["ltris", f"mb{tt}"], writes=[pares])
                    P.op("pe", lambda e: e.matmul(pa[:, NE:2 * NE], onesb[:], mb, start=True, stop=True), reads=["onesb", f"mb{tt}"], writes=[pares])
                    P.op("dve", lambda e: e.tensor_tensor(out=pos, in0=pa[:, 0:NE], in1=cum[:], op=ALU.add), reads=[pares, "cum"], writes=[f"pos{tt}"])
                    P.op("dve", lambda e: e.tensor_tensor(out=cum[:], in0=pa[:, NE:2 * NE], in1=cum[:], op=ALU.add), reads=[pares, "cum"], writes=["cum"])
                    P.op("dve", lambda e: e.tensor_scalar(out=ov, in0=pos, scalar1=float(CAP), scalar2=1.0e7, op0=ALU.is_ge, op1=ALU.mult),
                         reads=[f"pos{tt}"], writes=[f"ov{tt}"])
                    P.op("dve", lambda e: e.tensor_tensor(out=pos, in0=pos, in1=ebase[:], op=ALU.add), reads=[f"pos{tt}", "ebase"], writes=[f"pos{tt}"])
                    P.op("dve", lambda e: e.tensor_tensor(out=pos, in0=pos, in1=ov, op=ALU.add), reads=[f"pos{tt}", f"ov{tt}"], writes=[f"pos{tt}"])
                for tt in range(4):
                    ti = blk * 4 + tt
                    s = tt % 2
                    lg, top8, pos = lg4[:, tt, :], top84[:, tt, :], pos4[:, tt, :]
                    mk, junk, destf = mk4[:, tt, :], junk4[:, tt, :], destf4[:, tt, :]
                    nm1, eg, gs = nm14[:, tt, :], eg4[:, tt, :], gs4[:, tt, :]
                    P.op("dve", lambda e: e.tensor_scalar(out=nm1, in0=top8[:, 0:1], scalar1=-1.0, scalar2=None, op0=ALU.mult),
                         reads=[f"top8{tt}"], writes=[f"nm1{tt}"])
                    P.op("act", lambda e: e.activation(out=eg, in_=top8[:, 0:4], func=AF.Exp, bias=nm1[:, 0:1], scale=1.0, accum_out=gs),
                         reads=[f"top8{tt}", f"nm1{tt}"], writes=[f"eg{tt}", f"gs{tt}"])
                    P.op("dve", lambda e: e.reciprocal(out=gs, in_=gs), reads=[f"gs{tt}"], writes=[f"gs{tt}"])
                    P.op("dve", lambda e: e.tensor_scalar(out=gates_all[:, ti, :], in0=eg, scalar1=gs[:, 0:1], scalar2=None, op0=ALU.mult),
                         reads=[f"eg{tt}", f"gs{tt}"], writes=["gates_all"])
                    for k in range(4):
                        P.op("dve", lambda e: e.tensor_scalar(out=mk, in0=lg, scalar1=top8[:, k:k + 1], scalar2=None, op0=ALU.is_equal),
                             reads=[f"lg{tt}", f"top8{tt}"], writes=[f"mk{tt}"])
                        P.op("dve", lambda e: e.tensor_tensor(out=junk, in0=mk, in1=pos, op=ALU.mult),
                             reads=[f"mk{tt}", f"pos{tt}"], writes=[f"junk{tt}"])
                        P.op("dve", lambda e: e.tensor_reduce(out=destf[:, k:k + 1], in_=junk, axis=AX.X, op=ALU.add),
                             reads=[f"junk{tt}"], writes=[f"destf{tt}"])
                        if k == 0:
                            P.op("dve", lambda e: e.tensor_scalar(out=gmat_all[:, ti, :], in0=mk, scalar1=gates_all[:, ti, 0:1], scalar2=None,
                                                                  op0=ALU.mult),
                                 reads=[f"mk{tt}", "gates_all"], writes=["gmat_all"])
                        else:
                            P.op("dve", lambda e: e.scalar_tensor_tensor(out=gmat_all[:, ti, :], in0=mk, scalar=gates_all[:, ti, k:k + 1],
                                                                         in1=gmat_all[:, ti, :], op0=ALU.mult, op1=ALU.add),
                                 reads=[f"mk{tt}", "gates_all", "gmat_all"], writes=["gmat_all"])
                    P.op("act", lambda e: e.activation(out=dest_all[:, ti, :], in_=destf, func=AF.Copy), reads=[f"destf{tt}"], writes=["dest_all"])
                    P.op("act", lambda e: e.activation(out=tokb[s][:], in_=rb[tt], func=AF.Copy), reads=rbres[tt], writes=[f"tokb{s}"])
                    for k in range(4):
                        P.dma("pool", f"sc{s}", lambda e: e.indirect_dma_start(
                            out=xg[:, :], out_offset=bass.IndirectOffsetOnAxis(ap=dest_all[:, ti, k:k + 1], axis=0),
                            in_=tokb[s][:, :], in_offset=None, bounds_check=bc_reg, oob_is_err=False),
                            reads=[f"tokb{s}", "dest_all"], writes=[("xg", ti, k)])
            P.barrier()

    def phase_mix(l):
        with ExitStack() as ph:
            hT = alloc(ph, "hT", [128, 16, BLK], BF16)
            tokf = [alloc(ph, f"tokf{i}", [128, D], F32) for i in range(2)]
            tokb = [alloc(ph, f"tokb{i}", [128, D], BF16) for i in range(2)]
            qT = alloc(ph, "qT", [128, 8, BLK], BF16)
            kTd = alloc(ph, "kTd", [128, 2, 640], BF16)
            v_tok = alloc(ph, "v_tok", [128, 5, 128], BF16)
            qmT = alloc(ph, "qmT", [128, 4, BLK], BF16)
            aoT = alloc(ph, "aoT", [128, 8, BLK], BF16)
            moT = alloc(ph, "moT", [128, 4, BLK], BF16)
            soT = alloc(ph, "soT", [128, 4, BLK], BF16)
            mergedT = alloc(ph, "mergedT", [128, 16, BLK], BF16)
            wk = alloc(ph, "wk", [128, 4096], F32)
            PT = alloc(ph, "PT", [128, 16, 128], BF16)
            mem_kT = alloc(ph, "mem_kT", [128, 4, 256], BF16)
            mem_v = alloc(ph, "mem_v", [128, 2, 512], BF16)
            lng = alloc(ph, "lng", [128, D], F32)
            lnb = alloc(ph, "lnb", [128, D], F32)
            lsm = [ln_small(ph, f"l1s{i}") for i in range(4)]
            sinks = alloc(ph, "sinks", [128, 16], F32)
            mask_swa = alloc(ph, "mask_swa", [128, 256], F32)
            mask_first = alloc(ph, "mask_first", [128, 256], F32)
            wr = alloc(ph, "wr", [128, 16, 32], F32)
            brt = alloc(ph, "brt", [128, 32], F32)
            ebase = alloc(ph, "ebase", [128, NE], F32)
            cum = alloc(ph, "cum", [128, NE], F32)
            sg = [alloc(ph, f"sg{i}", [128, BLK], F32) for i in range(3)]
            mt = [alloc(ph, f"mt{i}", [128, BLK], F32) for i in range(3)]
            mx = alloc(ph, "mx", [128, 8], F32)
            negm = alloc(ph, "negm", [128, 8], F32)
            ssum = alloc(ph, "ssum", [128, 8], F32)
            t8 = alloc(ph, "t8", [128, 8], F32)
            es8 = alloc(ph, "es8", [128, 8], F32)
            rr = alloc(ph, "rr", [128, 8], F32)
            lg = alloc(ph, "lg", [128, NE], F32)
            top8 = alloc(ph, "top8", [128, 8], F32)
            mb = alloc(ph, "mb", [128, NE], BF16)
            pos = alloc(ph, "pos", [128, NE], F32)
            ov = alloc(ph, "ov", [128, NE], F32)
            mk = alloc(ph, "mk", [128, NE], F32)
            junk = alloc(ph, "junk", [128, NE], F32)
            destf = alloc(ph, "destf", [128, 4], F32)
            nm1 = alloc(ph, "nm1", [128, 1], F32)
            eg = alloc(ph, "eg", [128, 4], F32)
            gs = alloc(ph, "gs", [128, 1], F32)

            memT = mergedT[:, :, 0:256]
            Sm = wk[:, 0:2048].rearrange("p (a b) -> p a b", b=256)
            Pb = wk[:, 2048:3072].bitcast(BF16).rearrange("p (a b) -> p a b", b=256)
            Pn = wk[:, 3072:4096].bitcast(BF16).rearrange("p (a b) -> p a b", b=256)
            rb = [tokf[0][:], tokf[1][:], wk[:, 0:2048], wk[:, 2048:4096]]
            rbres = [["tokf0"], ["tokf1"], ["Sm"], ["Pb", "Pn"]]
            h1T = qT[:].rearrange("p a b -> p (a b)").bitcast(F32).rearrange("p (a b) -> p a b", b=128)

            P.dma("sp", "c9_1", lambda e: e.dma_start(out=lng[:], in_=din["lnp"][l, 0]), writes=["lng"])
            P.dma("sp", "c9_2", lambda e: e.dma_start(out=lnb[:], in_=din["lnp"][l, 1]), writes=["lnb"])
            P.dma("sp", "c9_3", lambda e: e.dma_start(out=sinks[:], in_=din["sinks"][l]), writes=["sinks"])
            P.dma("sp", "c9_4", lambda e: e.dma_start(out=mask_swa[:], in_=din["mask_swa"]), writes=["mask_swa"])
            P.dma("sp", "c9_5", lambda e: e.dma_start(out=mask_first[:], in_=din["mask_first"]), writes=["mask_first"])
            P.dma("sp", "c9_6", lambda e: e.dma_start(out=wr[:].rearrange("p a b -> p (a b)"), in_=din["wr"][l]), writes=["wr"])
            P.dma("sp", "c9_7", lambda e: e.dma_start(out=brt[:], in_=din["br"][l]), writes=["brt"])
            P.dma("sp", "c9_8", lambda e: e.dma_start(out=ebase[:], in_=din["ebase"]), writes=["ebase"])
            P.op("dve", lambda e: e.memset(cum[:], 0.0), writes=["cum"])

            def softmax_pv(nh, pieces, sink_ap, do_pv, out_evac):
                for ap3, res, h0 in pieces:
                    n = ap3.shape[1]
                    P.op("dve", lambda e: e.tensor_reduce(out=mx[:, h0:h0 + n], in_=ap3, axis=AX.X, op=ALU.max),
                         reads=[res], writes=["mx"])
                if sink_ap is not None:
                    P.op("dve", lambda e: e.tensor_tensor(out=mx[:, 0:nh], in0=mx[:, 0:nh], in1=sink_ap, op=ALU.max),
                         reads=["mx", "sinks"], writes=["mx"])
                for ap3, res, h0 in pieces:
                    n = ap3.shape[1]
                    P.op("dve", lambda e: e.tensor_tensor(out=Sm[:, h0:h0 + n, :], in0=ap3,
                                                          in1=mx[:, h0:h0 + n].unsqueeze(2).broadcast_to([128, n, 256]), op=ALU.subtract),
                         reads=[res, "mx"], writes=["Sm"])
                P.op("act", lambda e: e.activation(out=Pb[:, 0:nh, :], in_=Sm[:, 0:nh, :], func=AF.Exp), reads=["Sm"], writes=["Pb"])
                P.op("dve", lambda e: e.tensor_reduce(out=ssum[:, 0:nh], in_=Pb[:, 0:nh, :], axis=AX.X, op=ALU.add),
                     reads=["Pb"], writes=["ssum"])
                if sink_ap is not None:
                    P.op("dve", lambda e: e.tensor_tensor(out=t8[:, 0:nh], in0=sink_ap, in1=mx[:, 0:nh], op=ALU.subtract),
                         reads=["mx", "sinks"], writes=["t8"])
                    P.op("act", lambda e: e.activation(out=es8[:, 0:nh], in_=t8[:, 0:nh], func=AF.Exp), reads=["t8"], writes=["es8"])
                    P.op("dve", lambda e: e.tensor_tensor(out=ssum[:, 0:nh], in0=ssum[:, 0:nh], in1=es8[:, 0:nh], op=ALU.add),
                         reads=["ssum", "es8"], writes=["ssum"])
                P.op("dve", lambda e: e.reciprocal(out=rr[:, 0:nh], in_=ssum[:, 0:nh]), reads=["ssum"], writes=["rr"])
                P.op("dve", lambda e: e.tensor_tensor(out=Pn[:, 0:nh, :], in0=Pb[:, 0:nh, :],
                                                      in1=rr[:, 0:nh].unsqueeze(2).broadcast_to([128, nh, 256]), op=ALU.mult),
                     reads=["Pb", "rr"], writes=["Pn"])
                for a in range(nh // 4):
                    pb, pres = ps_next()
                    pbb = pb[:].bitcast(BF16)
                    for j in range(8):
                        idx = a * 8 + j
                        i, kb = idx // 2, idx % 2
                        P.op("pe", lambda e: e.transpose(out=pbb[:, j * 128:(j + 1) * 128], in_=Pn[:, i, kb * 128:(kb + 1) * 128],
                                                         identity=identb[:]),
                             reads=["Pn", "identb"], writes=[pres])
                    P.op("act", lambda e: e.activation(out=PT[:, a * 8:(a + 1) * 8, :], in_=pbb.rearrange("p (a b) -> p a b", b=128),
                                                       func=AF.Copy),
                         reads=[pres], writes=["PT"])
                pso, psores = ps_next()
                do_pv(pso, psores)
                out_evac(pso, psores)

            for blk in range(NBLK):
                seq = blk // 4
                if blk % 4 == 0:
                    for mtile in range(2):
                        s = mtile % 2
                        r0 = seq * 256 + mtile * 128
                        P.dma("sp", f"ldh{s}", lambda e: e.dma_start(out=tokf[s][:], in_=din["mem"][r0:r0 + 128, :]), writes=[f"tokf{s}"])
                        P.op("act", lambda e: e.activation(out=tokb[s][:], in_=tokf[s][:], func=AF.Copy),
                             reads=[f"tokf{s}"], writes=[f"tokb{s}"])
                        for a in range(2):
                            pb, pres = ps_next()
                            pbb = pb[:].bitcast(BF16)
                            for i in range(8):
                                kc = a * 8 + i
                                P.op("pe", lambda e: e.transpose(out=pbb[:, i * 128:(i + 1) * 128],
                                                                 in_=tokb[s][:, kc * 128:(kc + 1) * 128], identity=identb[:]),
                                     reads=[f"tokb{s}", "identb"], writes=[pres])
                            P.op("dve", lambda e: e.tensor_copy(out=memT[:, a * 8:(a + 1) * 8, mtile * 128:(mtile + 1) * 128],
                                                                in_=pbb.rearrange("p (a b) -> p a b", b=128)),
                                 reads=[pres], writes=["mergedT"])
                    for i in range(2):
                        wt, wres = WS.next(("wmkv", l, i))
                        w3 = wt.rearrange("p (k c) -> p k c", c=256)
                        for ct in range(2):
                            pb, pres = ps_next()
                            for kc in range(16):
                                P.op("pe", lambda e: e.matmul(pb[:, 0:256], w3[:, kc, ct * 128:(ct + 1) * 128], memT[:, kc, :],
                                                              start=(kc == 0), stop=(kc == 15)),
                                     reads=[wres, "mergedT"], writes=[pres])
                            P.op("act", lambda e: e.activation(out=mem_kT[:, 2 * i + ct, :], in_=pb[:, 0:256], func=AF.Copy),
                                 reads=[pres], writes=["mem_kT"])
                    for i in range(2):
                        wt, wres = WS.next(("wmkv", l, 2 + i))
                        w3 = wt.rearrange("p (k c) -> p k c", c=256)
                        for mtile in range(2):
                            pb, pres = ps_next()
                            for kc in range(16):
                                P.op("pe", lambda e: e.matmul(pb[:, 0:256], memT[:, kc, mtile * 128:(mtile + 1) * 128], w3[:, kc, :],
                                                              start=(kc == 0), stop=(kc == 15)),
                                     reads=[wres, "mergedT"], writes=[pres])
                            P.op("act", lambda e: e.activation(out=mem_v[:, mtile, i * 256:(i + 1) * 256], in_=pb[:, 0:256], func=AF.Copy),
                                 reads=[pres], writes=["mem_v"])
                    P.op("dve", lambda e: e.memset(kTd[:, :, 0:128], 0.0), writes=["kTd"])
                    P.op("dve", lambda e: e.memset(v_tok[:, 0, :], 0.0), writes=["v_tok"])

                load_hT(blk, hT, tokf, tokb)
                P.dma("sp", "lso", lambda e: e.dma_start(out=soT[:].rearrange("p a b -> p (a b)"), in_=ssm_scr[blk]),
                      reads=[("ssm_scr", blk)], writes=["soT"])
                for i in range(4):
                    wt, wres = WS.next(("win", l, i))

                    def ev_q(ct, pb, pres):
                        P.op("act", lambda e: e.activation(out=qT[:, 2 * i + ct, :], in_=pb[:], func=AF.Copy, scale=0.125),
                             reads=[pres], writes=["qT"])
                    proj_tile(wt, wres, 2, hT, ev_q)
                wt, wres = WS.next(("win", l, 4))

                def ev_k(ct, pb, pres):
                    P.op("act", lambda e: e.activation(out=kTd[:, ct, 128:640], in_=pb[:], func=AF.Copy), reads=[pres], writes=["kTd"])
                proj_tile(wt, wres, 2, hT, ev_k)
                wt, wres = WS.next(("win", l, 5))
                w3 = wt.rearrange("p (k c) -> p k c", c=256)
                for tt in range(4):
                    pb, pres = ps_next()
                    for kc in range(16):
                        P.op("pe", lambda e: e.matmul(pb[:, 0:128], hT[:, kc, tt * 128:(tt + 1) * 128], w3[:, kc, 0:128],
                                                      start=(kc == 0), stop=(kc == 15)),
                             reads=[wres, "hT"], writes=[pres])
                    P.op("act", lambda e: e.activation(out=v_tok[:, 1 + tt, :], in_=pb[:, 0:128], func=AF.Copy), reads=[pres], writes=["v_tok"])
                for i in range(2):
                    wt, wres = WS.next(("win", l, 8 + i))

                    def ev_qm(ct, pb, pres):
                        P.op("act", lambda e: e.activation(out=qmT[:, 2 * i + ct, :], in_=pb[:], func=AF.Copy, scale=128.0 ** -0.5),
                             reads=[pres], writes=["qmT"])
                    proj_tile(wt, wres, 2, hT, ev_qm)
                if blk == 0:
                    dump("qT", qT[:], "qT", [128, 8, BLK], BF16)
                    dump("kTd", kTd[:], "kTd", [128, 2, 640], BF16)
                    dump("v_tok", v_tok[:], "v_tok", [128, 5, 128], BF16)

                for qb in range(4):
                    qs = slice(qb * 128, (qb + 1) * 128)
                    msk = mask_first if (blk % 4 == 0 and qb == 0) else mask_swa
                    mres = "mask_first" if (blk % 4 == 0 and qb == 0) else "mask_swa"
                    for g in range(2):
                        for jp in range(2):
                            for p in range(2):
                                pb, pres = ps_next()
                                for c in range(2):
                                    j = 2 * jp + c
                                    P.op("pe", lambda e: e.matmul(pb[:, c * 256:(c + 1) * 256], qT[p * 64:(p + 1) * 64, 4 * g + j, qs],
                                                                  kTd[p * 64:(p + 1) * 64, g, qb * 128:qb * 128 + 256], start=True, stop=True),
                                         reads=["qT", "kTd"], writes=[pres])
                                s0 = jp * 4 + p * 2
                                P.op("dve", lambda e: e.tensor_tensor(out=Sm[:, s0:s0 + 2, :],
                                                                      in0=pb[:].rearrange("p (a b) -> p a b", b=256),
                                                                      in1=msk[:].unsqueeze(1).broadcast_to([128, 2, 256]), op=ALU.add),
                                     reads=[pres, mres], writes=["Sm"])

                        def pv(pso, psores):
                            for sl in range(8):
                                jp, p, c = sl // 4, (sl // 2) % 2, sl % 2
                                i = 2 * (2 * jp + c) + p
                                for kb in range(2):
                                    P.op("pe", lambda e: e.matmul(
                                        pso[(i % 2) * 64:(i % 2) * 64 + 64, (i // 2) * 128:(i // 2) * 128 + 128],
                                        v_tok[:, qb + kb, g * 64:(g + 1) * 64], PT[:, 2 * sl + kb, :], start=(kb == 0), stop=(kb == 1)),
                                        reads=["v_tok", "PT"], writes=[psores])

                        def oev(pso, psores):
                            P.op("act", lambda e: e.activation(out=aoT[:, 4 * g:4 * g + 4, qs], in_=pso[:].rearrange("p (a b) -> p a b", b=128),
                                                               func=AF.Copy),
                                 reads=[psores], writes=["aoT"])
                        softmax_pv(8, [(Sm[:, 0:8, :], "Sm", 0)], sinks[:, 8 * g:8 * g + 8], pv, oev)
                P.op("dve", lambda e: e.tensor_copy(out=kTd[:, :, 0:128], in_=kTd[:, :, 512:640]), reads=["kTd"], writes=["kTd"])
                P.op("dve", lambda e: e.tensor_copy(out=v_tok[:, 0, :], in_=v_tok[:, 4, :]), reads=["v_tok"], writes=["v_tok"])

                for tt in range(4):
                    ts_ = slice(tt * 128, (tt + 1) * 128)
                    pbs = []
                    for a in range(2):
                        pb, pres = ps_next()
                        pbs.append((pb, pres))
                        for p in range(2):
                            P.op("pe", lambda e: e.matmul(pb[:, p * 256:(p + 1) * 256], qmT[:, 2 * a + p, ts_], mem_kT[:, 2 * a + p, :],
                                                          start=True, stop=True),
                                 reads=["qmT", "mem_kT"], writes=[pres])

                    def pvm(pso, psores):
                        for i in range(4):
                            for kb in range(2):
                                P.op("pe", lambda e: e.matmul(pso[:, i * 128:(i + 1) * 128], mem_v[:, kb, i * 128:(i + 1) * 128],
                                                              PT[:, 2 * i + kb, :], start=(kb == 0), stop=(kb == 1)),
                                     reads=["mem_v", "PT"], writes=[psores])

                    def oevm(pso, psores):
                        P.op("act", lambda e: e.activation(out=moT[:, 0:4, ts_], in_=pso[:].rearrange("p (a b) -> p a b", b=128), func=AF.Copy),
                             reads=[psores], writes=["moT"])
                    softmax_pv(4, [(pbs[x][0][:].rearrange("p (a b) -> p a b", b=256), pbs[x][1], 2 * x) for x in range(2)], None, pvm, oevm)
                if blk == 0:
                    dump("aoT", aoT[:], "aoT", [128, 8, BLK], BF16)
                    dump("moT", moT[:], "moT", [128, 4, BLK], BF16)

                for jj in range(8):
                    wgt = [WS.next(("win", l, NT_IN + jj * 3 + b), held=b) for b in range(3)]
                    wbt, wbres = WS.next(("wbr", l, jj), held=3)
                    wb3 = wbt.rearrange("p (k c) -> p k c", c=256)
                    srcs = [(aoT, "aoT", 0, 8), (soT, "soT", 8, 4), (moT, "moT", 12, 4)]
                    for ct in range(2):
                        cs = slice(ct * 128, (ct + 1) * 128)
                        gps, bps = [], []
                        for b in range(3):
                            pb, pres = ps_next()
                            gps.append((pb, pres))
                            w3 = wgt[b][0].rearrange("p (k c) -> p k c", c=256)
                            for kc in range(16):
                                P.op("pe", lambda e: e.matmul(pb[:], w3[:, kc, cs], hT[:, kc, :], start=(kc == 0), stop=(kc == 15)),
                                     reads=[wgt[b][1], "hT"], writes=[pres])
                            pb2, pres2 = ps_next()
                            bps.append((pb2, pres2))
                            src, sres, k0, nk = srcs[b]
                            for kk in range(nk):
                                P.op("pe", lambda e: e.matmul(pb2[:], wb3[:, k0 + kk, cs], src[:, kk, :], start=(kk == 0), stop=(kk == nk - 1)),
                                     reads=[wbres, sres], writes=[pres2])
                        for b in range(3):
                            P.op("act", lambda e: e.activation(out=sg[b][:], in_=gps[b][0][:], func=AF.Sigmoid), reads=[gps[b][1]], writes=[f"sg{b}"])
                            P.op("dve", lambda e: e.tensor_tensor(out=mt[b][:], in0=bps[b][0][:], in1=sg[b][:], op=ALU.mult),
                                 reads=[bps[b][1], f"sg{b}"], writes=[f"mt{b}"])
                        P.op("pool", lambda e: e.tensor_tensor(out=mt[0][:], in0=mt[0][:], in1=mt[1][:], op=ALU.add), reads=["mt0", "mt1"], writes=["mt0"])
                        P.op("pool", lambda e: e.tensor_tensor(out=mergedT[:, 2 * jj + ct, :], in0=mt[0][:], in1=mt[2][:], op=ALU.add),
                             reads=["mt0", "mt2"], writes=["mergedT"])
                if blk == 0:
                    dump("mergedT", mergedT[:], "mergedT", [128, 16, BLK], BF16)

                for tt in range(4):
                    ti = blk * 4 + tt
                    P.dma("sp", f"ldr{tt}", lambda e: e.dma_start(out=rb[tt], in_=h_tok[ti * 128:(ti + 1) * 128, :]),
                          reads=[("htok", ti)], writes=rbres[tt])
                for c in range(8):
                    wt, wres = WS.next(("wout", l, c))
                    w3 = wt.rearrange("p (k c) -> p k c", c=256)
                    for tt in range(4):
                        pb, pres = ps_next()
                        for kc in range(16):
                            P.op("pe", lambda e: e.matmul(pb[:, 0:256], mergedT[:, kc, tt * 128:(tt + 1) * 128], w3[:, kc, :],
                                                          start=(kc == 0), stop=(kc == 15)),
                                 reads=[wres, "mergedT"], writes=[pres])
                        P.op("dve", lambda e: e.scalar_tensor_tensor(out=rb[tt][:, c * 256:(c + 1) * 256], in0=rb[tt][:, c * 256:(c + 1) * 256],
                                                                     scalar=ALPHA, in1=pb[:, 0:256], op0=ALU.mult, op1=ALU.add),
                             reads=[pres] + rbres[tt], writes=rbres[tt])
                for tt in range(4):
                    ti = blk * 4 + tt
                    s = tt % 2
                    x = rb[tt]
                    xr = rbres[tt]
                    st, mv, ve, rs = lsm[tt]
                    tag = f"l1{tt}"
                    for c in range(4):
                        P.op("dve", lambda e: e.bn_stats(out=st[:, c, :], in_=x[:, c * 512:(c + 1) * 512]), reads=xr, writes=[f"{tag}st{c}"])
                    P.op("dve", lambda e: e.bn_aggr(out=mv[:], in_=st[:].rearrange("p a b -> p (a b)")),
                         reads=[f"{tag}st{c}" for c in range(4)], writes=[tag + "mv"])
                    P.op("dve", lambda e: e.tensor_scalar(out=ve[:], in0=mv[:, 1:2], scalar1=EPS, scalar2=None, op0=ALU.add),
                         reads=[tag + "mv"], writes=[tag + "ve"])
                    P.op("pool", lambda e: e.tensor_tensor(out=rs[:], in0=ve[:], in1=neghalf[:], op=ALU.pow),
                         reads=[tag + "ve", "neghalf"], writes=[tag + "rs"])
                    P.op("dve", lambda e: e.tensor_scalar(out=x, in0=x, scalar1=mv[:, 0:1], scalar2=rs[:, 0:1], op0=ALU.subtract, op1=ALU.mult),
                         reads=xr + [tag + "mv", tag + "rs"], writes=xr)
                    P.op("dve", lambda e: e.tensor_tensor(out=x, in0=x, in1=lng[:], op=ALU.mult), reads=xr + ["lng"], writes=xr)
                    P.op("dve", lambda e: e.tensor_tensor(out=x, in0=x, in1=lnb[:], op=ALU.add), reads=xr + ["lnb"], writes=xr)
                    P.dma("sp", f"sth{tt}", lambda e: e.dma_start(out=h_tok[ti * 128:(ti + 1) * 128, :], in_=x), reads=xr, writes=[("htok", ti)])
                    P.op("act", lambda e: e.activation(out=tokb[s][:], in_=x, func=AF.Copy), reads=xr, writes=[f"tokb{s}"])
                    for a in range(4):
                        pb, pres = ps_next()
                        for i in range(4):
                            kc = a * 4 + i
                            P.op("pe", lambda e: e.transpose(out=pb[:, i * 128:(i + 1) * 128], in_=x[:, kc * 128:(kc + 1) * 128], identity=identf[:]),
                                 reads=xr + ["identf"], writes=[pres])
                        P.op("dve", lambda e: e.tensor_copy(out=h1T[:, a * 4:(a + 1) * 4, :], in_=pb[:].rearrange("p (a b) -> p a b", b=128)),
                             reads=[pres], writes=["qT"])
                    pb, pres = ps_next()
                    for kc in range(16):
                        P.op("pe", lambda e: e.matmul(pb[:, 0:NE], h1T[:, kc, :], wr[:, kc, :], start=(kc == 0), stop=(kc == 15)),
                             reads=["qT", "wr"], writes=[pres])
                    P.op("dve", lambda e: e.tensor_tensor(out=lg[:], in0=pb[:, 0:NE], in1=brt[:], op=ALU.add), reads=[pres, "brt"], writes=["lg"])
                    if ti == 0:
                        dump("lg", lg[:], "lg", [128, NE])
                    P.op("dve", lambda e: e.max(out=top8[:], in_=lg[:]), reads=["lg"], writes=["top8"])
                    P.op("dve", lambda e: e.tensor_scalar(out=mb[:], in0=lg[:], scalar1=top8[:, 3:4], scalar2=None, op0=ALU.is_ge),
                         reads=["lg", "top8"], writes=["mb"])
                    pa, pares = ps_next()
                    P.op("pe", lambda e: e.matmul(pa[:, 0:NE], ltris[:], mb[:], start=True, stop=True), reads=["ltris", "mb"], writes=[pares])
                    P.op("pe", lambda e: e.matmul(pa[:, NE:2 * NE], onesb[:], mb[:], start=True, stop=True), reads=["onesb", "mb"], writes=[pares])
                    P.op("dve", lambda e: e.tensor_tensor(out=pos[:], in0=pa[:, 0:NE], in1=cum[:], op=ALU.add), reads=[pares, "cum"], writes=["pos"])
                    P.op("dve", lambda e: e.tensor_tensor(out=cum[:], in0=pa[:, NE:2 * NE], in1=cum[:], op=ALU.add), reads=[pares, "cum"], writes=["cum"])
                    P.op("dve", lambda e: e.tensor_scalar(out=ov[:], in0=pos[:], scalar1=float(CAP), scalar2=1.0e7, op0=ALU.is_ge, op1=ALU.mult),
                         reads=["pos"], writes=["ov"])
                    P.op("dve", lambda e: e.tensor_tensor(out=pos[:], in0=pos[:], in1=ebase[:], op=ALU.add), reads=["pos", "ebase"], writes=["pos"])
                    P.op("dve", lambda e: e.tensor_tensor(out=pos[:], in0=pos[:], in1=ov[:], op=ALU.add), reads=["pos", "ov"], writes=["pos"])
                    P.op("dve", lambda e: e.tensor_scalar(out=nm1[:], in0=top8[:, 0:1], scalar1=-1.0, scalar2=None, op0=ALU.mult),
                         reads=["top8"], writes=["nm1"])
                    P.op("act", lambda e: e.activation(out=eg[:], in_=top8[:, 0:4], func=AF.Exp, bias=nm1[:, 0:1], scale=1.0, accum_out=gs[:]),
                         reads=["top8", "nm1"], writes=["eg", "gs"])
                    P.op("dve", lambda e: e.reciprocal(out=gs[:], in_=gs[:]), reads=["gs"], writes=["gs"])
                    P.op("dve", lambda e: e.tensor_scalar(out=gates_all[:, ti, :], in0=eg[:], scalar1=gs[:, 0:1], scalar2=None, op0=ALU.mult),
                         reads=["eg", "gs"], writes=["gates_all"])
                    for k in range(4):
                        P.op("dve", lambda e: e.tensor_scalar(out=mk[:], in0=lg[:], scalar1=top8[:, k:k + 1], scalar2=None, op0=ALU.is_equal),
                             reads=["lg", "top8"], writes=["mk"])
                        P.op("dve", lambda e: e.tensor_tensor(out=junk[:], in0=mk[:], in1=pos[:], op=ALU.mult),
                             reads=["mk", "pos"], writes=["junk"])
                        P.op("dve", lambda e: e.tensor_reduce(out=destf[:, k:k + 1], in_=junk[:], axis=AX.X, op=ALU.add),
                             reads=["junk"], writes=["destf"])
                        if k == 0:
                            P.op("dve", lambda e: e.tensor_scalar(out=gmat_all[:, ti, :], in0=mk[:], scalar1=gates_all[:, ti, 0:1], scalar2=None,
                                                                  op0=ALU.mult),
                                 reads=["mk", "gates_all"], writes=["gmat_all"])
                        else:
                            P.op("dve", lambda e: e.scalar_tensor_tensor(out=gmat_all[:, ti, :], in0=mk[:], scalar=gates_all[:, ti, k:k + 1],
                                                                         in1=gmat_all[:, ti, :], op0=ALU.mult, op1=ALU.add),
                                 reads=["mk", "gates_all", "gmat_all"], writes=["gmat_all"])
                    P.op("act", lambda e: e.activation(out=dest_all[:, ti, :], in_=destf[:], func=AF.Copy), reads=["destf"], writes=["dest_all"])
                    for k in range(4):
                        P.dma("pool", f"sc{s}", lambda e: e.indirect_dma_start(
                            out=xg[:, :], out_offset=bass.IndirectOffsetOnAxis(ap=dest_all[:, ti, k:k + 1], axis=0),
                            in_=tokb[s][:, :], in_offset=None, bounds_check=bc_reg, oob_is_err=False),
                            reads=[f"tokb{s}", "dest_all"], writes=[("xg", ti, k)])
            P.barrier()

    STILES = [(i * 128, min(128, CAP - i * 128)) for i in range((CAP + 127) // 128)]
    NH = CAP // 2

    def phase_experts(l):
        with ExitStack() as ph:
            NST = len(STILES)
            xrow = [[alloc(ph, f"xrow{a}{i}", [128, D], BF16) for i in range(NST)] for a in range(2)]
            xeTs = [alloc(ph, f"xeT{i}", [128, 16, CAP], BF16) for i in range(2)]
            actT = alloc(ph, "actT", [128, 8, CAP], BF16)
            yst = alloc(ph, "yst", [128, len(STILES), D], BF16)
            bup = alloc(ph, "bup", [128, NE, 16], F32)
            tg = [alloc(ph, f"tg{i}", [128, NH], F32) for i in range(2)]
            tsg = [alloc(ph, f"tsg{i}", [128, NH], F32) for i in range(2)]
            tl = [alloc(ph, f"tl{i}", [128, NH], F32) for i in range(2)]
            P.dma("sp", "c10", lambda e: e.dma_start(out=bup[:].rearrange("p a b -> p (a b)"), in_=din["bup"][l]), writes=["bup"])
            par = 0

            def load_rows(ex):
                a = ex % 2
                for si, (r0, rows) in enumerate(STILES):
                    P.dma("sp", f"ldxg{a}{si}", lambda e: e.dma_start(out=xrow[a][si][0:rows, :],
                                                                       in_=xg[ex * CAP + r0:ex * CAP + r0 + rows, :]),
                          writes=[f"xrow{a}{si}"])
            load_rows(0)
            for ex in range(NE):
                base = ex * CAP
                xeT = xeTs[ex % 2]
                xres = f"xeT{ex % 2}"
                if ex + 1 < NE:
                    load_rows(ex + 1)
                for si, (r0, rows) in enumerate(STILES):
                    xr_ = xrow[ex % 2][si]
                    xrr = f"xrow{ex % 2}{si}"
                    for a in range(2):
                        pb, pres = ps_next()
                        pbb = pb[:].bitcast(BF16)
                        for i in range(8):
                            kc = a * 8 + i
                            P.op("pe", lambda e: e.transpose(out=pbb[:, i * 128:i * 128 + rows], in_=xr_[0:rows, kc * 128:(kc + 1) * 128],
                                                             identity=identb[0:rows, 0:rows]),
                                 reads=[xrr, "identb"], writes=[pres])
                        P.op("dve", lambda e: e.tensor_copy(out=xeT[:, a * 8:(a + 1) * 8, r0:r0 + rows],
                                                            in_=pbb.rearrange("p (a b) -> p a b", b=128)[:, :, 0:rows]),
                             reads=[pres], writes=[xres])
                for j in range(8):
                    wt, wres = WS.next(("wup", l, ex, j))
                    w3 = wt.rearrange("p (k c) -> p k c", c=256)
                    for nh in range(2):
                        ns = slice(nh * NH, (nh + 1) * NH)
                        pg, pgres = ps_next()
                        pl, plres = ps_next()
                        for kc in range(16):
                            P.op("pe", lambda e: e.matmul(pg[:, 0:NH], w3[:, kc, 0:128], xeT[:, kc, ns], start=(kc == 0), stop=(kc == 15)),
                                 reads=[wres, xres], writes=[pgres])
                        for kc in range(16):
                            P.op("pe", lambda e: e.matmul(pl[:, 0:NH], w3[:, kc, 128:256], xeT[:, kc, ns], start=(kc == 0), stop=(kc == 15)),
                                 reads=[wres, xres], writes=[plres])
                        q = par
                        par ^= 1
                        P.op("dve", lambda e: e.tensor_scalar(out=tg[q][:], in0=pg[:, 0:NH], scalar1=bup[:, ex, j:j + 1], scalar2=7.0,
                                                              op0=ALU.add, op1=ALU.min),
                             reads=[pgres, "bup"], writes=[f"tg{q}"])
                        P.op("act", lambda e: e.activation(out=tsg[q][:], in_=tg[q][:], func=AF.Sigmoid, scale=1.702),
                             reads=[f"tg{q}"], writes=[f"tsg{q}"])
                        P.op("dve", lambda e: e.tensor_scalar(out=tl[q][:], in0=pl[:, 0:NH], scalar1=bup[:, ex, 8 + j:9 + j], scalar2=-7.0,
                                                              op0=ALU.add, op1=ALU.max),
                             reads=[plres, "bup"], writes=[f"tl{q}"])
                        P.op("dve", lambda e: e.tensor_scalar(out=tl[q][:], in0=tl[q][:], scalar1=7.0, scalar2=1.0, op0=ALU.min, op1=ALU.add),
                             reads=[f"tl{q}"], writes=[f"tl{q}"])
                        P.op("dve", lambda e: e.tensor_tensor(out=tg[q][:], in0=tg[q][:], in1=tsg[q][:], op=ALU.mult),
                             reads=[f"tg{q}", f"tsg{q}"], writes=[f"tg{q}"])
                        P.op("dve", lambda e: e.tensor_tensor(out=actT[:, j, ns], in0=tg[q][:], in1=tl[q][:], op=ALU.mult),
                             reads=[f"tg{q}", f"tl{q}"], writes=["actT"])
                for c in range(8):
                    wt, wres = WS.next(("wdn", l, ex, c))
                    w3 = wt.rearrange("p (k c) -> p k c", c=256)
                    for si, (r0, rows) in enumerate(STILES):
                        pb, pres = ps_next()
                        for k in range(8):
                            P.op("pe", lambda e: e.matmul(pb[0:rows, 0:256], actT[:, k, r0:r0 + rows], w3[:, k, :], start=(k == 0), stop=(k == 7)),
                                 reads=[wres, "actT"], writes=[pres])
                        P.op("act", lambda e: e.activation(out=yst[0:rows, si, c * 256:(c + 1) * 256], in_=pb[0:rows, 0:256], func=AF.Copy),
                             reads=[pres], writes=[("yst", si)])
                for si, (r0, rows) in enumerate(STILES):
                    P.dma("sp", f"sty{si}", lambda e: e.dma_start(out=ypad[base + r0:base + r0 + rows, :], in_=yst[0:rows, si, :]),
                          reads=[("yst", si)], writes=[("ypad", ex, si)])
            P.barrier()

    def phase_combine(l, dst):
        with ExitStack() as ph:
            yk = [[alloc(ph, f"yk{s}{k}", [128, D], BF16) for k in range(4)] for s in range(2)]
            hb = [alloc(ph, f"hb{i}", [128, D], F32) for i in range(2)]
            lng = alloc(ph, "lng2", [128, D], F32)
            lnb = alloc(ph, "lnb2", [128, D], F32)
            bdn = alloc(ph, "bdn", [NE, D], F32)
            gT = [alloc(ph, f"gT{i}", [NE, 128], F32) for i in range(2)]
            lsm = [ln_small(ph, f"l2s{i}") for i in range(2)]
            P.dma("sp", "c11", lambda e: e.dma_start(out=lng[:], in_=din["lnp"][l, 2]), writes=["lng2"])
            P.dma("sp", "c12", lambda e: e.dma_start(out=lnb[:], in_=din["lnp"][l, 3]), writes=["lnb2"])
            P.dma("sp", "c13", lambda e: e.dma_start(out=bdn[:], in_=din["bdn"][l]), writes=["bdn"])
            for ti in range(T // 128):
                s = ti % 2
                for k in range(4):
                    P.dma("pool", f"ga{s}{k}", lambda e: e.indirect_dma_start(
                        out=yk[s][k][:, :], out_offset=None, in_=ypad[:, :],
                        in_offset=bass.IndirectOffsetOnAxis(ap=dest_all[:, ti, k:k + 1], axis=0),
                        bounds_check=bc_reg, oob_is_err=False),
                        reads=["dest_all"], writes=[f"yk{s}{k}"])
                P.dma("sp", f"ldh2{s}", lambda e: e.dma_start(out=hb[s][:], in_=h_tok[ti * 128:(ti + 1) * 128, :]),
                      reads=[("htok", ti)], writes=[f"hb{s}"])
                pt, ptres = ps_next()
                P.op("pe", lambda e: e.transpose(out=pt[0:NE, 0:128], in_=gmat_all[:, ti, :], identity=identf[:]),
                     reads=["gmat_all", "identf"], writes=[ptres])
                P.op("dve", lambda e: e.tensor_copy(out=gT[s][:], in_=pt[0:NE, 0:128]), reads=[ptres], writes=[f"gT{s}"])
                for cgi in range(4):
                    cs = slice(cgi * 512, (cgi + 1) * 512)
                    pb, pres = ps_next()
                    P.op("pe", lambda e: e.matmul(pb[:], gT[s][:], bdn[:, cs], start=True, stop=True), reads=[f"gT{s}", "bdn"], writes=[pres])
                    P.op("dve", lambda e: e.scalar_tensor_tensor(out=hb[s][:, cs], in0=hb[s][:, cs], scalar=ALPHA, in1=pb[:], op0=ALU.mult, op1=ALU.add),
                         reads=[pres, f"hb{s}"], writes=[f"hb{s}"])
                for k in range(4):
                    P.op("dve", lambda e: e.scalar_tensor_tensor(out=hb[s][:], in0=yk[s][k][:], scalar=gates_all[:, ti, k:k + 1], in1=hb[s][:],
                                                                 op0=ALU.mult, op1=ALU.add),
                         reads=[f"yk{s}{k}", "gates_all", f"hb{s}"], writes=[f"hb{s}"])
                layer_norm(hb[s][:], f"hb{s}", lng[:], "lng2", lnb[:], "lnb2", lsm[s], f"l2{s}")
                P.dma("sp", f"sto{s}", lambda e: e.dma_start(out=dst[ti * 128:(ti + 1) * 128, :], in_=hb[s][:]),
                      reads=[f"hb{s}"], writes=[("htok", ti)])
            P.barrier()

    phase_ln0()
    for l in range(nlayers):
        if stop == "ln0":
            break
        phase_ssm(l)
        if stop == "ssm":
            break
        phase_mix(l)
        if stop == "mix":
            break
        phase_experts(l)
        if stop == "exp":
            break
        phase_combine(l, out if l == nlayers - 1 else h_tok)
    P.finish()
    return nc, dbg


def _kc(W):
    K, C = W.shape
    return np.ascontiguousarray(W.reshape(K // 128, 128, C).transpose(1, 0, 2)).reshape(128, (K // 128) * C)


def _rep(v, n=128):
    return np.ascontiguousarray(np.broadcast_to(np.asarray(v, np.float32)[None, :], (n, len(v))))


def prep_shared(inp, moe=True):
    f = np.float32
    sh = {}
    sh["lnin"] = np.stack([_rep(inp["ln_in_g"]), _rep(inp["ln_in_b"])])
    sh["lnp"] = np.stack([np.stack([_rep(inp[k][l]) for k in ("ln1_g", "ln1_b", "ln2_g", "ln2_b")]) for l in range(L)])
    win = np.zeros((L, NT_IN + 24, 128, 4096), f)
    for l in range(L):
        W = inp["w_in"][l]
        k0 = W[:, 1024:1088]
        k1 = W[:, 1088:1152]
        tiles = [W[:, i * 256:(i + 1) * 256] for i in range(4)]
        tiles.append(np.concatenate([k0, k0, k1, k1], axis=1))
        tiles.append(np.concatenate([W[:, 1152:1280], np.zeros((D, 128), f)], axis=1))
        tiles += [W[:, 1280 + i * 256:1280 + (i + 1) * 256] for i in range(2)]
        tiles += [W[:, 1792 + i * 256:1792 + (i + 1) * 256] for i in range(2)]
        for jj in range(8):
            for b in range(3):
                c0 = 2304 + b * 2048 + jj * 256
                tiles.append(W[:, c0:c0 + 256])
        for i, t in enumerate(tiles):
            win[l, i] = _kc(t)
    sh["win"] = win
    sh["wbr"] = np.stack([np.stack([_kc(inp["w_branch"][l][:, j * 256:(j + 1) * 256]) for j in range(8)]) for l in range(L)])
    sh["wout"] = np.stack([np.stack([_kc(inp["w_out"][l][:, j * 256:(j + 1) * 256]) for j in range(8)]) for l in range(L)])
    sh["wglu"] = np.stack([_kc(inp["w_glu"][l]) for l in range(L)])
    sh["wmkv"] = np.stack([np.stack([_kc(inp["w_mem_kv"][l][:, j * 256:(j + 1) * 256]) for j in range(4)]) for l in range(L)])
    sh["wr"] = np.stack([_kc(inp["w_router"][l]) for l in range(L)])
    sh["br"] = np.stack([_rep(inp["b_router"][l]) for l in range(L)])
    wup = np.empty((L, NE, 8, 128, 4096), f) if moe else None
    wdn = np.empty((L, NE, 8, 128, 2048), f) if moe else None
    bup = np.empty((L, 128, NE, 16), f)
    for l in range(L):
        for e in range(NE):
            bu = inp["b_up"][l, e]
            bup[l, :, e, 0:8] = bu[0::2].reshape(8, 128).T
            bup[l, :, e, 8:16] = bu[1::2].reshape(8, 128).T
            if not moe:
                continue
            Wu = inp["w_up"][l, e]
            g, li = Wu[:, 0::2], Wu[:, 1::2]
            for j in range(8):
                wup[l, e, j] = _kc(np.concatenate([g[:, j * 128:(j + 1) * 128], li[:, j * 128:(j + 1) * 128]], axis=1))
            Wd = inp["w_down"][l, e]
            for c in range(8):
                wdn[l, e, c] = _kc(Wd[:, c * 256:(c + 1) * 256])
    if moe:
        sh["wup"], sh["wdn"] = wup, wdn
    sh["bup"] = bup.reshape(L, 128, NE * 16)
    sh["bdn"] = np.ascontiguousarray(inp["b_down"]).astype(f)
    slot2head = [8 * g + 2 * (2 * (s // 4) + s % 2) + (s // 2) % 2 for g in range(2) for s in range(8)]
    sh["sinks"] = np.stack([_rep(np.asarray(inp["attn_sinks"][l])[slot2head]) for l in range(L)])
    sh["ident"] = np.eye(128, dtype=f)
    s_, t_ = np.meshgrid(np.arange(128), np.arange(128), indexing="ij")
    sh["ltri"] = (s_ <= t_).astype(f)
    sh["ltris"] = (s_ < t_).astype(f)
    sh["ones"] = np.ones((128, 128), f)
    q_, k_ = np.meshgrid(np.arange(128), np.arange(256), indexing="ij")
    valid = (k_ > q_) & (k_ <= q_ + 128)
    sh["mask_swa"] = np.where(valid, 0.0, -30000.0).astype(f)
    sh["mask_first"] = np.where(valid & (k_ >= 128), 0.0, -30000.0).astype(f)
    sh["ebase"] = _rep(np.arange(NE, dtype=f) * CAP)
    sh["sp1"] = (np.arange(128, dtype=f) + 1.0).reshape(128, 1)
    sh["tp1"] = _rep(np.arange(128, dtype=f) + 1.0)
    pp = np.arange(128)[:, None, None] // 16
    sh["bmask"] = np.broadcast_to((pp == np.arange(8)[None, :, None]), (128, 8, 64)).astype(f).reshape(128, 512)
    s_tok = np.empty((L, 3, 128, 2048), f)
    s_cm = np.empty((L, 3, 128, 16), f)
    s_b = np.empty((L, 5, 128, 256), f)
    s_c = np.zeros((L, 2, 128, 16, 4, 16), f)
    s_d = np.empty((L, 128, 4), f)
    for l in range(L):
        lre, lim = inp["ssm_lambda_re"][l], inp["ssm_lambda_im"][l]
        ldt = np.broadcast_to(inp["ssm_log_dt"][l][:, None], (32, 64))
        for i, a in enumerate((lre, lim, ldt)):
            flat = np.ascontiguousarray(a).reshape(2048)
            s_tok[l, i] = _rep(flat)
            s_cm[l, i] = flat.reshape(16, 128).T
            s_b[l, i] = np.broadcast_to(np.ascontiguousarray(a).reshape(4, 8, 1, 64), (4, 8, 16, 64)).transpose(1, 2, 0, 3).reshape(128, 256)
        for i, bb in enumerate((inp["ssm_b_re"][l], inp["ssm_b_im"][l])):
            s_b[l, 3 + i] = bb.reshape(4, 8, 64, 16).transpose(1, 3, 0, 2).reshape(128, 256)
        for i, cc in enumerate((inp["ssm_c_re"][l], inp["ssm_c_im"][l])):
            c4 = cc.reshape(16, 2, 16, 64)
            for g2 in range(2):
                for par in range(2):
                    s_c[l, i, g2 * 64:(g2 + 1) * 64, par::2, 2 * par + g2, :] = c4[par::2, g2].transpose(2, 0, 1)
        s_d[l] = inp["ssm_d"][l].reshape(4, 128).T
    sh["s_tok"], sh["s_cm"], sh["s_b"], sh["s_d"] = s_tok, s_cm, s_b, s_d
    sh["s_c"] = s_c.reshape(L, 2, 128, 1024)
    return {k: np.ascontiguousarray(v, dtype=f) for k, v in sh.items()}


def prep_core(inp, c):
    x = np.ascontiguousarray(inp["x"][NSEQ * c:NSEQ * (c + 1)]).reshape(T, D).astype(np.float32)
    mem = np.ascontiguousarray(inp["mem"][NSEQ * c:NSEQ * (c + 1)]).reshape(NSEQ * 256, D).astype(np.float32)
    return {"x": x, "mem": mem}


_CACHE = {}


def kernel(**inputs):
    inp = {k: np.asarray(v) for k, v in inputs.items()}
    sh = prep_shared(inp)
    if "nc" not in _CACHE:
        _CACHE["nc"] = build()[0]
    nc = _CACHE["nc"]
    in_maps = [{**sh, **prep_core(inp, c)} for c in range(NCORES)]
    res = run_bass_kernel_spmd(nc, in_maps, core_ids=list(range(NCORES)))
    outs = [np.asarray(r["out"]).reshape(NSEQ, S, D) for r in res.results]
    return np.concatenate(outs, axis=0).astype(np.float32)
```
